# Optimizing a Trainium2 kernel written in Bass

```python
import jax, jax.numpy as jnp
from jax import lax
import numpy as np

D_MODEL = 1024
BATCH = 8
SEQ = 4096
DEPTH = 4

N_EVEN = (DEPTH + 1) // 2
N_ODD = DEPTH // 2
EPS = 1e-6
ROPE_THETA = 10000.0
ROPE_DIM = 64
Q_BLOCK = 128

MLA_HEADS = 8
MLA_Q_LORA = 384
MLA_KV_LORA = 256
MLA_NOPE = 128
MLA_ROPE = ROPE_DIM
MLA_V = 128

SWA_Q_HEADS = 16
SWA_KV_HEADS = 2
SWA_HEAD_DIM = ROPE_DIM
SWA_RADIUS = 128

DIL_PATTERN = ((128, 1), (512, 4), (2048, 16))
DIL_GROUPS = len(DIL_PATTERN)
DIL_HEADS = 16
DIL_HEAD_DIM = ROPE_DIM

N_EXPERTS = 32
TOP_K = 4
D_FF = D_MODEL
SWIGLU_LIMIT = 7.0
SWIGLU_ALPHA = 1.702
MOE_BLOCK = 256

AB_SPLITS = (MLA_Q_LORA, MLA_KV_LORA, MLA_ROPE, SWA_Q_HEADS * SWA_HEAD_DIM,
             SWA_KV_HEADS * SWA_HEAD_DIM, SWA_KV_HEADS * SWA_HEAD_DIM)
AB_IN = sum(AB_SPLITS)
AB_MIX = MLA_HEADS * MLA_V + SWA_Q_HEADS * SWA_HEAD_DIM
C_IN = DIL_GROUPS * 3 * DIL_HEADS * DIL_HEAD_DIM
C_MIX = DIL_HEADS * DIL_HEAD_DIM

kernel_name = "hybrid_mla_swa_dilated_moe_encoder"


def rmsnorm(x, gain):
    xf = x.astype(jnp.float32)
    y = xf * lax.rsqrt(jnp.mean(xf * xf, axis=-1, keepdims=True) + EPS)
    return (y * gain.astype(jnp.float32)).astype(x.dtype)


def ada_norm(x, gain, shift, scale):
    h = rmsnorm(x, gain).astype(jnp.float32)
    return (h * (1.0 + scale[:, None, :]) + shift[:, None, :]).astype(x.dtype)


def rope_tables(positions):
    inv = ROPE_THETA ** (-jnp.arange(0, ROPE_DIM, 2, dtype=jnp.float32) / ROPE_DIM)
    ang = positions.astype(jnp.float32)[..., None] * inv
    return jnp.cos(ang), jnp.sin(ang)


def apply_rope(x, cos, sin):
    xf = x.astype(jnp.float32)
    x1, x2 = jnp.split(xf, 2, axis=-1)
    c = cos[:, :, None, :]
    s = sin[:, :, None, :]
    return jnp.concatenate([x1 * c - x2 * s, x2 * c + x1 * s], axis=-1).astype(x.dtype)


def dense_attention(q, k, v):
    B, S, H, dq = q.shape
    scale = dq ** -0.5
    qb = q.reshape(B, S // Q_BLOCK, Q_BLOCK, H, dq).swapaxes(0, 1)

    def one_block(qblk):
        s = jnp.einsum('bqhd,bkhd->bhqk', qblk, k, preferred_element_type=jnp.float32) * scale
        p = jax.nn.softmax(s, axis=-1).astype(v.dtype)
        return jnp.einsum('bhqk,bkhd->bqhd', p, v, preferred_element_type=jnp.float32).astype(q.dtype)

    o = lax.map(one_block, qb)
    return o.swapaxes(0, 1).reshape(B, S, H, v.shape[-1])


def banded_attention(q, k, v, radius, sink=None, return_lse=False):
    N, L, Hq, hd = q.shape
    Hkv = k.shape[2]
    G = Hq // Hkv
    blk = radius
    nb = -(-L // blk)
    Lp = nb * blk
    pad = Lp - L
    qb = jnp.pad(q, ((0, 0), (0, pad), (0, 0), (0, 0))).reshape(N, nb, blk, Hkv, G, hd)

    def windows(t):
        tp = jnp.pad(t, ((0, 0), (blk, pad + blk), (0, 0), (0, 0))).reshape(N, nb + 2, blk, Hkv, hd)
        return jnp.concatenate([tp[:, :-2], tp[:, 1:-1], tp[:, 2:]], axis=2)

    kw, vw = windows(k), windows(v)
    s = jnp.einsum('nbqhgd,nbjhd->nbhgqj', qb, kw, preferred_element_type=jnp.float32) * (hd ** -0.5)
    qpos = jnp.arange(nb)[:, None] * blk + jnp.arange(blk)[None, :]
    kpos = (jnp.arange(nb)[:, None] - 1) * blk + jnp.arange(3 * blk)[None, :]
    valid = ((jnp.abs(qpos[:, :, None] - kpos[:, None, :]) <= radius)
             & (kpos[:, None, :] >= 0) & (kpos[:, None, :] < L))
    s = jnp.where(valid[None, :, None, None], s, -jnp.inf)
    m = jnp.max(s, axis=-1, keepdims=True)
    if sink is not None:
        sk = sink.astype(jnp.float32).reshape(Hkv, G)[None, None, :, :, None, None]
        m = jnp.maximum(m, sk)
        p = jnp.exp(s - m)
        den = jnp.sum(p, axis=-1, keepdims=True) + jnp.exp(sk - m)
    else:
        p = jnp.exp(s - m)
        den = jnp.sum(p, axis=-1, keepdims=True)
    o = jnp.einsum('nbhgqj,nbjhd->nbqhgd', (p / den).astype(v.dtype), vw,
                   preferred_element_type=jnp.float32)
    o = o.reshape(N, Lp, Hq, hd)[:, :L].astype(q.dtype)
    if return_lse:
        lse = (m + jnp.log(den))[..., 0]
        lse = lse.transpose(0, 1, 4, 2, 3).reshape(N, Lp, Hq)[:, :L]
        return o, lse
    return o


def to_strided(t, dil):
    B, S = t.shape[:2]
    t = t.reshape((B, S // dil, dil) + t.shape[2:])
    t = jnp.moveaxis(t, 2, 1)
    return t.reshape((B * dil, S // dil) + t.shape[3:])


def from_strided(t, dil, B):
    L = t.shape[1]
    t = t.reshape((B, dil, L) + t.shape[2:])
    t = jnp.moveaxis(t, 1, 2)
    return t.reshape((B, L * dil) + t.shape[3:])


def mixer_mla_swa(h, cos, sin, w_in, g_q, w_qb, g_kv, w_kvb, sink, w_out):
    B, S, _ = h.shape
    proj = h @ w_in
    cq, ckv, kr, qs, ks, vs = jnp.split(proj, [int(i) for i in np.cumsum(AB_SPLITS)[:-1]], axis=-1)
    qa = (rmsnorm(cq, g_q) @ w_qb).reshape(B, S, MLA_HEADS, MLA_NOPE + MLA_ROPE)
    q_rope = apply_rope(qa[..., MLA_NOPE:], cos, sin)
    kv = (rmsnorm(ckv, g_kv) @ w_kvb).reshape(B, S, MLA_HEADS, MLA_NOPE + MLA_V)
    k_nope, v_a = kv[..., :MLA_NOPE], kv[..., MLA_NOPE:]
    k_rope = apply_rope(kr.reshape(B, S, 1, MLA_ROPE), cos, sin)
    q_a = jnp.concatenate([qa[..., :MLA_NOPE], q_rope], axis=-1)
    k_a = jnp.concatenate([k_nope, jnp.broadcast_to(k_rope, (B, S, MLA_HEADS, MLA_ROPE))], axis=-1)
    o_a = dense_attention(q_a, k_a, v_a)
    q_b = apply_rope(qs.reshape(B, S, SWA_Q_HEADS, SWA_HEAD_DIM), cos, sin)
    k_b = apply_rope(ks.reshape(B, S, SWA_KV_HEADS, SWA_HEAD_DIM), cos, sin)
    v_b = vs.reshape(B, S, SWA_KV_HEADS, SWA_HEAD_DIM)
    o_b = banded_attention(q_b, k_b, v_b, SWA_RADIUS, sink=sink)
    o = jnp.concatenate([o_a.reshape(B, S, -1), o_b.reshape(B, S, -1)], axis=-1)
    return o @ w_out


def mixer_dilated(h, cos, sin, w_in, w_out):
    B, S, _ = h.shape
    proj = (h @ w_in).reshape(B, S, DIL_GROUPS, 3, DIL_HEADS, DIL_HEAD_DIM)
    outs, lses = [], []
    for g, (window, dil) in enumerate(DIL_PATTERN):
        q = apply_rope(proj[:, :, g, 0], cos, sin)
        k = apply_rope(proj[:, :, g, 1], cos, sin)
        v = proj[:, :, g, 2]
        radius = window // (2 * dil)
        o, lse = banded_attention(to_strided(q, dil), to_strided(k, dil), to_strided(v, dil),
                                  radius, return_lse=True)
        outs.append(from_strided(o, dil, B))
        lses.append(from_strided(lse, dil, B))
    wts = jax.nn.softmax(jnp.stack(lses, axis=0), axis=0)
    o = jnp.sum(wts[..., None] * jnp.stack(outs, axis=0).astype(jnp.float32), axis=0).astype(h.dtype)
    return o.reshape(B, S, C_MIX) @ w_out


def moe_ffn(h, w_router, b_router, w_gu, b_gu, w_down, b_down):
    Bs, S, D = h.shape
    T = Bs * S
    ht = h.reshape(T, D)
    logits = jnp.dot(ht, w_router, preferred_element_type=jnp.float32) + b_router.astype(jnp.float32)
    top_val, top_idx = lax.top_k(logits, TOP_K)
    gate = jax.nn.softmax(top_val, axis=-1)
    e_flat = top_idx.reshape(-1).astype(jnp.int32)
    tok_flat = jnp.arange(T * TOP_K, dtype=jnp.int32) // TOP_K
    order = jnp.argsort(e_flat)
    e_sorted = e_flat[order]
    tok_sorted = tok_flat[order]
    g_sorted = gate.reshape(-1)[order]
    counts = jnp.bincount(e_flat, length=N_EXPERTS).astype(jnp.int32)
    padded = (counts + MOE_BLOCK - 1) // MOE_BLOCK * MOE_BLOCK
    start = jnp.cumsum(counts) - counts
    pend = jnp.cumsum(padded)
    pstart = pend - padded
    rank = jnp.arange(T * TOP_K, dtype=jnp.int32) - start[e_sorted]
    dest = pstart[e_sorted] + rank
    n_rows = T * TOP_K + N_EXPERTS * MOE_BLOCK
    n_blocks = n_rows // MOE_BLOCK
    row_tok = jnp.full((n_rows,), T, dtype=jnp.int32).at[dest].set(tok_sorted)
    ht_pad = jnp.concatenate([ht, jnp.zeros((1, D), ht.dtype)], axis=0)
    xb = ht_pad[row_tok].reshape(n_blocks, MOE_BLOCK, D)
    blk_start = jnp.arange(n_blocks, dtype=jnp.int32) * MOE_BLOCK
    blk_expert = jnp.minimum(jnp.searchsorted(pend, blk_start, side='right'), N_EXPERTS - 1)

    def expert_block(args):
        xblk, e = args
        gu = jnp.dot(xblk, w_gu[e], preferred_element_type=jnp.float32) + b_gu[e].astype(jnp.float32)
        glu = jnp.minimum(gu[:, :D_FF], SWIGLU_LIMIT)
        lin = jnp.clip(gu[:, D_FF:], -SWIGLU_LIMIT, SWIGLU_LIMIT)
        act = (glu * jax.nn.sigmoid(SWIGLU_ALPHA * glu) * (lin + 1.0)).astype(xblk.dtype)
        out = jnp.dot(act, w_down[e], preferred_element_type=jnp.float32) + b_down[e].astype(jnp.float32)
        return out.astype(xblk.dtype)

    yb = lax.map(expert_block, (xb, blk_expert)).reshape(n_rows, D)
    contrib = yb[dest] * g_sorted[:, None].astype(h.dtype)
    y = jnp.zeros((T, D), h.dtype).at[tok_sorted].add(contrib)
    return y.reshape(Bs, S, D)


def setup_inputs(seed: int = 0) -> dict:
    key = jax.random.key(seed)
    ks = jax.random.split(key, 24)
    f32 = jnp.float32

    def nrm(k, shape, fan_in, gain=1.0):
        return jax.random.normal(k, shape, f32) * (gain * fan_in ** -0.5)

    def gain_init(k, shape):
        return 1.0 + 0.05 * jax.random.normal(k, shape, f32)

    def small(k, shape, s):
        return s * jax.random.normal(k, shape, f32)

    D = D_MODEL
    x = jax.random.normal(ks[0], (BATCH, SEQ, D), f32)
    c = jax.random.normal(ks[1], (BATCH, D), f32)
    offs = jax.random.randint(ks[2], (BATCH, 1), 0, SEQ, dtype=jnp.int32)
    positions = jnp.arange(SEQ, dtype=jnp.int32)[None, :] + offs
    return {
        "x": x,
        "c": c,
        "positions": positions,
        "w_mod": nrm(ks[3], (DEPTH, D, 6 * D), D, 0.5),
        "b_mod": small(ks[4], (DEPTH, 6 * D), 0.02),
        "g_norm_mix": gain_init(ks[5], (DEPTH, D)),
        "g_norm_ffn": gain_init(ks[6], (DEPTH, D)),
        "w_in_ab": nrm(ks[7], (N_EVEN, D, AB_IN), D),
        "mla_g_q": gain_init(ks[8], (N_EVEN, MLA_Q_LORA)),
        "mla_w_qb": nrm(ks[9], (N_EVEN, MLA_Q_LORA, MLA_HEADS * (MLA_NOPE + MLA_ROPE)), MLA_Q_LORA),
        "mla_g_kv": gain_init(ks[10], (N_EVEN, MLA_KV_LORA)),
        "mla_w_kvb": nrm(ks[11], (N_EVEN, MLA_KV_LORA, MLA_HEADS * (MLA_NOPE + MLA_V)), MLA_KV_LORA),
        "swa_sink": small(ks[12], (N_EVEN, SWA_Q_HEADS), 0.5),
        "w_out_ab": nrm(ks[13], (N_EVEN, AB_MIX, D), AB_MIX),
        "w_in_c": nrm(ks[14], (N_ODD, D, C_IN), D),
        "w_out_c": nrm(ks[15], (N_ODD, C_MIX, D), C_MIX),
        "w_router": nrm(ks[16], (DEPTH, D, N_EXPERTS), D),
        "b_router": small(ks[17], (DEPTH, N_EXPERTS), 0.01),
        "w_gu": nrm(ks[18], (DEPTH, N_EXPERTS, D, 2 * D_FF), D),
        "b_gu": small(ks[19], (DEPTH, N_EXPERTS, 2 * D_FF), 0.02),
        "w_down": nrm(ks[20], (DEPTH, N_EXPERTS, D_FF, D), D_FF),
        "b_down": small(ks[21], (DEPTH, N_EXPERTS, D), 0.02),
        "g_final": gain_init(ks[22], (D,)),
    }


def reference(x, c, positions, w_mod, b_mod, g_norm_mix, g_norm_ffn,
              w_in_ab, mla_g_q, mla_w_qb, mla_g_kv, mla_w_kvb, swa_sink, w_out_ab,
              w_in_c, w_out_c, w_router, b_router, w_gu, b_gu, w_down, b_down, g_final):
    cos, sin = rope_tables(positions)
    c_act = jax.nn.silu(c)
    for layer in range(DEPTH):
        mod = c_act @ w_mod[layer] + b_mod[layer]
        sh1, sc1, g1, sh2, sc2, g2 = jnp.split(mod, 6, axis=-1)
        h = ada_norm(x, g_norm_mix[layer], sh1, sc1)
        li = layer // 2
        if layer % 2 == 0:
            mix = mixer_mla_swa(h, cos, sin, w_in_ab[li], mla_g_q[li], mla_w_qb[li],
                                mla_g_kv[li], mla_w_kvb[li], swa_sink[li], w_out_ab[li])
        else:
            mix = mixer_dilated(h, cos, sin, w_in_c[li], w_out_c[li])
        x = x + g1[:, None, :] * mix
        h = ada_norm(x, g_norm_ffn[layer], sh2, sc2)
        ffn = moe_ffn(h, w_router[layer], b_router[layer], w_gu[layer], b_gu[layer],
                      w_down[layer], b_down[layer])
        x = x + g2[:, None, :] * ffn
    return rmsnorm(x, g_final)
```

```python
import contextlib
import numpy as np
import ml_dtypes
import concourse.bass as bass
import concourse.mybir as mybir
from concourse.bass_utils import run_bass_kernel_spmd

F32 = mybir.dt.float32
BF16 = mybir.dt.bfloat16
I32 = mybir.dt.int32
AF = mybir.ActivationFunctionType
ALU = mybir.AluOpType
AX = mybir.AxisListType

S = 4096
D = 1024
DEPTH = 4
NE = 32
DFF = 1024
EPS = 1e-6
NQ = 8
STRICT = True

CFG = {"layers": [0, 1, 2, 3], "mixer": True, "ffn": True, "ncores": 8}


class Res:
    __slots__ = ("w", "r")

    def __init__(self):
        self.w = None
        self.r = {}


class Builder:
    def __init__(self):
        nc = self.nc = bass.Bass("TRN2", target_bir_lowering=False)
        self.es = contextlib.ExitStack()
        self.streams = {"pe": nc.tensor, "act": nc.scalar, "dve": nc.vector, "pool": nc.gpsimd, "sp": nc.sync}
        self.csem = {}
        self.ccount = {}
        for e in ("pe", "act", "dve", "pool"):
            self.csem[e] = self.es.enter_context(nc.semaphore("c_" + e))
            self.ccount[e] = 0
        self.qsems = {}
        self.qcount = {}
        self.qissuer = {"sp": "sp", "pq": "pool"}
        for q in ("sp", "pq"):
            self.qsems[q] = [self.es.enter_context(nc.semaphore("q_%s%d" % (q, j))) for j in range(NQ)]
            self.qcount[q] = 0
        self.waited = {s: {} for s in self.streams}
        self.uid = 0

    def name(self, p):
        self.uid += 1
        return "%s_%d" % (p, self.uid)

    def sb(self, ctx, shape, dt, nm="t"):
        return ctx.enter_context(self.nc.sbuf_tensor(self.name(nm), list(shape), dt))

    def _wait(self, stream, tok):
        sem, val, owner = tok
        if owner == stream and (stream == "pe" or not STRICT):
            return
        w = self.waited[stream]
        if w.get(id(sem), 0) >= val:
            return
        self.streams[stream].wait_ge(sem, val)
        w[id(sem)] = val

    def _deps(self, stream, reads, writes):
        for r in reads:
            if r.w is not None:
                self._wait(stream, r.w)
        for r in writes:
            if r.w is not None:
                self._wait(stream, r.w)
            for t in r.r.values():
                self._wait(stream, t)

    def _mark(self, tok, reads, writes):
        for r in reads:
            r.r[id(tok[0])] = tok
        for r in writes:
            r.w = tok
            r.r = {}

    def op(self, eng, fn, reads=(), writes=()):
        self._deps(eng, reads, writes)
        ins = fn(self.streams[eng])
        self.ccount[eng] += 1
        tok = (self.csem[eng], self.ccount[eng], eng)
        ins.then_inc(tok[0], 1)
        self._mark(tok, reads, writes)

    def dma(self, q, out, in_, reads=(), writes=(), **kw):
        issuer = self.qissuer[q]
        n = self.qcount[q]
        sem = self.qsems[q][n % NQ]
        if n >= NQ:
            self._wait(issuer, (sem, 16 * (n // NQ), None))
        self._deps(issuer, reads, writes)
        eng = self.nc.sync if q == "sp" else self.nc.gpsimd
        ins = eng.dma_start(out=out, in_=in_, **kw)
        ins.then_inc(sem, 16)
        self.qcount[q] += 1
        tok = (sem, 16 * (n // NQ + 1), None)
        self._mark(tok, reads, writes)

    def barrier(self):
        toks = []
        for e in self.csem:
            if self.ccount[e] > 0:
                toks.append((self.csem[e], self.ccount[e], e))
        for q in self.qsems:
            n = self.qcount[q]
            for j in range(NQ):
                cnt = (n - j + NQ - 1) // NQ if n > j else 0
                if cnt > 0:
                    toks.append((self.qsems[q][j], 16 * cnt, None))
        for s in self.streams:
            for t in toks:
                self._wait(s, t)


def build(cfg):
    b = Builder()
    nc = b.nc
    es = b.es
    layers = cfg["layers"]

    def din(name, shape, dt=F32):
        return nc.dram_tensor(name, list(shape), dt, kind="ExternalInput").ap()

    def dscr(name, shape, dt=F32):
        return nc.dram_tensor(name, list(shape), dt, kind="Internal").ap()

    x_in = din("x", [S, D])
    c_in = din("c", [D])
    pos_in = din("positions", [S], I32)
    w_mod = din("w_mod", [DEPTH, D, 6 * D])
    b_mod = din("b_mod", [DEPTH, 6 * D])
    g_mix = din("g_norm_mix", [DEPTH, D])
    g_ffn = din("g_norm_ffn", [DEPTH, D])
    w_in_ab = din("w_in_ab", [2, D, 1984])
    mla_g_q = din("mla_g_q", [2, 384])
    mla_w_qb = din("mla_w_qb", [2, 384, 1536])
    mla_g_kv = din("mla_g_kv", [2, 256])
    mla_w_kvb = din("mla_w_kvb", [2, 256, 2048])
    swa_sink = din("swa_sink", [2, 16])
    w_out_ab = din("w_out_ab", [2, 2048, D])
    w_in_c = din("w_in_c", [2, D, 9216])
    w_out_c = din("w_out_c", [2, 1024, D])
    w_router = din("w_router", [DEPTH, D, NE])
    b_router = din("b_router", [DEPTH, NE])
    w_gu = din("w_gu", [DEPTH, NE, D, 2 * DFF])
    b_gu = din("b_gu", [DEPTH, NE, 2 * DFF])
    w_down = din("w_down", [DEPTH, NE, DFF, D])
    b_down = din("b_down", [DEPTH, NE, D])
    g_final = din("g_final", [D])
    k_identf = din("k_identf", [128, 128])
    k_inv = din("k_inv", [128, 1])
    k_masks = din("k_masks", [128, 4, 128])
    y_out = nc.dram_tensor("y", [S, D], F32, kind="ExternalOutput").ap()

    xT_d = dscr("xT_d", [8, 128, S])
    oT_d = dscr("oT_d", [16, 128, S], BF16)
    tab_d = dscr("tab_d", [2, 128, S])
    gt_d = dscr("gt_d", [NE, 2048])
    xT_v = xT_d.rearrange("c p t -> p c t")
    oT_v = oT_d.rearrange("c p t -> p c t")
    R_x = Res()
    R_o = Res()
    R_tab = Res()

    identf = b.sb(es, [128, 128], F32, "identf")
    identb = b.sb(es, [128, 128], BF16, "identb")
    meanm = b.sb(es, [128, 128], F32, "meanm")
    onesb = b.sb(es, [128, 128], BF16, "onesb")
    modc = b.sb(es, [128, DEPTH, 6, 8], F32, "modc")
    gcol = b.sb(es, [128, DEPTH, 2, 8], F32, "gcol")
    gfin = b.sb(es, [128, 8], F32, "gfin")
    epsc = b.sb(es, [128, 1], F32, "epsc")
    R_const = Res()

    banks = []
    for i in range(6):
        t = es.enter_context(nc.psum_tensor(b.name("psf"), [128, 512], F32))
        banks.append((t, Res()))
    bbanks = []
    for i in range(2):
        t = es.enter_context(nc.psum_tensor(b.name("psb"), [128, 1024], BF16))
        bbanks.append((t, Res()))
    rr = {"f": 0, "b": 0}

    def bank():
        rr["f"] = (rr["f"] + 1) % 6
        return banks[rr["f"]]

    def bbank():
        rr["b"] = (rr["b"] + 1) % 2
        return bbanks[rr["b"]]

    def col_view(vec_ap, n):
        return vec_ap.rearrange("(j p) -> p j", p=128)

    b.dma("sp", identf[:], k_identf[:, :], writes=[R_const])
    b.op("dve", lambda e: e.tensor_copy(out=identb[:], in_=identf[:]), reads=[R_const], writes=[R_const])
    b.op("pool", lambda e: e.memset(meanm[:], 1.0 / D), writes=[R_const])
    b.op("pool", lambda e: e.memset(onesb[:], 1.0), writes=[R_const])
    b.op("pool", lambda e: e.memset(epsc[:], EPS), writes=[R_const])
    b.dma("sp", gfin[:], col_view(g_final, 8), writes=[R_const], allow_slow_non_contiguous=True)
    for l in layers:
        b.dma("sp", gcol[:, l, 0, :], col_view(g_mix[l], 8), writes=[R_const], allow_slow_non_contiguous=True)
        b.dma("sp", gcol[:, l, 1, :], col_view(g_ffn[l], 8), writes=[R_const], allow_slow_non_contiguous=True)

    with contextlib.ExitStack() as ps:
        cT = b.sb(ps, [128, 8], F32, "cT")
        cS = b.sb(ps, [128, 8], F32, "cS")
        bmc = b.sb(ps, [128, DEPTH, 48], F32, "bmc")
        wm = [b.sb(ps, [128, 8, 1024], F32, "wm%d" % i) for i in range(2)]
        R_wm = [Res(), Res()]
        R_c = Res()
        b.dma("sp", cT[:], col_view(c_in, 8), writes=[R_c], allow_slow_non_contiguous=True)
        for l in layers:
            b.dma("sp", bmc[:, l, :], col_view(b_mod[l], 48), writes=[R_c], allow_slow_non_contiguous=True)
        b.op("act", lambda e: e.activation(out=cS[:], in_=cT[:], func=AF.Sigmoid), reads=[R_c], writes=[R_c])
        b.op("dve", lambda e: e.tensor_tensor(out=cS[:], in0=cS[:], in1=cT[:], op=ALU.mult), reads=[R_c], writes=[R_c])
        k = 0
        for l in layers:
            for v in range(6):
                slot = k % 2
                k += 1
                b.dma("sp", wm[slot][:], w_mod[l].rearrange("(dc p) f -> p dc f", p=128)[:, :, v * 1024:(v + 1) * 1024],
                      writes=[R_wm[slot]])
                pt, pr = bank()
                for j in range(8):
                    for dc in range(8):
                        b.op("pe", lambda e, j=j, dc=dc: e.matmul(pt[:, j:j + 1], lhsT=wm[slot][:, dc, j * 128:(j + 1) * 128],
                                                                   rhs=cS[:, dc:dc + 1], start=(dc == 0), stop=(dc == 7)),
                             reads=[R_wm[slot], R_c], writes=[pr])
                b.op("dve", lambda e: e.tensor_tensor(out=modc[:, l, v, :], in0=pt[:, 0:8], in1=bmc[:, l, v * 8:(v + 1) * 8], op=ALU.add),
                     reads=[pr, R_c], writes=[R_const])
        for l in layers:
            for (v, gi) in ((1, 0), (4, 1)):
                b.op("dve", lambda e, l=l, v=v, gi=gi: e.scalar_tensor_tensor(out=modc[:, l, v, :], in0=modc[:, l, v, :], scalar=1.0,
                                                                              in1=gcol[:, l, gi, :], op0=ALU.add, op1=ALU.mult),
                     reads=[R_const], writes=[R_const])
        b.barrier()

    with contextlib.ExitStack() as ps:
        xt = [b.sb(ps, [128, D], F32, "xt%d" % i) for i in range(2)]
        xo = [b.sb(ps, [128, 8, 128], F32, "xo%d" % i) for i in range(2)]
        R_xt = [Res(), Res()]
        R_xo = [Res(), Res()]
        for i in range(32):
            s = i % 2
            b.dma("sp", xt[s][:], x_in[i * 128:(i + 1) * 128, :], writes=[R_xt[s]])
            for hb in range(2):
                pt, pr = bank()
                for cc in range(4):
                    c = hb * 4 + cc
                    b.op("pe", lambda e, c=c, cc=cc: e.transpose(out=pt[:, cc * 128:(cc + 1) * 128], in_=xt[s][:, c * 128:(c + 1) * 128],
                                                                 identity=identf[:]),
                         reads=[R_xt[s]], writes=[pr])
                eng = "dve" if hb == 0 else "act"
                if eng == "dve":
                    b.op("dve", lambda e: e.tensor_copy(out=xo[s][:, hb * 4:(hb + 1) * 4, :],
                                                        in_=pt[:, :].rearrange("p (c t) -> p c t", c=4)),
                         reads=[pr], writes=[R_xo[s]])
                else:
                    b.op("act", lambda e: e.copy(out=xo[s][:, hb * 4:(hb + 1) * 4, :],
                                                 in_=pt[:, :].rearrange("p (c t) -> p c t", c=4)),
                         reads=[pr], writes=[R_xo[s]])
            b.dma("sp", xT_v[:, :, i * 128:(i + 1) * 128], xo[s][:], reads=[R_xo[s]], writes=[R_x])
        b.barrier()

    def norm_chunk(ctx_bufs, l, which, t0, hT, hcol0, R_h):
        xc, R_xc, sq, R_sq, rs, R_rs, tmp, R_tmp = ctx_bufs[(t0 // 512) % 2]
        va = 1 if which == 0 else 4
        vb = 0 if which == 0 else 3
        b.dma("sp", xc[:], xT_v[:, :, t0:t0 + 512], reads=[R_x], writes=[R_xc])
        b.op("act", lambda e: e.activation(out=sq[:], in_=xc[:], func=AF.Square), reads=[R_xc], writes=[R_sq])
        pt, pr = bank()
        for dc in range(8):
            b.op("pe", lambda e, dc=dc: e.matmul(pt[:, :], lhsT=meanm[:], rhs=sq[:, dc, :], start=(dc == 0), stop=(dc == 7)),
                 reads=[R_sq], writes=[pr])
        b.op("act", lambda e: e.activation(out=rs[:], in_=pt[:, :], func=AF.Sqrt, bias=epsc[:], scale=1.0),
             reads=[pr], writes=[R_rs])
        b.op("dve", lambda e: e.reciprocal(out=rs[:], in_=rs[:]), reads=[R_rs], writes=[R_rs])
        for dc in range(8):
            b.op("dve", lambda e, dc=dc: e.scalar_tensor_tensor(out=tmp[:, dc, :], in0=xc[:, dc, :], scalar=modc[:, l, va, dc:dc + 1],
                                                                in1=rs[:], op0=ALU.mult, op1=ALU.mult),
                 reads=[R_xc, R_rs], writes=[R_tmp])
            b.op("act", lambda e, dc=dc: e.activation(out=hT[:, dc, hcol0:hcol0 + 512], in_=tmp[:, dc, :], func=AF.Identity,
                                                      bias=modc[:, l, vb, dc:dc + 1], scale=1.0),
                 reads=[R_tmp], writes=[R_h])

    def norm_bufs(ps):
        sets = []
        for i in range(2):
            xc = b.sb(ps, [128, 8, 512], F32, "xc%d" % i)
            sq = b.sb(ps, [128, 8, 512], F32, "sq%d" % i)
            rs = b.sb(ps, [128, 512], F32, "rs%d" % i)
            R_sq = Res()
            sets.append((xc, Res(), sq, R_sq, rs, Res(), sq, R_sq))
        return sets

    def moe_layer(l):
        for hf in range(2):
            T0 = hf * 2048
            with contextlib.ExitStack() as hs:
                hT = b.sb(hs, [128, 8, 2048], BF16, "hT")
                acc = b.sb(hs, [128, 8, 2048], F32, "acc")
                R_h = Res()
                R_G = Res()
                R_acc = [[Res() for _ in range(4)] for _ in range(8)]
                bgc = b.sb(hs, [128, 16, NE], F32, "bgc")
                R_b = Res()
                R_gt = Res()
                with contextlib.ExitStack() as ps:
                    nb = norm_bufs(ps)
                    wrf = b.sb(ps, [128, 8, NE], F32, "wrf")
                    wrb = b.sb(ps, [128, 8, NE], BF16, "wrb")
                    brb = b.sb(ps, [128, NE], F32, "brb")
                    R_wr = Res()
                    lg = b.sb(ps, [128, NE], F32, "lg")
                    t8 = b.sb(ps, [128, 8], F32, "t8")
                    ng = b.sb(ps, [128, 1], F32, "ng")
                    ee = b.sb(ps, [128, NE], F32, "ee")
                    mk = b.sb(ps, [128, NE], F32, "mk")
                    sm = b.sb(ps, [128, 1], F32, "sm")
                    R_r = Res()
                    bgr = b.sb(ps, [32, 2048], F32, "bgr")
                    GT = b.sb(ps, [32, 2048], F32, "GT")
                    bdn = b.sb(ps, [32, D], F32, "bdn")
                    b.dma("sp", bgr[:], b_gu[l], writes=[R_b])
                    b.dma("sp", bdn[:], b_down[l], writes=[R_b])
                    pt, pr = bank()
                    for c in range(16):
                        b.op("pe", lambda e, c=c: e.transpose(out=pt[:, c * NE:(c + 1) * NE], in_=bgr[:, c * 128:(c + 1) * 128],
                                                              identity=identf[0:32, 0:32]),
                             reads=[R_b], writes=[pr])
                    b.op("dve", lambda e: e.tensor_copy(out=bgc[:], in_=pt[:, :].rearrange("p (c e) -> p c e", c=16)),
                         reads=[pr], writes=[R_b])
                    b.op("dve", lambda e: e.tensor_scalar(out=bgc[:, 8:16, :], in0=bgc[:, 8:16, :], scalar1=1.0, scalar2=None, op0=ALU.add),
                         reads=[R_b], writes=[R_b])
                    b.dma("sp", wrf[:], w_router[l].rearrange("(dc p) e -> p dc e", p=128), writes=[R_wr])
                    b.dma("sp", brb[:], b_router[l].partition_broadcast(128), writes=[R_wr])
                    b.op("dve", lambda e: e.tensor_copy(out=wrb[:], in_=wrf[:]), reads=[R_wr], writes=[R_wr])
                    for tc in range(4):
                        norm_chunk(nb, l, 1, T0 + tc * 512, hT, tc * 512, R_h)
                    for i in range(16):
                        pt, pr = bank()
                        for dc in range(8):
                            b.op("pe", lambda e, dc=dc: e.matmul(pt[:, 0:NE], lhsT=hT[:, dc, i * 128:(i + 1) * 128], rhs=wrb[:, dc, :],
                                                                 start=(dc == 0), stop=(dc == 7)),
                                 reads=[R_h, R_wr], writes=[pr])
                        b.op("dve", lambda e: e.tensor_tensor(out=lg[:], in0=pt[:, 0:NE], in1=brb[:], op=ALU.add),
                             reads=[pr, R_wr], writes=[R_r])
                        b.op("dve", lambda e: e.max(out=t8[:], in_=lg[:]), reads=[R_r], writes=[R_r])
                        b.op("dve", lambda e: e.tensor_scalar(out=ng[:], in0=t8[:, 0:1], scalar1=-1.0, scalar2=None, op0=ALU.mult),
                             reads=[R_r], writes=[R_r])
                        b.op("act", lambda e: e.activation(out=ee[:], in_=lg[:], func=AF.Exp, bias=ng[:], scale=1.0),
                             reads=[R_r], writes=[R_r])
                        b.op("dve", lambda e: e.tensor_scalar(out=mk[:], in0=lg[:], scalar1=t8[:, 3:4], scalar2=None, op0=ALU.is_ge),
                             reads=[R_r], writes=[R_r])
                        b.op("dve", lambda e: e.tensor_tensor(out=ee[:], in0=ee[:], in1=mk[:], op=ALU.mult), reads=[R_r], writes=[R_r])
                        b.op("dve", lambda e: e.reduce_sum(out=sm[:], in_=ee[:], axis=AX.X), reads=[R_r], writes=[R_r])
                        b.op("dve", lambda e: e.reciprocal(out=sm[:], in_=sm[:]), reads=[R_r], writes=[R_r])
                        b.op("dve", lambda e: e.tensor_scalar(out=ee[:], in0=ee[:], scalar1=sm[:, 0:1], scalar2=None, op0=ALU.mult),
                             reads=[R_r], writes=[R_r])
                        pt2, pr2 = bank()
                        b.op("pe", lambda e: e.transpose(out=pt2[0:NE, 0:128], in_=ee[:], identity=identf[:]), reads=[R_r], writes=[pr2])
                        b.op("act", lambda e: e.copy(out=GT[:, i * 128:(i + 1) * 128], in_=pt2[0:NE, 0:128]), reads=[pr2], writes=[R_G])
                    b.dma("sp", gt_d[:, :], GT[:], reads=[R_G], writes=[R_gt])
                    for dc in range(8):
                        for tc in range(4):
                            pt, pr = bank()
                            b.op("pe", lambda e, dc=dc, tc=tc: e.matmul(pt[:, :], lhsT=bdn[:, dc * 128:(dc + 1) * 128],
                                                                        rhs=GT[:, tc * 512:(tc + 1) * 512], start=True, stop=True),
                                 reads=[R_b, R_G], writes=[pr])
                            b.op("act", lambda e, dc=dc, tc=tc: e.copy(out=acc[:, dc, tc * 512:(tc + 1) * 512], in_=pt[:, :]),
                                 reads=[pr], writes=[R_acc[dc][tc]])
                    b.barrier()
                with contextlib.ExitStack() as ps:
                    actT = b.sb(ps, [128, 8, 2048], BF16, "actT")
                    R_act = [[Res() for _ in range(4)] for _ in range(8)]
                    NST = 4
                    stg = [b.sb(ps, [128, 8, 256], F32, "stg%d" % i) for i in range(NST)]
                    wbf = [b.sb(ps, [128, 8, 256], BF16, "wbf%d" % i) for i in range(NST)]
                    R_stg = [Res() for _ in range(NST)]
                    R_wbf = [Res() for _ in range(NST)]
                    tt = [b.sb(ps, [128, 512], F32, "tt%d" % i) for i in range(2)]
                    sg = [b.sb(ps, [128, 512], F32, "sg%d" % i) for i in range(2)]
                    uu = [b.sb(ps, [128, 512], F32, "uu%d" % i) for i in range(2)]
                    R_tt = [Res(), Res()]
                    R_sg = [Res(), Res()]
                    R_uu = [Res(), Res()]
                    gbc = b.sb(ps, [128, 2048], F32, "gbc")
                    R_gbc = [Res() for _ in range(4)]
                    steps = []
                    for ex in range(NE):
                        wg = w_gu[l, ex].rearrange("(dc p) f -> p dc f", p=128)
                        wd = w_down[l, ex].rearrange("(fc p) d -> p fc d", p=128)
                        for q2 in range(4):
                            steps.append((ex, "gu", q2, [wg[:, :, q2 * 256:(q2 + 1) * 256], wg[:, :, DFF + q2 * 256:DFF + (q2 + 1) * 256]]))
                        for r2 in range(2):
                            steps.append((ex, "dn", r2, [wd[:, :, (2 * r2) * 256:(2 * r2 + 1) * 256], wd[:, :, (2 * r2 + 1) * 256:(2 * r2 + 2) * 256]]))
                    NS = len(steps)

                    def slots_of(i):
                        return (0, 1) if i % 2 == 0 else (2, 3)

                    def load_dma(i):
                        if i >= NS:
                            return
                        for s_, src in zip(slots_of(i), steps[i][3]):
                            b.dma("sp", stg[s_][:], src, writes=[R_stg[s_]])

                    def load_cast(i):
                        if i >= NS:
                            return
                        for s_ in slots_of(i):
                            b.op("act", lambda e: e.copy(out=wbf[s_][:], in_=stg[s_][:]), reads=[R_stg[s_]], writes=[R_wbf[s_]])

                    def load_gate(ex):
                        for tc in range(4):
                            b.dma("sp", gbc[:, tc * 512:(tc + 1) * 512], gt_d[ex, tc * 512:(tc + 1) * 512].partition_broadcast(128),
                                  reads=[R_gt], writes=[R_gbc[tc]])

                    cnt = [0]

                    def gu_unit(ex, q, tc, sA, sB, sub):
                        pa, pra = bank()
                        pb, prb = bank()
                        for dc in range(8):
                            b.op("pe", lambda e, dc=dc: e.matmul(pa[:, :], lhsT=wbf[sA][:, dc, sub * 128:(sub + 1) * 128],
                                                                 rhs=hT[:, dc, tc * 512:(tc + 1) * 512], start=(dc == 0), stop=(dc == 7)),
                                 reads=[R_wbf[sA], R_h], writes=[pra])
                        for dc in range(8):
                            b.op("pe", lambda e, dc=dc: e.matmul(pb[:, :], lhsT=wbf[sB][:, dc, sub * 128:(sub + 1) * 128],
                                                                 rhs=hT[:, dc, tc * 512:(tc + 1) * 512], start=(dc == 0), stop=(dc == 7)),
                                 reads=[R_wbf[sB], R_h], writes=[prb])
                        i2 = cnt[0] % 2
                        cnt[0] += 1
                        b.op("dve", lambda e: e.tensor_scalar(out=tt[i2][:], in0=pa[:, :], scalar1=bgc[:, q, ex:ex + 1], scalar2=7.0,
                                                              op0=ALU.add, op1=ALU.min),
                             reads=[pra, R_b], writes=[R_tt[i2]])
                        b.op("act", lambda e: e.activation(out=uu[i2][:], in_=pb[:, :], func=AF.Identity,
                                                           bias=bgc[:, 8 + q, ex:ex + 1], scale=1.0),
                             reads=[prb, R_b], writes=[R_uu[i2]])
                        b.op("act", lambda e: e.activation(out=sg[i2][:], in_=tt[i2][:], func=AF.Sigmoid, scale=1.702),
                             reads=[R_tt[i2]], writes=[R_sg[i2]])
                        b.op("pool", lambda e: e.tensor_scalar(out=uu[i2][:], in0=uu[i2][:], scalar1=8.0, scalar2=-6.0,
                                                               op0=ALU.min, op1=ALU.max),
                             reads=[R_uu[i2]], writes=[R_uu[i2]])
                        b.op("pool", lambda e: e.tensor_tensor(out=uu[i2][:], in0=uu[i2][:], in1=gbc[:, tc * 512:(tc + 1) * 512],
                                                               op=ALU.mult),
                             reads=[R_uu[i2], R_gbc[tc]], writes=[R_uu[i2]])
                        b.op("dve", lambda e: e.tensor_tensor(out=tt[i2][:], in0=tt[i2][:], in1=sg[i2][:], op=ALU.mult),
                             reads=[R_tt[i2], R_sg[i2]], writes=[R_tt[i2]])
                        b.op("dve", lambda e: e.tensor_tensor(out=actT[:, q, tc * 512:(tc + 1) * 512], in0=tt[i2][:], in1=uu[i2][:],
                                                              op=ALU.mult),
                             reads=[R_tt[i2], R_uu[i2]], writes=[R_act[q][tc]])

                    def dn_unit(s_, dc, ds, tc):
                        pt, pr = bank()
                        for fc in range(8):
                            b.op("pe", lambda e, fc=fc: e.matmul(pt[:, :], lhsT=wbf[s_][:, fc, ds * 128:(ds + 1) * 128],
                                                                 rhs=actT[:, fc, tc * 512:(tc + 1) * 512],
                                                                 start=(fc == 0), stop=(fc == 7)),
                                 reads=[R_wbf[s_], R_act[fc][tc]], writes=[pr])
                        b.op("dve", lambda e: e.tensor_tensor(out=acc[:, dc, tc * 512:(tc + 1) * 512], in0=pt[:, :],
                                                              in1=acc[:, dc, tc * 512:(tc + 1) * 512], op=ALU.add),
                             reads=[pr, R_acc[dc][tc]], writes=[R_acc[dc][tc]])

                    load_gate(0)
                    load_dma(0)
                    load_dma(1)
                    load_cast(0)
                    for i in range(NS):
                        ex, kind, idx, _ = steps[i]
                        load_dma(i + 2)
                        sl = slots_of(i)
                        if kind == "gu":
                            units = [(idx * 2 + sub, tc, sub) for sub in range(2) for tc in range(4)]
                            for ui, (q, tc, sub) in enumerate(units):
                                if ui == 4:
                                    load_cast(i + 1)
                                gu_unit(ex, q, tc, sl[0], sl[1], sub)
                        else:
                            if idx == 0 and ex + 1 < NE:
                                load_gate(ex + 1)
                            units = [(sl[hh], (idx * 2 + hh) * 2 + ds, ds, tc) for hh in range(2) for tc in range(4) for ds in range(2)]
                            for ui, (s_, dc, ds, tc) in enumerate(units):
                                if ui == 8:
                                    load_cast(i + 1)
                                dn_unit(s_, dc, ds, tc)
                    b.barrier()
                with contextlib.ExitStack() as ps:
                    xc = [b.sb(ps, [128, 8, 512], F32, "xr%d" % i) for i in range(2)]
                    R_xc = [Res(), Res()]
                    for tc in range(4):
                        s = tc % 2
                        t0 = T0 + tc * 512
                        b.dma("sp", xc[s][:], xT_v[:, :, t0:t0 + 512], reads=[R_x], writes=[R_xc[s]])
                        for dc in range(8):
                            b.op("dve", lambda e, dc=dc: e.scalar_tensor_tensor(out=xc[s][:, dc, :], in0=acc[:, dc, tc * 512:(tc + 1) * 512],
                                                                                scalar=modc[:, l, 5, dc:dc + 1], in1=xc[s][:, dc, :],
                                                                                op0=ALU.mult, op1=ALU.add),
                                 reads=[R_xc[s]], writes=[R_xc[s]])
                        b.dma("sp", xT_v[:, :, t0:t0 + 512], xc[s][:], reads=[R_xc[s]], writes=[R_x])
                    b.barrier()

    maskb = b.sb(es, [128, 4, 128], BF16, "maskb")
    invc = b.sb(es, [128, 1], F32, "invc")
    pic = b.sb(es, [128, 1], F32, "pic")
    with contextlib.ExitStack() as ps:
        mkf = b.sb(ps, [128, 4, 128], F32, "mkf")
        posi = b.sb(ps, [128, S], I32, "posi")
        posf = b.sb(ps, [128, S], F32, "posf")
        ang = b.sb(ps, [128, S], F32, "ang")
        R_t = Res()
        b.dma("sp", mkf[:], k_masks[:, :, :], writes=[R_t])
        b.dma("sp", invc[:], k_inv[:, :], writes=[R_t])
        b.dma("sp", posi[:], pos_in.partition_broadcast(128), writes=[R_t])
        b.op("pool", lambda e: e.memset(pic[:], float(np.pi / 2)), writes=[R_t])
        b.op("dve", lambda e: e.tensor_copy(out=maskb[:], in_=mkf[:]), reads=[R_t], writes=[R_t])
        b.op("dve", lambda e: e.tensor_copy(out=posf[:], in_=posi[:]), reads=[R_t], writes=[R_t])
        b.op("dve", lambda e: e.tensor_scalar(out=posf[:], in0=posf[:], scalar1=invc[:, 0:1], scalar2=None, op0=ALU.mult),
             reads=[R_t], writes=[R_t])
        C1 = 6.28125
        C2 = float(np.float32(2 * np.pi - 6.28125))
        C3 = float(2 * np.pi - 6.28125 - np.float64(np.float32(2 * np.pi - 6.28125)))
        kf = posi[:, :].bitcast(F32)
        b.op("dve", lambda e: e.tensor_scalar(out=ang[:], in0=posf[:], scalar1=float(1 / (2 * np.pi)), scalar2=None, op0=ALU.mult),
             reads=[R_t], writes=[R_t])
        b.op("dve", lambda e: e.tensor_copy(out=posi[:], in_=ang[:]), reads=[R_t], writes=[R_t])
        b.op("dve", lambda e: e.tensor_copy(out=ang[:], in_=posi[:]), reads=[R_t], writes=[R_t])
        for cc in (C1, C2, C3):
            b.op("dve", lambda e: e.scalar_tensor_tensor(out=posf[:], in0=ang[:], scalar=-cc, in1=posf[:], op0=ALU.mult, op1=ALU.add),
                 reads=[R_t], writes=[R_t])
        b.op("dve", lambda e: e.tensor_scalar(out=ang[:], in0=posf[:], scalar1=float(np.pi), scalar2=float(-2 * np.pi), op0=ALU.is_gt, op1=ALU.mult),
             reads=[R_t], writes=[R_t])
        b.op("dve", lambda e: e.tensor_tensor(out=posf[:], in0=posf[:], in1=ang[:], op=ALU.add), reads=[R_t], writes=[R_t])
        b.op("dve", lambda e: e.tensor_scalar(out=ang[:], in0=posf[:], scalar1=float(-np.pi), scalar2=float(2 * np.pi), op0=ALU.is_lt, op1=ALU.mult),
             reads=[R_t], writes=[R_t])
        b.op("dve", lambda e: e.tensor_tensor(out=posf[:], in0=posf[:], in1=ang[:], op=ALU.add), reads=[R_t], writes=[R_t])
        b.op("dve", lambda e: e.tensor_scalar(out=posf[:], in0=posf[:], scalar1=3.14159, scalar2=-3.14159, op0=ALU.min, op1=ALU.max),
             reads=[R_t], writes=[R_t])
        b.op("dve", lambda e: e.scalar_tensor_tensor(out=ang[:], in0=posf[:], scalar=-1.0, in1=posf[:], op0=ALU.mult, op1=ALU.max),
             reads=[R_t], writes=[R_t])
        b.op("act", lambda e: e.activation(out=ang[:], in_=ang[:], func=AF.Sin, bias=pic[:], scale=-1.0), reads=[R_t], writes=[R_t])
        b.op("act", lambda e: e.activation(out=posf[:], in_=posf[:], func=AF.Sin), reads=[R_t], writes=[R_t])
        b.dma("sp", tab_d[0], ang[:], reads=[R_t], writes=[R_tab])
        b.dma("sp", tab_d[1], posf[:], reads=[R_t], writes=[R_tab])
        b.barrier()

    def fbank(lst, st):
        st[0] = (st[0] + 1) % len(lst)
        return banks[lst[st[0]]]

    def rot_weights(dst, src, nblk, R_w):
        sv = src.rearrange("p (k h i) -> p k h i", h=2, i=32)
        dv = dst.rearrange("p (k h i) -> p k h i", h=2, i=32)
        b.op("dve", lambda e: e.tensor_scalar(out=dv[:, :, 0, :], in0=sv[:, :, 1, :], scalar1=-1.0, scalar2=None, op0=ALU.mult),
             reads=[R_w], writes=[R_w])
        b.op("dve", lambda e: e.tensor_copy(out=dv[:, :, 1, :], in_=sv[:, :, 0, :]), reads=[R_w], writes=[R_w])

    def rope_evac(out_ap, pa, pb_, C_ap, S_ap, t1, t2, R_tmp, reads, writes, npart=128, perm=None):
        b.op("dve", lambda e: e.tensor_tensor(out=t1[0:npart, :], in0=pa, in1=C_ap, op=ALU.mult), reads=reads, writes=[R_tmp])
        b.op("dve", lambda e: e.tensor_tensor(out=t2[0:npart, :], in0=pb_, in1=S_ap, op=ALU.mult), reads=reads, writes=[R_tmp])
        a1 = t1[0:npart, :]
        a2 = t2[0:npart, :]
        if perm is not None:
            a1 = a1.rearrange("p (m r) -> p r m", r=perm)
            a2 = a2.rearrange("p (m r) -> p r m", r=perm)
        b.op("pool", lambda e: e.tensor_tensor(out=out_ap, in0=a1, in1=a2, op=ALU.add), reads=[R_tmp], writes=writes)

    def load_tables(ps):
        Ct = b.sb(ps, [128, S], F32, "Ct")
        St = b.sb(ps, [128, S], F32, "St")
        R_T = Res()
        b.dma("sp", Ct[:], tab_d[0], reads=[R_tab], writes=[R_T])
        b.dma("sp", St[:], tab_d[1], reads=[R_tab], writes=[R_T])
        return Ct, St, R_T

    def norm_all(l, which, hT, R_h):
        with contextlib.ExitStack() as ps:
            nb = norm_bufs(ps)
            for tc in range(8):
                norm_chunk(nb, l, which, tc * 512, hT, tc * 512, R_h)
            b.barrier()

    def out_proj(l, w_out_l, nfc):
        with contextlib.ExitStack() as ps:
            wo = b.sb(ps, [128, nfc, D], BF16, "wo")
            R_wo = Res()
            ot = [b.sb(ps, [128, nfc, 512], BF16, "ot%d" % i) for i in range(2)]
            xc = [b.sb(ps, [128, 8, 512], F32, "ox%d" % i) for i in range(2)]
            R_ot = [Res(), Res()]
            R_xc = [Res(), Res()]
            wv_ = w_out_l.rearrange("(fc p) d -> p fc d", p=128)
            for i in range(nfc // 4):
                b.dma("pq", wo[:, i * 4:(i + 1) * 4, :], wv_[:, i * 4:(i + 1) * 4, :], writes=[R_wo])
            for tc in range(8):
                s = tc % 2
                t0 = tc * 512
                b.dma("sp", ot[s][:], oT_v[:, 0:nfc, t0:t0 + 512], reads=[R_o], writes=[R_ot[s]])
                b.dma("sp", xc[s][:], xT_v[:, :, t0:t0 + 512], reads=[R_x], writes=[R_xc[s]])
                for dc in range(8):
                    pt, pr = bank()
                    for fc in range(nfc):
                        b.op("pe", lambda e, fc=fc: e.matmul(pt[:, :], lhsT=wo[:, fc, dc * 128:(dc + 1) * 128], rhs=ot[s][:, fc, :],
                                                             start=(fc == 0), stop=(fc == nfc - 1)),
                             reads=[R_wo, R_ot[s]], writes=[pr])
                    b.op("dve", lambda e: e.scalar_tensor_tensor(out=xc[s][:, dc, :], in0=pt[:, :], scalar=modc[:, l, 2, dc:dc + 1],
                                                                 in1=xc[s][:, dc, :], op0=ALU.mult, op1=ALU.add),
                         reads=[pr, R_xc[s]], writes=[R_xc[s]])
                b.dma("sp", xT_v[:, :, t0:t0 + 512], xc[s][:], reads=[R_xc[s]], writes=[R_x])
            b.barrier()

    mla_d = dscr("mla_d", [6, 128, S], BF16)
    mla_v = mla_d.rearrange("c p t -> p c t")
    R_mla = Res()

    def mixer_even(l):
        li = l // 2
        win = w_in_ab[li].rearrange("(dc p) f -> p dc f", p=128)
        with contextlib.ExitStack() as hs:
            hT = b.sb(hs, [128, 8, S], BF16, "hTe")
            R_h = Res()
            norm_all(l, 0, hT, R_h)
            with contextlib.ExitStack() as ps:
                Ct, St, R_T = load_tables(ps)
                wA = b.sb(ps, [128, 8, 704], BF16, "wA")
                wKr = b.sb(ps, [128, 8, 64], BF16, "wKr")
                R_w = Res()
                gq = b.sb(ps, [128, 3], F32, "gq")
                gkv = b.sb(ps, [128, 2], F32, "gkv")
                b.dma("pq", wA[:], win[:, :, 0:704], writes=[R_w])
                b.dma("sp", gq[:], col_view(mla_g_q[li], 3), writes=[R_w], allow_slow_non_contiguous=True)
                b.dma("sp", gkv[:], col_view(mla_g_kv[li], 2), writes=[R_w], allow_slow_non_contiguous=True)
                for dc in range(8):
                    rot_weights(wKr[:, dc, :], wA[:, dc, 640:704], 1, R_w)
                sqb = [b.sb(ps, [128, 3, 512], BF16, "sqb%d" % i) for i in range(2)]
                R_sqb = [Res(), Res()]
                rsq = [b.sb(ps, [128, 512], F32, "rsq%d" % i) for i in range(2)]
                R_rsq = [Res(), Res()]
                ob = [b.sb(ps, [128, 6, 512], BF16, "mob%d" % i) for i in range(2)]
                R_ob = [Res(), Res()]
                t1 = b.sb(ps, [128, 512], F32, "t1")
                t2 = b.sb(ps, [128, 512], F32, "t2")
                R_tmp = Res()
                kk = 0
                for tc in range(8):
                    t0 = tc * 512
                    so = tc % 2
                    for (c0, nch, gcolv, oc0) in ((0, 3, gq, 0), (384, 2, gkv, 3)):
                        s2 = kk % 2
                        kk += 1
                        pcs = []
                        for c in range(nch):
                            pt, pr = bank()
                            for dc in range(8):
                                b.op("pe", lambda e, dc=dc: e.matmul(pt[:, :], lhsT=wA[:, dc, c0 + c * 128:c0 + (c + 1) * 128], rhs=hT[:, dc, t0:t0 + 512],
                                                                     start=(dc == 0), stop=(dc == 7)),
                                     reads=[R_w, R_h], writes=[pr])
                            b.op("act", lambda e: e.activation(out=sqb[s2][:, c, :], in_=pt[:, :], func=AF.Square), reads=[pr], writes=[R_sqb[s2]])
                            pcs.append((pt, pr))
                        pss, prs = bank()
                        for c in range(nch):
                            b.op("pe", lambda e: e.matmul(pss[:, :], lhsT=onesb[:], rhs=sqb[s2][:, c, :], start=(c == 0), stop=(c == nch - 1)),
                                 reads=[R_sqb[s2]], writes=[prs])
                        b.op("act", lambda e: e.activation(out=rsq[s2][:], in_=pss[:, :], func=AF.Sqrt, bias=epsc[:], scale=1.0 / (nch * 128)),
                             reads=[prs], writes=[R_rsq[s2]])
                        b.op("dve", lambda e: e.reciprocal(out=rsq[s2][:], in_=rsq[s2][:]), reads=[R_rsq[s2]], writes=[R_rsq[s2]])
                        for c in range(nch):
                            pt, pr = pcs[c]
                            b.op("dve", lambda e: e.scalar_tensor_tensor(out=ob[so][:, oc0 + c, :], in0=pt[:, :], scalar=gcolv[:, c:c + 1],
                                                                         in1=rsq[s2][:], op0=ALU.mult, op1=ALU.mult),
                                 reads=[pr, R_rsq[s2], R_w], writes=[R_ob[so]])
                    pa, pra = bank()
                    pb_, prb = bank()
                    for dc in range(8):
                        b.op("pe", lambda e, dc=dc: e.matmul(pa[0:64, :], lhsT=wA[:, dc, 640:704], rhs=hT[:, dc, t0:t0 + 512],
                                                             start=(dc == 0), stop=(dc == 7)), reads=[R_w, R_h], writes=[pra])
                    for dc in range(8):
                        b.op("pe", lambda e, dc=dc: e.matmul(pb_[0:64, :], lhsT=wKr[:, dc, :], rhs=hT[:, dc, t0:t0 + 512],
                                                             start=(dc == 0), stop=(dc == 7)), reads=[R_w, R_h], writes=[prb])
                    rope_evac(ob[so][0:64, 5, :], pa[0:64, :], pb_[0:64, :], Ct[0:64, t0:t0 + 512], St[0:64, t0:t0 + 512], t1, t2, R_tmp,
                              [pra, prb, R_T], [R_ob[so]], npart=64)
                    b.dma("sp", mla_v[:, 0:5, t0:t0 + 512], ob[so][:, 0:5, :], reads=[R_ob[so]], writes=[R_mla])
                    b.dma("sp", mla_v[0:64, 5, t0:t0 + 512], ob[so][0:64, 5, :], reads=[R_ob[so]], writes=[R_mla])
                b.barrier()
            with contextlib.ExitStack() as ps:
                Ct, St, R_T = load_tables(ps)
                kT = b.sb(ps, [128, 2, S], BF16, "kTb")
                vd = b.sb(ps, [128, 2, 32, 128], BF16, "vdb")
                R_k = Res()
                R_v = Res()
                wk = b.sb(ps, [128, 8, 256], BF16, "wkd")
                wkr = b.sb(ps, [128, 8, 256], BF16, "wkdr")
                wv = b.sb(ps, [128, 8, 256], BF16, "wvd")
                R_w = Res()
                es_ = b.sb(ps, [128, 16], F32, "esink")
                b.dma("sp", es_[:], swa_sink[li].partition_broadcast(128), writes=[R_w])
                b.op("act", lambda e: e.activation(out=es_[:], in_=es_[:], func=AF.Exp), reads=[R_w], writes=[R_w])
                for g in range(2):
                    for dup in range(2):
                        o0 = g * 128 + dup * 64
                        b.dma("pq", wk[:, :, o0:o0 + 64], win[:, :, 1728 + g * 64:1728 + (g + 1) * 64], writes=[R_w])
                        b.dma("pq", wv[:, :, o0:o0 + 64], win[:, :, 1856 + g * 64:1856 + (g + 1) * 64], writes=[R_w])
                for dc in range(8):
                    rot_weights(wkr[:, dc, :], wk[:, dc, :], 4, R_w)
                t1 = b.sb(ps, [128, 512], F32, "t1")
                t2 = b.sb(ps, [128, 512], F32, "t2")
                R_tmp = Res()
                for tc in range(8):
                    t0 = tc * 512
                    for g in range(2):
                        pa, pra = bank()
                        pb_, prb = bank()
                        for dc in range(8):
                            b.op("pe", lambda e, dc=dc: e.matmul(pa[:, :], lhsT=wk[:, dc, g * 128:(g + 1) * 128], rhs=hT[:, dc, t0:t0 + 512],
                                                                 start=(dc == 0), stop=(dc == 7)), reads=[R_w, R_h], writes=[pra])
                        for dc in range(8):
                            b.op("pe", lambda e, dc=dc: e.matmul(pb_[:, :], lhsT=wkr[:, dc, g * 128:(g + 1) * 128], rhs=hT[:, dc, t0:t0 + 512],
                                                                 start=(dc == 0), stop=(dc == 7)), reads=[R_w, R_h], writes=[prb])
                        rope_evac(kT[:, g, t0:t0 + 512], pa[:, :], pb_[:, :], Ct[:, t0:t0 + 512], St[:, t0:t0 + 512], t1, t2, R_tmp,
                                  [pra, prb, R_T], [R_k])
                for i in range(32):
                    pt, pr = bank()
                    for dc in range(8):
                        b.op("pe", lambda e, dc=dc: e.matmul(pt[:, 0:256], lhsT=hT[:, dc, i * 128:(i + 1) * 128], rhs=wv[:, dc, :],
                                                             start=(dc == 0), stop=(dc == 7)), reads=[R_w, R_h], writes=[pr])
                    b.op("act", lambda e: e.copy(out=vd[:, :, i, :], in_=pt[:, 0:256].rearrange("p (g f) -> p g f", g=2)),
                         reads=[pr], writes=[R_v])
                wq = [b.sb(ps, [128, 8, 128], BF16, "wq%d" % i) for i in range(2)]
                wqr = [b.sb(ps, [128, 8, 128], BF16, "wqr%d" % i) for i in range(2)]
                R_wq = [Res(), Res()]
                qT = [b.sb(ps, [128, S], BF16, "qTb%d" % i) for i in range(2)]
                R_q = [Res(), Res()]
                oTj = [b.sb(ps, [128, S], BF16, "oTb%d" % i) for i in range(2)]
                R_oj = [Res(), Res()]
                Pb = [b.sb(ps, [128, 384], BF16, "Pb%d" % i) for i in range(3)]
                R_P = [Res() for _ in range(3)]
                dn = [b.sb(ps, [128, 128], F32, "dn%d" % i) for i in range(2)]
                R_dn = [Res(), Res()]
                pks = [0, 0]
                for j in range(8):
                    s = j % 2
                    g = j // 4
                    b.dma("pq", wq[s][:], win[:, :, 704 + j * 128:704 + (j + 1) * 128], writes=[R_wq[s]])
                    for dc in range(8):
                        rot_weights(wqr[s][:, dc, :], wq[s][:, dc, :], 2, R_wq[s])
                    for tc in range(8):
                        t0 = tc * 512
                        pa, pra = bank()
                        pb_, prb = bank()
                        for dc in range(8):
                            b.op("pe", lambda e, dc=dc: e.matmul(pa[:, :], lhsT=wq[s][:, dc, :], rhs=hT[:, dc, t0:t0 + 512],
                                                                 start=(dc == 0), stop=(dc == 7)), reads=[R_wq[s], R_h], writes=[pra])
                        for dc in range(8):
                            b.op("pe", lambda e, dc=dc: e.matmul(pb_[:, :], lhsT=wqr[s][:, dc, :], rhs=hT[:, dc, t0:t0 + 512],
                                                                 start=(dc == 0), stop=(dc == 7)), reads=[R_wq[s], R_h], writes=[prb])
                        rope_evac(qT[s][:, t0:t0 + 512], pa[:, :], pb_[:, :], Ct[:, t0:t0 + 512], St[:, t0:t0 + 512], t1, t2, R_tmp,
                                  [pra, prb, R_T], [R_q[s]])
                    for hh in range(2):
                        h = 2 * j + hh
                        p0 = hh * 64
                        p1 = p0 + 64
                        def swaA(qb):
                            kbs = [kb for kb in (qb - 1, qb, qb + 1) if 0 <= kb < 32]
                            n = len(kbs)
                            pS, prS = bank()
                            for idx, kb in enumerate(kbs):
                                b.op("pe", lambda e: e.matmul(pS[:, idx * 128:(idx + 1) * 128], lhsT=kT[p0:p1, g, kb * 128:(kb + 1) * 128],
                                                              rhs=qT[s][p0:p1, qb * 128:(qb + 1) * 128], start=True, stop=(kb == qb)),
                                     reads=[R_k, R_q[s]], writes=[prS])
                                if kb != qb:
                                    m = 0 if kb < qb else 1
                                    b.op("pe", lambda e: e.matmul(pS[:, idx * 128:(idx + 1) * 128], lhsT=identb[:], rhs=maskb[:, m, :],
                                                                  start=False, stop=True), writes=[prS])
                            pi = pks[0] % 3
                            pks[0] += 1
                            b.op("act", lambda e: e.activation(out=Pb[pi][:, 0:n * 128], in_=pS[:, 0:n * 128], func=AF.Exp, scale=0.125),
                                 reads=[prS], writes=[R_P[pi]])
                            return (qb, kbs, pi)

                        def swaB(st_):
                            qb, kbs, pi = st_
                            n = len(kbs)
                            po, pro = bank()
                            pd, prd = bank()
                            for idx, kb in enumerate(kbs):
                                b.op("pe", lambda e: e.matmul(po[:, 0:128], lhsT=vd[:, g, kb, :], rhs=Pb[pi][:, idx * 128:(idx + 1) * 128],
                                                              start=(idx == 0), stop=(idx == n - 1)), reads=[R_v, R_P[pi]], writes=[pro])
                            for idx, kb in enumerate(kbs):
                                b.op("pe", lambda e: e.matmul(pd[:, 0:128], lhsT=onesb[:], rhs=Pb[pi][:, idx * 128:(idx + 1) * 128],
                                                              start=(idx == 0), stop=(idx == n - 1)), reads=[R_P[pi]], writes=[prd])
                            di = pks[1] % 2
                            pks[1] += 1
                            b.op("dve", lambda e: e.tensor_scalar(out=dn[di][p0:p1, :], in0=pd[p0:p1, 0:128], scalar1=es_[p0:p1, h:h + 1], scalar2=None,
                                                                  op0=ALU.add), reads=[prd, R_w], writes=[R_dn[di]])
                            b.op("dve", lambda e: e.reciprocal(out=dn[di][p0:p1, :], in_=dn[di][p0:p1, :]), reads=[R_dn[di]], writes=[R_dn[di]])
                            b.op("dve", lambda e: e.tensor_tensor(out=oTj[s][p0:p1, qb * 128:(qb + 1) * 128], in0=po[p0:p1, 0:128],
                                                                  in1=dn[di][p0:p1, :], op=ALU.mult), reads=[pro, R_dn[di]], writes=[R_oj[s]])

                        nxt = swaA(0)
                        for qb in range(32):
                            cur = nxt
                            if qb + 1 < 32:
                                nxt = swaA(qb + 1)
                            swaB(cur)
                    b.dma("sp", oT_v[:, 8 + j, :], oTj[s][:], reads=[R_oj[s]], writes=[R_o])
                b.barrier()
        with contextlib.ExitStack() as ps:
            Ct, St, R_T = load_tables(ps)
            cqn = b.sb(ps, [128, 3, S], BF16, "cqn")
            ckvn = b.sb(ps, [128, 2, S], BF16, "ckvn")
            krT = b.sb(ps, [64, S], BF16, "krT")
            R_c = Res()
            b.dma("sp", cqn[:], mla_v[:, 0:3, :], reads=[R_mla], writes=[R_c])
            b.dma("sp", ckvn[:], mla_v[:, 3:5, :], reads=[R_mla], writes=[R_c])
            b.dma("sp", krT[:], mla_v[0:64, 5, :], reads=[R_mla], writes=[R_c])
            wqh = [b.sb(ps, [128, 3, 192], BF16, "wqh%d" % i) for i in range(2)]
            wqhr = [b.sb(ps, [128, 3, 64], BF16, "wqhr%d" % i) for i in range(2)]
            wkvh = [b.sb(ps, [128, 2, 256], BF16, "wkvh%d" % i) for i in range(2)]
            R_wh = [Res(), Res()]
            qn = [b.sb(ps, [128, S], BF16, "qn%d" % i) for i in range(2)]
            qr = [b.sb(ps, [64, S], BF16, "qr%d" % i) for i in range(2)]
            kn = [b.sb(ps, [128, S], BF16, "kn%d" % i) for i in range(2)]
            vv = [b.sb(ps, [128, 32, 128], BF16, "vv%d" % i) for i in range(2)]
            R_hd = [Res(), Res()]
            oTh = [b.sb(ps, [128, S], BF16, "oTh%d" % i) for i in range(2)]
            R_oh = [Res(), Res()]
            Pm = [b.sb(ps, [128, 512], BF16, "Pm%d" % i) for i in range(4)]
            R_Pm = [Res() for _ in range(4)]
            Pacc = [[b.sb(ps, [128, 512], F32, "Pacc%d%d" % (i, k_)) for k_ in range(2)] for i in range(2)]
            R_Pacc = [[Res(), Res()], [Res(), Res()]]
            dn = [b.sb(ps, [128, 512], F32, "dnm%d" % i) for i in range(2)]
            R_dn = [Res(), Res()]
            t1 = b.sb(ps, [128, 512], F32, "t1")
            t2 = b.sb(ps, [128, 512], F32, "t2")
            R_tmp = Res()
            wqb_v = mla_w_qb[li].rearrange("(c p) f -> p c f", p=128)
            wkvb_v = mla_w_kvb[li].rearrange("(c p) f -> p c f", p=128)
            SC = float(192.0 ** -0.5)
            pkm = [0]
            acc_st = [0]
            s_st = [0]
            for h in range(8):
                s = h % 2
                b.dma("pq", wqh[s][:], wqb_v[:, :, h * 192:(h + 1) * 192], writes=[R_wh[s]])
                b.dma("pq", wkvh[s][:], wkvb_v[:, :, h * 256:(h + 1) * 256], writes=[R_wh[s]])
                for c in range(3):
                    rot_weights(wqhr[s][:, c, :], wqh[s][:, c, 128:192], 1, R_wh[s])
                for tc in range(8):
                    t0 = tc * 512
                    pt, pr = fbank([4, 5], s_st)
                    for c in range(3):
                        b.op("pe", lambda e: e.matmul(pt[:, :], lhsT=wqh[s][:, c, 0:128], rhs=cqn[:, c, t0:t0 + 512], start=(c == 0), stop=(c == 2)),
                             reads=[R_wh[s], R_c], writes=[pr])
                    b.op("act", lambda e: e.copy(out=qn[s][:, t0:t0 + 512], in_=pt[:, :]), reads=[pr], writes=[R_hd[s]])
                    pt, pr = fbank([4, 5], s_st)
                    for c in range(2):
                        b.op("pe", lambda e: e.matmul(pt[:, :], lhsT=wkvh[s][:, c, 0:128], rhs=ckvn[:, c, t0:t0 + 512], start=(c == 0), stop=(c == 1)),
                             reads=[R_wh[s], R_c], writes=[pr])
                    b.op("act", lambda e: e.copy(out=kn[s][:, t0:t0 + 512], in_=pt[:, :]), reads=[pr], writes=[R_hd[s]])
                    pa, pra = fbank([4, 5], s_st)
                    pb_, prb = fbank([4, 5], s_st)
                    for c in range(3):
                        b.op("pe", lambda e: e.matmul(pa[0:64, :], lhsT=wqh[s][:, c, 128:192], rhs=cqn[:, c, t0:t0 + 512], start=(c == 0), stop=(c == 2)),
                             reads=[R_wh[s], R_c], writes=[pra])
                    for c in range(3):
                        b.op("pe", lambda e: e.matmul(pb_[0:64, :], lhsT=wqhr[s][:, c, :], rhs=cqn[:, c, t0:t0 + 512], start=(c == 0), stop=(c == 2)),
                             reads=[R_wh[s], R_c], writes=[prb])
                    rope_evac(qr[s][:, t0:t0 + 512], pa[0:64, :], pb_[0:64, :], Ct[0:64, t0:t0 + 512], St[0:64, t0:t0 + 512], t1, t2, R_tmp,
                              [pra, prb, R_T], [R_hd[s]], npart=64)
                for i in range(32):
                    pt, pr = fbank([4, 5], s_st)
                    for c in range(2):
                        b.op("pe", lambda e: e.matmul(pt[:, 0:128], lhsT=ckvn[:, c, i * 128:(i + 1) * 128], rhs=wkvh[s][:, c, 128:256],
                                                      start=(c == 0), stop=(c == 1)), reads=[R_wh[s], R_c], writes=[pr])
                    b.op("act", lambda e: e.copy(out=vv[s][:, i, :], in_=pt[:, 0:128]), reads=[pr], writes=[R_hd[s]])
                for qc in range(8):
                    q0 = qc * 512
                    a = acc_st[0] % 2
                    acc_st[0] += 1
                    po, pro = banks[0 + 2 * a]
                    pd, prd = banks[1 + 2 * a]
                    def emitS(kt):
                        pS, prS = fbank([4, 5], s_st)
                        b.op("pe", lambda e: e.matmul(pS[:, :], lhsT=kn[s][:, kt * 128:(kt + 1) * 128], rhs=qn[s][:, q0:q0 + 512], start=True, stop=False),
                             reads=[R_hd[s]], writes=[prS])
                        b.op("pe", lambda e: e.matmul(pS[:, :], lhsT=krT[:, kt * 128:(kt + 1) * 128], rhs=qr[s][:, q0:q0 + 512], start=False, stop=True),
                             reads=[R_hd[s], R_c], writes=[prS])
                        pi = pkm[0] % 4
                        pkm[0] += 1
                        b.op("act", lambda e: e.activation(out=Pm[pi][:], in_=pS[:, :], func=AF.Exp, scale=SC), reads=[prS], writes=[R_Pm[pi]])
                        return pi

                    nxt = emitS(0)
                    for kt in range(32):
                        pi = nxt
                        if kt + 1 < 32:
                            nxt = emitS(kt + 1)
                        b.op("pe", lambda e: e.matmul(po[:, :], lhsT=vv[s][:, kt, :], rhs=Pm[pi][:], start=(kt == 0), stop=(kt == 31)),
                             reads=[R_hd[s], R_Pm[pi]], writes=[pro])
                        w_ = 1 if (kt % 3 == 2) else 0
                        eng_ = "pool" if w_ else "dve"
                        if kt == 0 or kt == 2:
                            b.op(eng_, lambda e: e.tensor_copy(out=Pacc[a][w_][:], in_=Pm[pi][:]), reads=[R_Pm[pi]], writes=[R_Pacc[a][w_]])
                        else:
                            b.op(eng_, lambda e: e.tensor_tensor(out=Pacc[a][w_][:], in0=Pacc[a][w_][:], in1=Pm[pi][:], op=ALU.add),
                                 reads=[R_Pm[pi], R_Pacc[a][w_]], writes=[R_Pacc[a][w_]])
                    for w_ in range(2):
                        b.op("pe", lambda e: e.matmul(pd[:, :], lhsT=meanm[:], rhs=Pacc[a][w_][:], start=(w_ == 0), stop=(w_ == 1)),
                             reads=[R_Pacc[a][w_]], writes=[prd])
                    b.op("dve", lambda e: e.reciprocal(out=dn[a][:], in_=pd[:, :]), reads=[prd], writes=[R_dn[a]])
                    b.op("dve", lambda e: e.scalar_tensor_tensor(out=oTh[s][:, q0:q0 + 512], in0=po[:, :], scalar=1.0 / D, in1=dn[a][:],
                                                                 op0=ALU.mult, op1=ALU.mult),
                         reads=[pro, R_dn[a]], writes=[R_oh[s]])
                b.dma("sp", oT_v[:, h, :], oTh[s][:], reads=[R_oh[s]], writes=[R_o])
            b.barrier()
        out_proj(l, w_out_ab[li], 16)

    def mixer_odd(l):
        li = l // 2
        win = w_in_c[li].rearrange("(dc p) f -> p dc f", p=128)
        with contextlib.ExitStack() as hs:
            hT = b.sb(hs, [128, 8, S], BF16, "hTo")
            R_h = Res()
            norm_all(l, 0, hT, R_h)
            Ct, St, R_T = load_tables(hs)
            num = b.sb(hs, [128, S], F32, "num")
            den = b.sb(hs, [128, S], F32, "den")
            R_nd = Res()
            oTj = b.sb(hs, [128, S], BF16, "oTc")
            R_oj = Res()
            t1 = b.sb(hs, [128, 512], F32, "t1")
            t2 = b.sb(hs, [128, 512], F32, "t2")
            R_tmp = Res()
            wts = [b.sb(hs, [128, 8, 128], BF16, "wc%d" % i) for i in range(5)]
            R_w = Res()
            KPMAX = S + 128 * 16
            qP = b.sb(hs, [128, S], BF16, "qP")
            kP = b.sb(hs, [128, KPMAX], BF16, "kP")
            vP = b.sb(hs, [128, KPMAX], BF16, "vP")
            Vt = b.sb(hs, [128, KPMAX // 128, 128], BF16, "Vt")
            R_q = Res()
            R_k = Res()
            R_vp = Res()
            R_vt = Res()
            Pb = [b.sb(hs, [128, 256], BF16, "Pc%d" % i) for i in range(3)]
            R_P = [Res() for _ in range(3)]
            pkd = [0]
            def load_w(j_, g_):
                base = g_ * 3072 + j_ * 128
                b.dma("pq", wts[0][:], win[:, :, base:base + 128], writes=[R_w])
                b.dma("pq", wts[2][:], win[:, :, base + 1024:base + 1152], writes=[R_w])
                b.dma("pq", wts[4][:], win[:, :, base + 2048:base + 2176], writes=[R_w])
                for dc in range(8):
                    rot_weights(wts[1][:, dc, :], wts[0][:, dc, :], 2, R_w)
                    rot_weights(wts[3][:, dc, :], wts[2][:, dc, :], 2, R_w)

            load_w(0, 0)
            for j in range(8):
                for g, d in enumerate((1, 4, 16)):
                    L = S // d
                    Lp = L + 128
                    nt = L // 128 + 1
                    nq = L // 128
                    qv = qP[:, :].rearrange("p (r m) -> p r m", r=d)
                    kv = kP[:, 0:d * Lp].rearrange("p (r m) -> p r m", r=d)
                    vv_ = vP[:, 0:d * Lp].rearrange("p (r m) -> p r m", r=d)
                    b.op("pool", lambda e: e.memset(kv[:, :, 0:64], 0.0), writes=[R_k])
                    b.op("pool", lambda e: e.memset(kv[:, :, 64 + L:Lp], 0.0), writes=[R_k])
                    b.op("pool", lambda e: e.memset(vv_[:, :, 0:64], 0.0), writes=[R_vp])
                    b.op("pool", lambda e: e.memset(vv_[:, :, 64 + L:Lp], 0.0), writes=[R_vp])
                    for tc in range(8):
                        t0 = tc * 512
                        m0 = t0 // d
                        mw = 512 // d
                        prs = []
                        for wi in range(5):
                            pt, pr = bank()
                            for dc in range(8):
                                b.op("pe", lambda e, dc=dc: e.matmul(pt[:, :], lhsT=wts[wi][:, dc, :], rhs=hT[:, dc, t0:t0 + 512],
                                                                     start=(dc == 0), stop=(dc == 7)), reads=[R_w, R_h], writes=[pr])
                            prs.append((pt, pr))
                            if wi == 1:
                                rope_evac(qv[:, :, m0:m0 + mw], prs[0][0][:, :], prs[1][0][:, :], Ct[:, t0:t0 + 512], St[:, t0:t0 + 512],
                                          t1, t2, R_tmp, [prs[0][1], prs[1][1], R_T], [R_q], perm=d)
                            if wi == 3:
                                rope_evac(kv[:, :, 64 + m0:64 + m0 + mw], prs[2][0][:, :], prs[3][0][:, :], Ct[:, t0:t0 + 512], St[:, t0:t0 + 512],
                                          t1, t2, R_tmp, [prs[2][1], prs[3][1], R_T], [R_k], perm=d)
                            if wi == 4:
                                b.op("act", lambda e: e.copy(out=vv_[:, :, 64 + m0:64 + m0 + mw],
                                                             in_=pt[:, :].rearrange("p (m r) -> p r m", r=d)), reads=[pr], writes=[R_vp])
                    if not (j == 7 and g == 2):
                        load_w(j + (g + 1) // 3, (g + 1) % 3)
                    ntile = d * nt
                    for i0 in range(0, ntile, 8):
                        nn = min(8, ntile - i0)
                        pbt, pbr = bbank()
                        for ii in range(nn):
                            i = i0 + ii
                            r_, jt = divmod(i, nt)
                            c0 = r_ * Lp + jt * 128
                            b.op("pe", lambda e: e.transpose(out=pbt[:, ii * 128:(ii + 1) * 128], in_=vP[:, c0:c0 + 128], identity=identb[:]),
                                 reads=[R_vp], writes=[pbr])
                        b.op("act", lambda e: e.copy(out=Vt[:, i0:i0 + nn, :], in_=pbt[:, 0:nn * 128].rearrange("p (i f) -> p i f", f=128)),
                             reads=[pbr], writes=[R_vt])
                    for hh in range(2):
                        p0 = hh * 64
                        p1 = p0 + 64
                        def dilA(r_, n):
                            q0 = r_ * L + n * 128
                            k0 = r_ * Lp + n * 128
                            pS, prS = bank()
                            for idx in range(2):
                                if idx == 0:
                                    m = 2 if n == 0 else 0
                                else:
                                    m = 3 if n == nq - 1 else 1
                                b.op("pe", lambda e: e.matmul(pS[:, idx * 128:(idx + 1) * 128], lhsT=kP[p0:p1, k0 + idx * 128:k0 + (idx + 1) * 128],
                                                              rhs=qP[p0:p1, q0:q0 + 128], start=True, stop=False),
                                     reads=[R_k, R_q], writes=[prS])
                                b.op("pe", lambda e: e.matmul(pS[:, idx * 128:(idx + 1) * 128], lhsT=identb[:], rhs=maskb[:, m, :],
                                                              start=False, stop=True), writes=[prS])
                            pi = pkd[0] % 3
                            pkd[0] += 1
                            b.op("act", lambda e: e.activation(out=Pb[pi][:], in_=pS[:, 0:256], func=AF.Exp, scale=0.125),
                                 reads=[prS], writes=[R_P[pi]])
                            return (r_, n, pi)

                        def dilB(st_):
                            r_, n, pi = st_
                            po, pro = bank()
                            pd, prd = bank()
                            ti = r_ * nt + n
                            for idx in range(2):
                                b.op("pe", lambda e: e.matmul(po[:, 0:128], lhsT=Vt[:, ti + idx, :], rhs=Pb[pi][:, idx * 128:(idx + 1) * 128],
                                                              start=(idx == 0), stop=(idx == 1)), reads=[R_vt, R_P[pi]], writes=[pro])
                            for idx in range(2):
                                b.op("pe", lambda e: e.matmul(pd[:, 0:128], lhsT=onesb[:], rhs=Pb[pi][:, idx * 128:(idx + 1) * 128],
                                                              start=(idx == 0), stop=(idx == 1)), reads=[R_P[pi]], writes=[prd])
                            nv = num[p0:p1, :].rearrange("p (m r) -> p r m", r=d)[:, r_, n * 128:(n + 1) * 128]
                            dv = den[p0:p1, :].rearrange("p (m r) -> p r m", r=d)[:, r_, n * 128:(n + 1) * 128]
                            if g == 0:
                                b.op("act", lambda e: e.copy(out=nv, in_=po[p0:p1, 0:128]), reads=[pro], writes=[R_nd])
                                b.op("dve", lambda e: e.tensor_copy(out=dv, in_=pd[p0:p1, 0:128]), reads=[prd], writes=[R_nd])
                            else:
                                b.op("dve", lambda e: e.tensor_tensor(out=nv, in0=po[p0:p1, 0:128], in1=nv, op=ALU.add), reads=[pro, R_nd], writes=[R_nd])
                                b.op("dve", lambda e: e.tensor_tensor(out=dv, in0=pd[p0:p1, 0:128], in1=dv, op=ALU.add), reads=[prd, R_nd], writes=[R_nd])

                        ulist = [(r_, n) for r_ in range(d) for n in range(nq)]
                        nxt = dilA(*ulist[0])
                        for ui in range(len(ulist)):
                            cur = nxt
                            if ui + 1 < len(ulist):
                                nxt = dilA(*ulist[ui + 1])
                            dilB(cur)
                for q4 in range(8):
                    sl = slice(q4 * 512, (q4 + 1) * 512)
                    b.op("dve", lambda e: e.reciprocal(out=den[:, sl], in_=den[:, sl]), reads=[R_nd], writes=[R_nd])
                    b.op("dve", lambda e: e.tensor_tensor(out=oTj[:, sl], in0=num[:, sl], in1=den[:, sl], op=ALU.mult), reads=[R_nd], writes=[R_oj])
                b.dma("sp", oT_v[:, j, :], oTj[:], reads=[R_oj], writes=[R_o])
            b.barrier()
        out_proj(l, w_out_c[li], 8)

    for l in layers:
        if cfg["mixer"]:
            if l % 2 == 0:
                mixer_even(l)
            else:
                mixer_odd(l)
        if cfg["ffn"]:
            moe_layer(l)

    with contextlib.ExitStack() as ps:
        xc = [b.sb(ps, [128, 8, 512], F32, "fx%d" % i) for i in range(2)]
        sq = b.sb(ps, [128, 8, 512], F32, "fsq")
        rs = b.sb(ps, [128, 512], F32, "frs")
        yo = [b.sb(ps, [128, D], F32, "fy%d" % i) for i in range(2)]
        R_xc = [Res(), Res()]
        R_sq = Res()
        R_rs = Res()
        R_yo = [Res(), Res()]
        k = 0
        for tc in range(8):
            s = tc % 2
            t0 = tc * 512
            b.dma("sp", xc[s][:], xT_v[:, :, t0:t0 + 512], reads=[R_x], writes=[R_xc[s]])
            b.op("act", lambda e: e.activation(out=sq[:], in_=xc[s][:], func=AF.Square), reads=[R_xc[s]], writes=[R_sq])
            pt, pr = bank()
            for dc in range(8):
                b.op("pe", lambda e, dc=dc: e.matmul(pt[:, :], lhsT=meanm[:], rhs=sq[:, dc, :], start=(dc == 0), stop=(dc == 7)),
                     reads=[R_sq], writes=[pr])
            b.op("act", lambda e: e.activation(out=rs[:], in_=pt[:, :], func=AF.Sqrt, bias=epsc[:], scale=1.0),
                 reads=[pr], writes=[R_rs])
            b.op("dve", lambda e: e.reciprocal(out=rs[:], in_=rs[:]), reads=[R_rs], writes=[R_rs])
            for dc in range(8):
                b.op("dve", lambda e, dc=dc: e.scalar_tensor_tensor(out=xc[s][:, dc, :], in0=xc[s][:, dc, :], scalar=gfin[:, dc:dc + 1],
                                                                    in1=rs[:], op0=ALU.mult, op1=ALU.mult),
                     reads=[R_rs, R_xc[s]], writes=[R_xc[s]])
            for i in range(4):
                ys = k % 2
                k += 1
                for hb in range(2):
                    pt, pr = bank()
                    for cc in range(4):
                        dc = hb * 4 + cc
                        b.op("pe", lambda e, dc=dc, cc=cc: e.transpose(out=pt[:, cc * 128:(cc + 1) * 128], in_=xc[s][:, dc, i * 128:(i + 1) * 128],
                                                                       identity=identf[:]),
                             reads=[R_xc[s]], writes=[pr])
                    if hb == 0:
                        b.op("dve", lambda e: e.tensor_copy(out=yo[ys][:, 0:512], in_=pt[:, :]), reads=[pr], writes=[R_yo[ys]])
                    else:
                        b.op("act", lambda e: e.copy(out=yo[ys][:, 512:1024], in_=pt[:, :]), reads=[pr], writes=[R_yo[ys]])
                r0 = t0 + i * 128
                b.dma("sp", y_out[r0:r0 + 128, :], yo[ys][:], reads=[R_yo[ys]], writes=[R_o])
        b.barrier()
    return nc


def make_consts():
    identf = np.eye(128, dtype=np.float32)
    i = np.arange(128) % 32
    inv = (10000.0 ** (-(2.0 * i.astype(np.float32)) / 64.0)).astype(np.float32).reshape(128, 1)
    a = np.arange(128)[:, None]
    bq = np.arange(128)[None, :]
    NEG = -30000.0
    masks = np.zeros((128, 4, 128), dtype=np.float32)
    masks[:, 0, :] = np.where(a >= bq, 0.0, NEG)
    masks[:, 1, :] = np.where(a <= bq, 0.0, NEG)
    masks[:, 2, :] = np.where((a >= bq) & (a >= 64), 0.0, NEG)
    masks[:, 3, :] = np.where((a <= bq) & (a < 64), 0.0, NEG)
    return {"k_identf": identf, "k_inv": inv, "k_masks": masks}


_CACHE = {}


def kernel(**inputs):
    cfg = CFG
    ncores = cfg["ncores"]
    key = repr(cfg)
    if key not in _CACHE:
        _CACHE[key] = build(cfg)
    nc = _CACHE[key]
    consts = make_consts()
    shared = {}
    for k_, v in inputs.items():
        if k_ in ("x", "c", "positions"):
            continue
        shared[k_] = np.ascontiguousarray(v)
    in_maps = []
    for cidx in range(ncores):
        m = dict(shared)
        m.update(consts)
        m["x"] = np.ascontiguousarray(inputs["x"][cidx])
        m["c"] = np.ascontiguousarray(inputs["c"][cidx])
        m["positions"] = np.ascontiguousarray(inputs["positions"][cidx]).astype(np.int32)
        in_maps.append(m)
    res = run_bass_kernel_spmd(nc, in_maps, core_ids=list(range(ncores)))
    out = np.stack([np.asarray(r["y"]) for r in res.results], axis=0)
    if ncores < 8:
        full = np.zeros((8, S, D), dtype=np.float32)
        full[:ncores] = out
        out = full
    return out.astype(np.float32)
```

```python
import contextlib
import numpy as np
import ml_dtypes
import concourse.bass as bass
import concourse.mybir as mybir
from concourse.bass_utils import run_bass_kernel_spmd

F32 = mybir.dt.float32
BF16 = mybir.dt.bfloat16
I32 = mybir.dt.int32
AF = mybir.ActivationFunctionType
ALU = mybir.AluOpType
AX = mybir.AxisListType

S = 4096
D = 1024
DEPTH = 4
NE = 32
DFF = 1024
EPS = 1e-6
NQ = 8
STRICT = True

CFG = {"layers": [0, 1, 2, 3], "mixer": True, "ffn": True, "ncores": 8}


class Res:
    __slots__ = ("w", "r")

    def __init__(self):
        self.w = None
        self.r = {}


class Builder:
    def __init__(self):
        nc = self.nc = bass.Bass("TRN2", target_bir_lowering=False)
        self.es = contextlib.ExitStack()
        self.streams = {"pe": nc.tensor, "act": nc.scalar, "dve": nc.vector, "pool": nc.gpsimd, "sp": nc.sync}
        self.csem = {}
        self.ccount = {}
        for e in ("pe", "act", "dve", "pool"):
            self.csem[e] = self.es.enter_context(nc.semaphore("c_" + e))
            self.ccount[e] = 0
        self.qsems = {}
        self.qcount = {}
        self.qissuer = {"sp": "sp", "pq": "pool"}
        for q in ("sp", "pq"):
            self.qsems[q] = [self.es.enter_context(nc.semaphore("q_%s%d" % (q, j))) for j in range(NQ)]
            self.qcount[q] = 0
        self.waited = {s: {} for s in self.streams}
        self.uid = 0

    def name(self, p):
        self.uid += 1
        return "%s_%d" % (p, self.uid)

    def sb(self, ctx, shape, dt, nm="t"):
        return ctx.enter_context(self.nc.sbuf_tensor(self.name(nm), list(shape), dt))

    def _wait(self, stream, tok):
        sem, val, owner = tok
        if owner == stream and (stream == "pe" or not STRICT):
            return
        w = self.waited[stream]
        if w.get(id(sem), 0) >= val:
            return
        self.streams[stream].wait_ge(sem, val)
        w[id(sem)] = val

    def _deps(self, stream, reads, writes):
        for r in reads:
            if r.w is not None:
                self._wait(stream, r.w)
        for r in writes:
            if r.w is not None:
                self._wait(stream, r.w)
            for t in r.r.values():
                self._wait(stream, t)

    def _mark(self, tok, reads, writes):
        for r in reads:
            r.r[id(tok[0])] = tok
        for r in writes:
            r.w = tok
            r.r = {}

    def op(self, eng, fn, reads=(), writes=()):
        self._deps(eng, reads, writes)
        ins = fn(self.streams[eng])
        self.ccount[eng] += 1
        tok = (self.csem[eng], self.ccount[eng], eng)
        ins.then_inc(tok[0], 1)
        self._mark(tok, reads, writes)

    def dma(self, q, out, in_, reads=(), writes=(), **kw):
        issuer = self.qissuer[q]
        n = self.qcount[q]
        sem = self.qsems[q][n % NQ]
        if n >= NQ:
            self._wait(issuer, (sem, 16 * (n // NQ), None))
        self._deps(issuer, reads, writes)
        eng = self.nc.sync if q == "sp" else self.nc.gpsimd
        ins = eng.dma_start(out=out, in_=in_, **kw)
        ins.then_inc(sem, 16)
        self.qcount[q] += 1
        tok = (sem, 16 * (n // NQ + 1), None)
        self._mark(tok, reads, writes)

    def barrier(self):
        toks = []
        for e in self.csem:
            if self.ccount[e] > 0:
                toks.append((self.csem[e], self.ccount[e], e))
        for q in self.qsems:
            n = self.qcount[q]
            for j in range(NQ):
                cnt = (n - j + NQ - 1) // NQ if n > j else 0
                if cnt > 0:
                    toks.append((self.qsems[q][j], 16 * cnt, None))
        for s in self.streams:
            for t in toks:
                self._wait(s, t)


def build(cfg):
    b = Builder()
    nc = b.nc
    es = b.es
    layers = cfg["layers"]

    def din(name, shape, dt=F32):
        return nc.dram_tensor(name, list(shape), dt, kind="ExternalInput").ap()

    def dscr(name, shape, dt=F32):
        return nc.dram_tensor(name, list(shape), dt, kind="Internal").ap()

    x_in = din("x", [S, D])
    c_in = din("c", [D])
    pos_in = din("positions", [S], I32)
    w_mod = din("w_mod", [DEPTH, D, 6 * D])
    b_mod = din("b_mod", [DEPTH, 6 * D])
    g_mix = din("g_norm_mix", [DEPTH, D])
    g_ffn = din("g_norm_ffn", [DEPTH, D])
    w_in_ab = din("w_in_ab", [2, D, 1984])
    mla_g_q = din("mla_g_q", [2, 384])
    mla_w_qb = din("mla_w_qb", [2, 384, 1536])
    mla_g_kv = din("mla_g_kv", [2, 256])
    mla_w_kvb = din("mla_w_kvb", [2, 256, 2048])
    swa_sink = din("swa_sink", [2, 16])
    w_out_ab = din("w_out_ab", [2, 2048, D])
    w_in_c = din("w_in_c", [2, D, 9216])
    w_out_c = din("w_out_c", [2, 1024, D])
    w_router = din("w_router", [DEPTH, D, NE])
    b_router = din("b_router", [DEPTH, NE])
    w_gu = din("w_gu", [DEPTH, NE, D, 2 * DFF])
    b_gu = din("b_gu", [DEPTH, NE, 2 * DFF])
    w_down = din("w_down", [DEPTH, NE, DFF, D])
    b_down = din("b_down", [DEPTH, NE, D])
    g_final = din("g_final", [D])
    k_identf = din("k_identf", [128, 128])
    k_inv = din("k_inv", [128, 1])
    k_masks = din("k_masks", [128, 4, 128])
    y_out = nc.dram_tensor("y", [S, D], F32, kind="ExternalOutput").ap()

    xT_d = dscr("xT_d", [8, 128, S])
    oT_d = dscr("oT_d", [16, 128, S], BF16)
    tab_d = dscr("tab_d", [2, 128, S])
    gt_d = dscr("gt_d", [NE, 2048])
    xT_v = xT_d.rearrange("c p t -> p c t")
    oT_v = oT_d.rearrange("c p t -> p c t")
    R_x = Res()
    R_o = Res()
    R_tab = Res()

    identf = b.sb(es, [128, 128], F32, "identf")
    identb = b.sb(es, [128, 128], BF16, "identb")
    meanm = b.sb(es, [128, 128], F32, "meanm")
    onesb = b.sb(es, [128, 128], BF16, "onesb")
    modc = b.sb(es, [128, DEPTH, 6, 8], F32, "modc")
    gcol = b.sb(es, [128, DEPTH, 2, 8], F32, "gcol")
    gfin = b.sb(es, [128, 8], F32, "gfin")
    epsc = b.sb(es, [128, 1], F32, "epsc")
    R_const = Res()

    banks = []
    for i in range(6):
        t = es.enter_context(nc.psum_tensor(b.name("psf"), [128, 512], F32))
        banks.append((t, Res()))
    bbanks = []
    for i in range(2):
        t = es.enter_context(nc.psum_tensor(b.name("psb"), [128, 1024], BF16))
        bbanks.append((t, Res()))
    rr = {"f": 0, "b": 0}

    def bank():
        rr["f"] = (rr["f"] + 1) % 6
        return banks[rr["f"]]

    def bbank():
        rr["b"] = (rr["b"] + 1) % 2
        return bbanks[rr["b"]]

    def col_view(vec_ap, n):
        return vec_ap.rearrange("(j p) -> p j", p=128)

    b.dma("sp", identf[:], k_identf[:, :], writes=[R_const])
    b.op("dve", lambda e: e.tensor_copy(out=identb[:], in_=identf[:]), reads=[R_const], writes=[R_const])
    b.op("pool", lambda e: e.memset(meanm[:], 1.0 / D), writes=[R_const])
    b.op("pool", lambda e: e.memset(onesb[:], 1.0), writes=[R_const])
    b.op("pool", lambda e: e.memset(epsc[:], EPS), writes=[R_const])
    b.dma("sp", gfin[:], col_view(g_final, 8), writes=[R_const], allow_slow_non_contiguous=True)
    for l in layers:
        b.dma("sp", gcol[:, l, 0, :], col_view(g_mix[l], 8), writes=[R_const], allow_slow_non_contiguous=True)
        b.dma("sp", gcol[:, l, 1, :], col_view(g_ffn[l], 8), writes=[R_const], allow_slow_non_contiguous=True)

    with contextlib.ExitStack() as ps:
        cT = b.sb(ps, [128, 8], F32, "cT")
        cS = b.sb(ps, [128, 8], F32, "cS")
        bmc = b.sb(ps, [128, DEPTH, 48], F32, "bmc")
        wm = [b.sb(ps, [128, 8, 1024], F32, "wm%d" % i) for i in range(2)]
        R_wm = [Res(), Res()]
        R_c = Res()
        b.dma("sp", cT[:], col_view(c_in, 8), writes=[R_c], allow_slow_non_contiguous=True)
        for l in layers:
            b.dma("sp", bmc[:, l, :], col_view(b_mod[l], 48), writes=[R_c], allow_slow_non_contiguous=True)
        b.op("act", lambda e: e.activation(out=cS[:], in_=cT[:], func=AF.Sigmoid), reads=[R_c], writes=[R_c])
        b.op("dve", lambda e: e.tensor_tensor(out=cS[:], in0=cS[:], in1=cT[:], op=ALU.mult), reads=[R_c], writes=[R_c])
        k = 0
        for l in layers:
            for v in range(6):
                slot = k % 2
                k += 1
                b.dma("sp", wm[slot][:], w_mod[l].rearrange("(dc p) f -> p dc f", p=128)[:, :, v * 1024:(v + 1) * 1024],
                      writes=[R_wm[slot]])
                pt, pr = bank()
                for j in range(8):
                    for dc in range(8):
                        b.op("pe", lambda e, j=j, dc=dc: e.matmul(pt[:, j:j + 1], lhsT=wm[slot][:, dc, j * 128:(j + 1) * 128],
                                                                   rhs=cS[:, dc:dc + 1], start=(dc == 0), stop=(dc == 7)),
                             reads=[R_wm[slot], R_c], writes=[pr])
                b.op("dve", lambda e: e.tensor_tensor(out=modc[:, l, v, :], in0=pt[:, 0:8], in1=bmc[:, l, v * 8:(v + 1) * 8], op=ALU.add),
                     reads=[pr, R_c], writes=[R_const])
        for l in layers:
            for (v, gi) in ((1, 0), (4, 1)):
                b.op("dve", lambda e, l=l, v=v, gi=gi: e.scalar_tensor_tensor(out=modc[:, l, v, :], in0=modc[:, l, v, :], scalar=1.0,
                                                                              in1=gcol[:, l, gi, :], op0=ALU.add, op1=ALU.mult),
                     reads=[R_const], writes=[R_const])
        b.barrier()

    with contextlib.ExitStack() as ps:
        xt = [b.sb(ps, [128, D], F32, "xt%d" % i) for i in range(2)]
        xo = [b.sb(ps, [128, 8, 128], F32, "xo%d" % i) for i in range(2)]
        R_xt = [Res(), Res()]
        R_xo = [Res(), Res()]
        for i in range(32):
            s = i % 2
            b.dma("sp", xt[s][:], x_in[i * 128:(i + 1) * 128, :], writes=[R_xt[s]])
            for hb in range(2):
                pt, pr = bank()
                for cc in range(4):
                    c = hb * 4 + cc
                    b.op("pe", lambda e, c=c, cc=cc: e.transpose(out=pt[:, cc * 128:(cc + 1) * 128], in_=xt[s][:, c * 128:(c + 1) * 128],
                                                                 identity=identf[:]),
                         reads=[R_xt[s]], writes=[pr])
                eng = "dve" if hb == 0 else "act"
                if eng == "dve":
                    b.op("dve", lambda e: e.tensor_copy(out=xo[s][:, hb * 4:(hb + 1) * 4, :],
                                                        in_=pt[:, :].rearrange("p (c t) -> p c t", c=4)),
                         reads=[pr], writes=[R_xo[s]])
                else:
                    b.op("act", lambda e: e.copy(out=xo[s][:, hb * 4:(hb + 1) * 4, :],
                                                 in_=pt[:, :].rearrange("p (c t) -> p c t", c=4)),
                         reads=[pr], writes=[R_xo[s]])
            b.dma("sp", xT_v[:, :, i * 128:(i + 1) * 128], xo[s][:], reads=[R_xo[s]], writes=[R_x])
        b.barrier()

    def norm_chunk(ctx_bufs, l, which, t0, hT, hcol0, R_h):
        xc, R_xc, sq, R_sq, rs, R_rs, tmp, R_tmp = ctx_bufs[(t0 // 512) % 2]
        va = 1 if which == 0 else 4
        vb = 0 if which == 0 else 3
        b.dma("sp", xc[:], xT_v[:, :, t0:t0 + 512], reads=[R_x], writes=[R_xc])
        b.op("act", lambda e: e.activation(out=sq[:], in_=xc[:], func=AF.Square), reads=[R_xc], writes=[R_sq])
        pt, pr = bank()
        for dc in range(8):
            b.op("pe", lambda e, dc=dc: e.matmul(pt[:, :], lhsT=meanm[:], rhs=sq[:, dc, :], start=(dc == 0), stop=(dc == 7)),
                 reads=[R_sq], writes=[pr])
        b.op("act", lambda e: e.activation(out=rs[:], in_=pt[:, :], func=AF.Sqrt, bias=epsc[:], scale=1.0),
             reads=[pr], writes=[R_rs])
        b.op("dve", lambda e: e.reciprocal(out=rs[:], in_=rs[:]), reads=[R_rs], writes=[R_rs])
        for dc in range(8):
            b.op("dve", lambda e, dc=dc: e.scalar_tensor_tensor(out=tmp[:, dc, :], in0=xc[:, dc, :], scalar=modc[:, l, va, dc:dc + 1],
                                                                in1=rs[:], op0=ALU.mult, op1=ALU.mult),
                 reads=[R_xc, R_rs], writes=[R_tmp])
            b.op("act", lambda e, dc=dc: e.activation(out=hT[:, dc, hcol0:hcol0 + 512], in_=tmp[:, dc, :], func=AF.Identity,
                                                      bias=modc[:, l, vb, dc:dc + 1], scale=1.0),
                 reads=[R_tmp], writes=[R_h])

    def norm_bufs(ps):
        sets = []
        for i in range(2):
            xc = b.sb(ps, [128, 8, 512], F32, "xc%d" % i)
            sq = b.sb(ps, [128, 8, 512], F32, "sq%d" % i)
            rs = b.sb(ps, [128, 512], F32, "rs%d" % i)
            R_sq = Res()
            sets.append((xc, Res(), sq, R_sq, rs, Res(), sq, R_sq))
        return sets

    def moe_layer(l):
        for hf in range(2):
            T0 = hf * 2048
            with contextlib.ExitStack() as hs:
                hT = b.sb(hs, [128, 8, 2048], BF16, "hT")
                acc = b.sb(hs, [128, 8, 2048], F32, "acc")
                R_h = Res()
                R_G = Res()
                R_acc = [[Res() for _ in range(4)] for _ in range(8)]
                bgc = b.sb(hs, [128, 16, NE], F32, "bgc")
                R_b = Res()
                R_gt = Res()
                with contextlib.ExitStack() as ps:
                    nb = norm_bufs(ps)
                    wrf = b.sb(ps, [128, 8, NE], F32, "wrf")
                    wrb = b.sb(ps, [128, 8, NE], BF16, "wrb")
                    brb = b.sb(ps, [128, NE], F32, "brb")
                    R_wr = Res()
                    lg = b.sb(ps, [128, NE], F32, "lg")
                    t8 = b.sb(ps, [128, 8], F32, "t8")
                    ng = b.sb(ps, [128, 1], F32, "ng")
                    ee = b.sb(ps, [128, NE], F32, "ee")
                    mk = b.sb(ps, [128, NE], F32, "mk")
                    sm = b.sb(ps, [128, 1], F32, "sm")
                    R_r = Res()
                    bgr = b.sb(ps, [32, 2048], F32, "bgr")
                    GT = b.sb(ps, [32, 2048], F32, "GT")
                    bdn = b.sb(ps, [32, D], F32, "bdn")
                    b.dma("sp", bgr[:], b_gu[l], writes=[R_b])
                    b.dma("sp", bdn[:], b_down[l], writes=[R_b])
                    pt, pr = bank()
                    for c in range(16):
                        b.op("pe", lambda e, c=c: e.transpose(out=pt[:, c * NE:(c + 1) * NE], in_=bgr[:, c * 128:(c + 1) * 128],
                                                              identity=identf[0:32, 0:32]),
                             reads=[R_b], writes=[pr])
                    b.op("dve", lambda e: e.tensor_copy(out=bgc[:], in_=pt[:, :].rearrange("p (c e) -> p c e", c=16)),
                         reads=[pr], writes=[R_b])
                    b.op("dve", lambda e: e.tensor_scalar(out=bgc[:, 8:16, :], in0=bgc[:, 8:16, :], scalar1=1.0, scalar2=None, op0=ALU.add),
                         reads=[R_b], writes=[R_b])
                    b.dma("sp", wrf[:], w_router[l].rearrange("(dc p) e -> p dc e", p=128), writes=[R_wr])
                    b.dma("sp", brb[:], b_router[l].partition_broadcast(128), writes=[R_wr])
                    b.op("dve", lambda e: e.tensor_copy(out=wrb[:], in_=wrf[:]), reads=[R_wr], writes=[R_wr])
                    for tc in range(4):
                        norm_chunk(nb, l, 1, T0 + tc * 512, hT, tc * 512, R_h)
                    for i in range(16):
                        pt, pr = bank()
                        for dc in range(8):
                            b.op("pe", lambda e, dc=dc: e.matmul(pt[:, 0:NE], lhsT=hT[:, dc, i * 128:(i + 1) * 128], rhs=wrb[:, dc, :],
                                                                 start=(dc == 0), stop=(dc == 7)),
                                 reads=[R_h, R_wr], writes=[pr])
                        b.op("dve", lambda e: e.tensor_tensor(out=lg[:], in0=pt[:, 0:NE], in1=brb[:], op=ALU.add),
                             reads=[pr, R_wr], writes=[R_r])
                        b.op("dve", lambda e: e.max(out=t8[:], in_=lg[:]), reads=[R_r], writes=[R_r])
                        b.op("dve", lambda e: e.tensor_scalar(out=ng[:], in0=t8[:, 0:1], scalar1=-1.0, scalar2=None, op0=ALU.mult),
                             reads=[R_r], writes=[R_r])
                        b.op("act", lambda e: e.activation(out=ee[:], in_=lg[:], func=AF.Exp, bias=ng[:], scale=1.0),
                             reads=[R_r], writes=[R_r])
                        b.op("dve", lambda e: e.tensor_scalar(out=mk[:], in0=lg[:], scalar1=t8[:, 3:4], scalar2=None, op0=ALU.is_ge),
                             reads=[R_r], writes=[R_r])
                        b.op("dve", lambda e: e.tensor_tensor(out=ee[:], in0=ee[:], in1=mk[:], op=ALU.mult), reads=[R_r], writes=[R_r])
                        b.op("dve", lambda e: e.reduce_sum(out=sm[:], in_=ee[:], axis=AX.X), reads=[R_r], writes=[R_r])
                        b.op("dve", lambda e: e.reciprocal(out=sm[:], in_=sm[:]), reads=[R_r], writes=[R_r])
                        b.op("dve", lambda e: e.tensor_scalar(out=ee[:], in0=ee[:], scalar1=sm[:, 0:1], scalar2=None, op0=ALU.mult),
                             reads=[R_r], writes=[R_r])
                        pt2, pr2 = bank()
                        b.op("pe", lambda e: e.transpose(out=pt2[0:NE, 0:128], in_=ee[:], identity=identf[:]), reads=[R_r], writes=[pr2])
                        b.op("act", lambda e: e.copy(out=GT[:, i * 128:(i + 1) * 128], in_=pt2[0:NE, 0:128]), reads=[pr2], writes=[R_G])
                    b.dma("sp", gt_d[:, :], GT[:], reads=[R_G], writes=[R_gt])
                    for dc in range(8):
                        for tc in range(4):
                            pt, pr = bank()
                            b.op("pe", lambda e, dc=dc, tc=tc: e.matmul(pt[:, :], lhsT=bdn[:, dc * 128:(dc + 1) * 128],
                                                                        rhs=GT[:, tc * 512:(tc + 1) * 512], start=True, stop=True),
                                 reads=[R_b, R_G], writes=[pr])
                            b.op("act", lambda e, dc=dc, tc=tc: e.copy(out=acc[:, dc, tc * 512:(tc + 1) * 512], in_=pt[:, :]),
                                 reads=[pr], writes=[R_acc[dc][tc]])
                    b.barrier()
                with contextlib.ExitStack() as ps:
                    actT = b.sb(ps, [128, 8, 2048], BF16, "actT")
                    R_act = [[Res() for _ in range(4)] for _ in range(8)]
                    NST = 4
                    stg = [b.sb(ps, [128, 8, 256], F32, "stg%d" % i) for i in range(NST)]
                    wbf = [b.sb(ps, [128, 8, 256], BF16, "wbf%d" % i) for i in range(NST)]
                    R_stg = [Res() for _ in range(NST)]
                    R_wbf = [Res() for _ in range(NST)]
                    tt = [b.sb(ps, [128, 512], F32, "tt%d" % i) for i in range(2)]
                    sg = [b.sb(ps, [128, 512], F32, "sg%d" % i) for i in range(2)]
                    uu = [b.sb(ps, [128, 512], F32, "uu%d" % i) for i in range(2)]
                    R_tt = [Res(), Res()]
                    R_sg = [Res(), Res()]
                    R_uu = [Res(), Res()]
                    gbc = b.sb(ps, [128, 2048], F32, "gbc")
                    R_gbc = [Res() for _ in range(4)]
                    steps = []
                    for ex in range(NE):
                        wg = w_gu[l, ex].rearrange("(dc p) f -> p dc f", p=128)
                        wd = w_down[l, ex].rearrange("(fc p) d -> p fc d", p=128)
                        for q2 in range(4):
                            steps.append((ex, "gu", q2, [wg[:, :, q2 * 256:(q2 + 1) * 256], wg[:, :, DFF + q2 * 256:DFF + (q2 + 1) * 256]]))
                        for r2 in range(2):
                            steps.append((ex, "dn", r2, [wd[:, :, (2 * r2) * 256:(2 * r2 + 1) * 256], wd[:, :, (2 * r2 + 1) * 256:(2 * r2 + 2) * 256]]))
                    NS = len(steps)

                    def slots_of(i):
                        return (0, 1) if i % 2 == 0 else (2, 3)

                    def load_dma(i):
                        if i >= NS:
                            return
                        for s_, src in zip(slots_of(i), steps[i][3]):
                            b.dma("sp", stg[s_][:], src, writes=[R_stg[s_]])

                    def load_cast(i):
                        if i >= NS:
                            return
                        for s_ in slots_of(i):
                            b.op("act", lambda e: e.copy(out=wbf[s_][:], in_=stg[s_][:]), reads=[R_stg[s_]], writes=[R_wbf[s_]])

                    def load_gate(ex):
                        for tc in range(4):
                            b.dma("sp", gbc[:, tc * 512:(tc + 1) * 512], gt_d[ex, tc * 512:(tc + 1) * 512].partition_broadcast(128),
                                  reads=[R_gt], writes=[R_gbc[tc]])

                    cnt = [0]

                    def gu_unit(ex, q, tc, sA, sB, sub):
                        pa, pra = bank()
                        pb, prb = bank()
                        for dc in range(8):
                            b.op("pe", lambda e, dc=dc: e.matmul(pa[:, :], lhsT=wbf[sA][:, dc, sub * 128:(sub + 1) * 128],
                                                                 rhs=hT[:, dc, tc * 512:(tc + 1) * 512], start=(dc == 0), stop=(dc == 7)),
                                 reads=[R_wbf[sA], R_h], writes=[pra])
                        for dc in range(8):
                            b.op("pe", lambda e, dc=dc: e.matmul(pb[:, :], lhsT=wbf[sB][:, dc, sub * 128:(sub + 1) * 128],
                                                                 rhs=hT[:, dc, tc * 512:(tc + 1) * 512], start=(dc == 0), stop=(dc == 7)),
                                 reads=[R_wbf[sB], R_h], writes=[prb])
                        i2 = cnt[0] % 2
                        cnt[0] += 1
                        b.op("dve", lambda e: e.tensor_scalar(out=tt[i2][:], in0=pa[:, :], scalar1=bgc[:, q, ex:ex + 1], scalar2=7.0,
                                                              op0=ALU.add, op1=ALU.min),
                             reads=[pra, R_b], writes=[R_tt[i2]])
                        b.op("act", lambda e: e.activation(out=uu[i2][:], in_=pb[:, :], func=AF.Identity,
                                                           bias=bgc[:, 8 + q, ex:ex + 1], scale=1.0),
                             reads=[prb, R_b], writes=[R_uu[i2]])
                        b.op("act", lambda e: e.activation(out=sg[i2][:], in_=tt[i2][:], func=AF.Sigmoid, scale=1.702),
                             reads=[R_tt[i2]], writes=[R_sg[i2]])
                        b.op("pool", lambda e: e.tensor_scalar(out=uu[i2][:], in0=uu[i2][:], scalar1=8.0, scalar2=-6.0,
                                                               op0=ALU.min, op1=ALU.max),
                             reads=[R_uu[i2]], writes=[R_uu[i2]])
                        b.op("pool", lambda e: e.tensor_tensor(out=uu[i2][:], in0=uu[i2][:], in1=gbc[:, tc * 512:(tc + 1) * 512],
                                                               op=ALU.mult),
                             reads=[R_uu[i2], R_gbc[tc]], writes=[R_uu[i2]])
                        b.op("dve", lambda e: e.tensor_tensor(out=tt[i2][:], in0=tt[i2][:], in1=sg[i2][:], op=ALU.mult),
                             reads=[R_tt[i2], R_sg[i2]], writes=[R_tt[i2]])
                        b.op("dve", lambda e: e.tensor_tensor(out=actT[:, q, tc * 512:(tc + 1) * 512], in0=tt[i2][:], in1=uu[i2][:],
                                                              op=ALU.mult),
                             reads=[R_tt[i2], R_uu[i2]], writes=[R_act[q][tc]])

                    def dn_unit(s_, dc, ds, tc):
                        pt, pr = bank()
                        for fc in range(8):
                            b.op("pe", lambda e, fc=fc: e.matmul(pt[:, :], lhsT=wbf[s_][:, fc, ds * 128:(ds + 1) * 128],
                                                                 rhs=actT[:, fc, tc * 512:(tc + 1) * 512],
                                                                 start=(fc == 0), stop=(fc == 7)),
                                 reads=[R_wbf[s_], R_act[fc][tc]], writes=[pr])
                        b.op("dve", lambda e: e.tensor_tensor(out=acc[:, dc, tc * 512:(tc + 1) * 512], in0=pt[:, :],
                                                              in1=acc[:, dc, tc * 512:(tc + 1) * 512], op=ALU.add),
                             reads=[pr, R_acc[dc][tc]], writes=[R_acc[dc][tc]])

                    load_gate(0)
                    load_dma(0)
                    load_dma(1)
                    load_cast(0)
                    for i in range(NS):
                        ex, kind, idx, _ = steps[i]
                        load_dma(i + 2)
                        sl = slots_of(i)
                        if kind == "gu":
                            units = [(idx * 2 + sub, tc, sub) for sub in range(2) for tc in range(4)]
                            for ui, (q, tc, sub) in enumerate(units):
                                if ui == 4:
                                    load_cast(i + 1)
                                gu_unit(ex, q, tc, sl[0], sl[1], sub)
                        else:
                            if idx == 0 and ex + 1 < NE:
                                load_gate(ex + 1)
                            units = [(sl[hh], (idx * 2 + hh) * 2 + ds, ds, tc) for hh in range(2) for tc in range(4) for ds in range(2)]
                            for ui, (s_, dc, ds, tc) in enumerate(units):
                                if ui == 8:
                                    load_cast(i + 1)
                                dn_unit(s_, dc, ds, tc)
                    b.barrier()
                with contextlib.ExitStack() as ps:
                    xc = [b.sb(ps, [128, 8, 512], F32, "xr%d" % i) for i in range(2)]
                    R_xc = [Res(), Res()]
                    for tc in range(4):
                        s = tc % 2
                        t0 = T0 + tc * 512
                        b.dma("sp", xc[s][:], xT_v[:, :, t0:t0 + 512], reads=[R_x], writes=[R_xc[s]])
                        for dc in range(8):
                            b.op("dve", lambda e, dc=dc: e.scalar_tensor_tensor(out=xc[s][:, dc, :], in0=acc[:, dc, tc * 512:(tc + 1) * 512],
                                                                                scalar=modc[:, l, 5, dc:dc + 1], in1=xc[s][:, dc, :],
                                                                                op0=ALU.mult, op1=ALU.add),
                                 reads=[R_xc[s]], writes=[R_xc[s]])
                        b.dma("sp", xT_v[:, :, t0:t0 + 512], xc[s][:], reads=[R_xc[s]], writes=[R_x])
                    b.barrier()

    maskb = b.sb(es, [128, 4, 128], BF16, "maskb")
    invc = b.sb(es, [128, 1], F32, "invc")
    pic = b.sb(es, [128, 1], F32, "pic")
    with contextlib.ExitStack() as ps:
        mkf = b.sb(ps, [128, 4, 128], F32, "mkf")
        posi = b.sb(ps, [128, S], I32, "posi")
        posf = b.sb(ps, [128, S], F32, "posf")
        ang = b.sb(ps, [128, S], F32, "ang")
        R_t = Res()
        b.dma("sp", mkf[:], k_masks[:, :, :], writes=[R_t])
        b.dma("sp", invc[:], k_inv[:, :], writes=[R_t])
        b.dma("sp", posi[:], pos_in.partition_broadcast(128), writes=[R_t])
        b.op("pool", lambda e: e.memset(pic[:], float(np.pi / 2)), writes=[R_t])
        b.op("dve", lambda e: e.tensor_copy(out=maskb[:], in_=mkf[:]), reads=[R_t], writes=[R_t])
        b.op("dve", lambda e: e.tensor_copy(out=posf[:], in_=posi[:]), reads=[R_t], writes=[R_t])
        b.op("dve", lambda e: e.tensor_scalar(out=posf[:], in0=posf[:], scalar1=invc[:, 0:1], scalar2=None, op0=ALU.mult),
             reads=[R_t], writes=[R_t])
        C1 = 6.28125
        C2 = float(np.float32(2 * np.pi - 6.28125))
        C3 = float(2 * np.pi - 6.28125 - np.float64(np.float32(2 * np.pi - 6.28125)))
        kf = posi[:, :].bitcast(F32)
        b.op("dve", lambda e: e.tensor_scalar(out=ang[:], in0=posf[:], scalar1=float(1 / (2 * np.pi)), scalar2=None, op0=ALU.mult),
             reads=[R_t], writes=[R_t])
        b.op("dve", lambda e: e.tensor_copy(out=posi[:], in_=ang[:]), reads=[R_t], writes=[R_t])
        b.op("dve", lambda e: e.tensor_copy(out=ang[:], in_=posi[:]), reads=[R_t], writes=[R_t])
        for cc in (C1, C2, C3):
            b.op("dve", lambda e: e.scalar_tensor_tensor(out=posf[:], in0=ang[:], scalar=-cc, in1=posf[:], op0=ALU.mult, op1=ALU.add),
                 reads=[R_t], writes=[R_t])
        b.op("dve", lambda e: e.tensor_scalar(out=ang[:], in0=posf[:], scalar1=float(np.pi), scalar2=float(-2 * np.pi), op0=ALU.is_gt, op1=ALU.mult),
             reads=[R_t], writes=[R_t])
        b.op("dve", lambda e: e.tensor_tensor(out=posf[:], in0=posf[:], in1=ang[:], op=ALU.add), reads=[R_t], writes=[R_t])
        b.op("dve", lambda e: e.tensor_scalar(out=ang[:], in0=posf[:], scalar1=float(-np.pi), scalar2=float(2 * np.pi), op0=ALU.is_lt, op1=ALU.mult),
             reads=[R_t], writes=[R_t])
        b.op("dve", lambda e: e.tensor_tensor(out=posf[:], in0=posf[:], in1=ang[:], op=ALU.add), reads=[R_t], writes=[R_t])
        b.op("dve", lambda e: e.tensor_scalar(out=posf[:], in0=posf[:], scalar1=3.14159, scalar2=-3.14159, op0=ALU.min, op1=ALU.max),
             reads=[R_t], writes=[R_t])
        b.op("dve", lambda e: e.scalar_tensor_tensor(out=ang[:], in0=posf[:], scalar=-1.0, in1=posf[:], op0=ALU.mult, op1=ALU.max),
             reads=[R_t], writes=[R_t])
        b.op("act", lambda e: e.activation(out=ang[:], in_=ang[:], func=AF.Sin, bias=pic[:], scale=-1.0), reads=[R_t], writes=[R_t])
        b.op("act", lambda e: e.activation(out=posf[:], in_=posf[:], func=AF.Sin), reads=[R_t], writes=[R_t])
        b.dma("sp", tab_d[0], ang[:], reads=[R_t], writes=[R_tab])
        b.dma("sp", tab_d[1], posf[:], reads=[R_t], writes=[R_tab])
        b.barrier()

    def fbank(lst, st):
        st[0] = (st[0] + 1) % len(lst)
        return banks[lst[st[0]]]

    def rot_weights(dst, src, nblk, R_w):
        sv = src.rearrange("p (k h i) -> p k h i", h=2, i=32)
        dv = dst.rearrange("p (k h i) -> p k h i", h=2, i=32)
        b.op("dve", lambda e: e.tensor_scalar(out=dv[:, :, 0, :], in0=sv[:, :, 1, :], scalar1=-1.0, scalar2=None, op0=ALU.mult),
             reads=[R_w], writes=[R_w])
        b.op("dve", lambda e: e.tensor_copy(out=dv[:, :, 1, :], in_=sv[:, :, 0, :]), reads=[R_w], writes=[R_w])

    def rope_evac(out_ap, pa, pb_, C_ap, S_ap, t1, t2, R_tmp, reads, writes, npart=128, perm=None):
        b.op("dve", lambda e: e.tensor_tensor(out=t1[0:npart, :], in0=pa, in1=C_ap, op=ALU.mult), reads=reads, writes=[R_tmp])
        b.op("dve", lambda e: e.tensor_tensor(out=t2[0:npart, :], in0=pb_, in1=S_ap, op=ALU.mult), reads=reads, writes=[R_tmp])
        a1 = t1[0:npart, :]
        a2 = t2[0:npart, :]
        if perm is not None:
            a1 = a1.rearrange("p (m r) -> p r m", r=perm)
            a2 = a2.rearrange("p (m r) -> p r m", r=perm)
        b.op("pool", lambda e: e.tensor_tensor(out=out_ap, in0=a1, in1=a2, op=ALU.add), reads=[R_tmp], writes=writes)

    def load_tables(ps):
        Ct = b.sb(ps, [128, S], F32, "Ct")
        St = b.sb(ps, [128, S], F32, "St")
        R_T = Res()
        b.dma("sp", Ct[:], tab_d[0], reads=[R_tab], writes=[R_T])
        b.dma("sp", St[:], tab_d[1], reads=[R_tab], writes=[R_T])
        return Ct, St, R_T

    def norm_all(l, which, hT, R_h):
        with contextlib.ExitStack() as ps:
            nb = norm_bufs(ps)
            for tc in range(8):
                norm_chunk(nb, l, which, tc * 512, hT, tc * 512, R_h)
            b.barrier()

    def out_proj(l, w_out_l, nfc):
        with contextlib.ExitStack() as ps:
            wo = b.sb(ps, [128, nfc, D], BF16, "wo")
            R_wo = Res()
            ot = [b.sb(ps, [128, nfc, 512], BF16, "ot%d" % i) for i in range(2)]
            xc = [b.sb(ps, [128, 8, 512], F32, "ox%d" % i) for i in range(2)]
            R_ot = [Res(), Res()]
            R_xc = [Res(), Res()]
            wv_ = w_out_l.rearrange("(fc p) d -> p fc d", p=128)
            for i in range(nfc // 4):
                b.dma("pq", wo[:, i * 4:(i + 1) * 4, :], wv_[:, i * 4:(i + 1) * 4, :], writes=[R_wo])
            for tc in range(8):
                s = tc % 2
                t0 = tc * 512
                b.dma("sp", ot[s][:], oT_v[:, 0:nfc, t0:t0 + 512], reads=[R_o], writes=[R_ot[s]])
                b.dma("sp", xc[s][:], xT_v[:, :, t0:t0 + 512], reads=[R_x], writes=[R_xc[s]])
                for dc in range(8):
                    pt, pr = bank()
                    for fc in range(nfc):
                        b.op("pe", lambda e, fc=fc: e.matmul(pt[:, :], lhsT=wo[:, fc, dc * 128:(dc + 1) * 128], rhs=ot[s][:, fc, :],
                                                             start=(fc == 0), stop=(fc == nfc - 1)),
                             reads=[R_wo, R_ot[s]], writes=[pr])
                    b.op("dve", lambda e: e.scalar_tensor_tensor(out=xc[s][:, dc, :], in0=pt[:, :], scalar=modc[:, l, 2, dc:dc + 1],
                                                                 in1=xc[s][:, dc, :], op0=ALU.mult, op1=ALU.add),
                         reads=[pr, R_xc[s]], writes=[R_xc[s]])
                b.dma("sp", xT_v[:, :, t0:t0 + 512], xc[s][:], reads=[R_xc[s]], writes=[R_x])
            b.barrier()

    mla_d = dscr("mla_d", [6, 128, S], BF16)
    mla_v = mla_d.rearrange("c p t -> p c t")
    R_mla = Res()

    def mixer_even(l):
        li = l // 2
        win = w_in_ab[li].rearrange("(dc p) f -> p dc f", p=128)
        with contextlib.ExitStack() as hs:
            hT = b.sb(hs, [128, 8, S], BF16, "hTe")
            R_h = Res()
            norm_all(l, 0, hT, R_h)
            with contextlib.ExitStack() as ps:
                Ct, St, R_T = load_tables(ps)
                wA = b.sb(ps, [128, 8, 704], BF16, "wA")
                wKr = b.sb(ps, [128, 8, 64], BF16, "wKr")
                R_w = Res()
                gq = b.sb(ps, [128, 3], F32, "gq")
                gkv = b.sb(ps, [128, 2], F32, "gkv")
                b.dma("pq", wA[:], win[:, :, 0:704], writes=[R_w])
                b.dma("sp", gq[:], col_view(mla_g_q[li], 3), writes=[R_w], allow_slow_non_contiguous=True)
                b.dma("sp", gkv[:], col_view(mla_g_kv[li], 2), writes=[R_w], allow_slow_non_contiguous=True)
                for dc in range(8):
                    rot_weights(wKr[:, dc, :], wA[:, dc, 640:704], 1, R_w)
                sqb = [b.sb(ps, [128, 3, 512], BF16, "sqb%d" % i) for i in range(2)]
                R_sqb = [Res(), Res()]
                rsq = [b.sb(ps, [128, 512], F32, "rsq%d" % i) for i in range(2)]
                R_rsq = [Res(), Res()]
                ob = [b.sb(ps, [128, 6, 512], BF16, "mob%d" % i) for i in range(2)]
                R_ob = [Res(), Res()]
                t1 = b.sb(ps, [128, 512], F32, "t1")
                t2 = b.sb(ps, [128, 512], F32, "t2")
                R_tmp = Res()
                kk = 0
                for tc in range(8):
                    t0 = tc * 512
                    so = tc % 2
                    for (c0, nch, gcolv, oc0) in ((0, 3, gq, 0), (384, 2, gkv, 3)):
                        s2 = kk % 2
                        kk += 1
                        pcs = []
                        for c in range(nch):
                            pt, pr = bank()
                            for dc in range(8):
                                b.op("pe", lambda e, dc=dc: e.matmul(pt[:, :], lhsT=wA[:, dc, c0 + c * 128:c0 + (c + 1) * 128], rhs=hT[:, dc, t0:t0 + 512],
                                                                     start=(dc == 0), stop=(dc == 7)),
                                     reads=[R_w, R_h], writes=[pr])
                            b.op("act", lambda e: e.activation(out=sqb[s2][:, c, :], in_=pt[:, :], func=AF.Square), reads=[pr], writes=[R_sqb[s2]])
                            pcs.append((pt, pr))
                        pss, prs = bank()
                        for c in range(nch):
                            b.op("pe", lambda e: e.matmul(pss[:, :], lhsT=onesb[:], rhs=sqb[s2][:, c, :], start=(c == 0), stop=(c == nch - 1)),
                                 reads=[R_sqb[s2]], writes=[prs])
                        b.op("act", lambda e: e.activation(out=rsq[s2][:], in_=pss[:, :], func=AF.Sqrt, bias=epsc[:], scale=1.0 / (nch * 128)),
                             reads=[prs], writes=[R_rsq[s2]])
                        b.op("dve", lambda e: e.reciprocal(out=rsq[s2][:], in_=rsq[s2][:]), reads=[R_rsq[s2]], writes=[R_rsq[s2]])
                        for c in range(nch):
                            pt, pr = pcs[c]
                            b.op("dve", lambda e: e.scalar_tensor_tensor(out=ob[so][:, oc0 + c, :], in0=pt[:, :], scalar=gcolv[:, c:c + 1],
                                                                         in1=rsq[s2][:], op0=ALU.mult, op1=ALU.mult),
                                 reads=[pr, R_rsq[s2], R_w], writes=[R_ob[so]])
                    pa, pra = bank()
                    pb_, prb = bank()
                    for dc in range(8):
                        b.op("pe", lambda e, dc=dc: e.matmul(pa[0:64, :], lhsT=wA[:, dc, 640:704], rhs=hT[:, dc, t0:t0 + 512],
                                                             start=(dc == 0), stop=(dc == 7)), reads=[R_w, R_h], writes=[pra])
                    for dc in range(8):
                        b.op("pe", lambda e, dc=dc: e.matmul(pb_[0:64, :], lhsT=wKr[:, dc, :], rhs=hT[:, dc, t0:t0 + 512],
                                                             start=(dc == 0), stop=(dc == 7)), reads=[R_w, R_h], writes=[prb])
                    rope_evac(ob[so][0:64, 5, :], pa[0:64, :], pb_[0:64, :], Ct[0:64, t0:t0 + 512], St[0:64, t0:t0 + 512], t1, t2, R_tmp,
                              [pra, prb, R_T], [R_ob[so]], npart=64)
                    b.dma("sp", mla_v[:, 0:5, t0:t0 + 512], ob[so][:, 0:5, :], reads=[R_ob[so]], writes=[R_mla])
                    b.dma("sp", mla_v[0:64, 5, t0:t0 + 512], ob[so][0:64, 5, :], reads=[R_ob[so]], writes=[R_mla])
                b.barrier()
            with contextlib.ExitStack() as ps:
                Ct, St, R_T = load_tables(ps)
                kT = b.sb(ps, [128, 2, S], BF16, "kTb")
                vd = b.sb(ps, [128, 2, 32, 128], BF16, "vdb")
                R_k = Res()
                R_v = Res()
                wk = b.sb(ps, [128, 8, 256], BF16, "wkd")
                wkr = b.sb(ps, [128, 8, 256], BF16, "wkdr")
                wv = b.sb(ps, [128, 8, 256], BF16, "wvd")
                R_w = Res()
                es_ = b.sb(ps, [128, 16], F32, "esink")
                b.dma("sp", es_[:], swa_sink[li].partition_broadcast(128), writes=[R_w])
                b.op("act", lambda e: e.activation(out=es_[:], in_=es_[:], func=AF.Exp), reads=[R_w], writes=[R_w])
                for g in range(2):
                    for dup in range(2):
                        o0 = g * 128 + dup * 64
                        b.dma("pq", wk[:, :, o0:o0 + 64], win[:, :, 1728 + g * 64:1728 + (g + 1) * 64], writes=[R_w])
                        b.dma("pq", wv[:, :, o0:o0 + 64], win[:, :, 1856 + g * 64:1856 + (g + 1) * 64], writes=[R_w])
                for dc in range(8):
                    rot_weights(wkr[:, dc, :], wk[:, dc, :], 4, R_w)
                t1 = b.sb(ps, [128, 512], F32, "t1")
                t2 = b.sb(ps, [128, 512], F32, "t2")
                R_tmp = Res()
                for tc in range(8):
                    t0 = tc * 512
                    for g in range(2):
                        pa, pra = bank()
                        pb_, prb = bank()
                        for dc in range(8):
                            b.op("pe", lambda e, dc=dc: e.matmul(pa[:, :], lhsT=wk[:, dc, g * 128:(g + 1) * 128], rhs=hT[:, dc, t0:t0 + 512],
                                                                 start=(dc == 0), stop=(dc == 7)), reads=[R_w, R_h], writes=[pra])
                        for dc in range(8):
                            b.op("pe", lambda e, dc=dc: e.matmul(pb_[:, :], lhsT=wkr[:, dc, g * 128:(g + 1) * 128], rhs=hT[:, dc, t0:t0 + 512],
                                                                 start=(dc == 0), stop=(dc == 7)), reads=[R_w, R_h], writes=[prb])
                        rope_evac(kT[:, g, t0:t0 + 512], pa[:, :], pb_[:, :], Ct[:, t0:t0 + 512], St[:, t0:t0 + 512], t1, t2, R_tmp,
                                  [pra, prb, R_T], [R_k])
                for i in range(32):
                    pt, pr = bank()
                    for dc in range(8):
                        b.op("pe", lambda e, dc=dc: e.matmul(pt[:, 0:256], lhsT=hT[:, dc, i * 128:(i + 1) * 128], rhs=wv[:, dc, :],
                                                             start=(dc == 0), stop=(dc == 7)), reads=[R_w, R_h], writes=[pr])
                    b.op("act", lambda e: e.copy(out=vd[:, :, i, :], in_=pt[:, 0:256].rearrange("p (g f) -> p g f", g=2)),
                         reads=[pr], writes=[R_v])
                wq = [b.sb(ps, [128, 8, 128], BF16, "wq%d" % i) for i in range(2)]
                wqr = [b.sb(ps, [128, 8, 128], BF16, "wqr%d" % i) for i in range(2)]
                R_wq = [Res(), Res()]
                qT = [b.sb(ps, [128, S], BF16, "qTb%d" % i) for i in range(2)]
                R_q = [Res(), Res()]
                oTj = [b.sb(ps, [128, S], BF16, "oTb%d" % i) for i in range(2)]
                R_oj = [Res(), Res()]
                Pb = [b.sb(ps, [128, 384], BF16, "Pb%d" % i) for i in range(3)]
                R_P = [Res() for _ in range(3)]
                dn = [b.sb(ps, [128, 128], F32, "dn%d" % i) for i in range(2)]
                R_dn = [Res(), Res()]
                pks = [0, 0]
                for j in range(8):
                    s = j % 2
                    g = j // 4
                    b.dma("pq", wq[s][:], win[:, :, 704 + j * 128:704 + (j + 1) * 128], writes=[R_wq[s]])
                    for dc in range(8):
                        rot_weights(wqr[s][:, dc, :], wq[s][:, dc, :], 2, R_wq[s])
                    for tc in range(8):
                        t0 = tc * 512
                        pa, pra = bank()
                        pb_, prb = bank()
                        for dc in range(8):
                            b.op("pe", lambda e, dc=dc: e.matmul(pa[:, :], lhsT=wq[s][:, dc, :], rhs=hT[:, dc, t0:t0 + 512],
                                                                 start=(dc == 0), stop=(dc == 7)), reads=[R_wq[s], R_h], writes=[pra])
                        for dc in range(8):
                            b.op("pe", lambda e, dc=dc: e.matmul(pb_[:, :], lhsT=wqr[s][:, dc, :], rhs=hT[:, dc, t0:t0 + 512],
                                                                 start=(dc == 0), stop=(dc == 7)), reads=[R_wq[s], R_h], writes=[prb])
                        rope_evac(qT[s][:, t0:t0 + 512], pa[:, :], pb_[:, :], Ct[:, t0:t0 + 512], St[:, t0:t0 + 512], t1, t2, R_tmp,
                                  [pra, prb, R_T], [R_q[s]])
                    for hh in range(2):
                        h = 2 * j + hh
                        p0 = hh * 64
                        p1 = p0 + 64
                        def swaA(qb):
                            kbs = [kb for kb in (qb - 1, qb, qb + 1) if 0 <= kb < 32]
                            n = len(kbs)
                            pS, prS = bank()
                            for idx, kb in enumerate(kbs):
                                b.op("pe", lambda e: e.matmul(pS[:, idx * 128:(idx + 1) * 128], lhsT=kT[p0:p1, g, kb * 128:(kb + 1) * 128],
                                                              rhs=qT[s][p0:p1, qb * 128:(qb + 1) * 128], start=True, stop=(kb == qb)),
                                     reads=[R_k, R_q[s]], writes=[prS])
                                if kb != qb:
                                    m = 0 if kb < qb else 1
                                    b.op("pe", lambda e: e.matmul(pS[:, idx * 128:(idx + 1) * 128], lhsT=identb[:], rhs=maskb[:, m, :],
                                                                  start=False, stop=True), writes=[prS])
                            pi = pks[0] % 3
                            pks[0] += 1
                            b.op("act", lambda e: e.activation(out=Pb[pi][:, 0:n * 128], in_=pS[:, 0:n * 128], func=AF.Exp, scale=0.125),
                                 reads=[prS], writes=[R_P[pi]])
                            return (qb, kbs, pi)

                        def swaB(st_):
                            qb, kbs, pi = st_
                            n = len(kbs)
                            po, pro = bank()
                            pd, prd = bank()
                            for idx, kb in enumerate(kbs):
                                b.op("pe", lambda e: e.matmul(po[:, 0:128], lhsT=vd[:, g, kb, :], rhs=Pb[pi][:, idx * 128:(idx + 1) * 128],
                                                              start=(idx == 0), stop=(idx == n - 1)), reads=[R_v, R_P[pi]], writes=[pro])
                            for idx, kb in enumerate(kbs):
                                b.op("pe", lambda e: e.matmul(pd[:, 0:128], lhsT=onesb[:], rhs=Pb[pi][:, idx * 128:(idx + 1) * 128],
                                                              start=(idx == 0), stop=(idx == n - 1)), reads=[R_P[pi]], writes=[prd])
                            di = pks[1] % 2
                            pks[1] += 1
                            b.op("dve", lambda e: e.tensor_scalar(out=dn[di][p0:p1, :], in0=pd[p0:p1, 0:128], scalar1=es_[p0:p1, h:h + 1], scalar2=None,
                                                                  op0=ALU.add), reads=[prd, R_w], writes=[R_dn[di]])
                            b.op("dve", lambda e: e.reciprocal(out=dn[di][p0:p1, :], in_=dn[di][p0:p1, :]), reads=[R_dn[di]], writes=[R_dn[di]])
                            b.op("dve", lambda e: e.tensor_tensor(out=oTj[s][p0:p1, qb * 128:(qb + 1) * 128], in0=po[p0:p1, 0:128],
                                                                  in1=dn[di][p0:p1, :], op=ALU.mult), reads=[pro, R_dn[di]], writes=[R_oj[s]])

                        nxt = swaA(0)
                        for qb in range(32):
                            cur = nxt
                            if qb + 1 < 32:
                                nxt = swaA(qb + 1)
                            swaB(cur)
                    b.dma("sp", oT_v[:, 8 + j, :], oTj[s][:], reads=[R_oj[s]], writes=[R_o])
                b.barrier()
        with contextlib.ExitStack() as ps:
            Ct, St, R_T = load_tables(ps)
            cqn = b.sb(ps, [128, 3, S], BF16, "cqn")
            ckvn = b.sb(ps, [128, 2, S], BF16, "ckvn")
            krT = b.sb(ps, [128, S], BF16, "krT")
            R_c = Res()
            b.op("pool", lambda e: e.memset(krT[64:128, :], 0.0), writes=[R_c])
            b.dma("sp", cqn[:], mla_v[:, 0:3, :], reads=[R_mla], writes=[R_c])
            b.dma("sp", ckvn[:], mla_v[:, 3:5, :], reads=[R_mla], writes=[R_c])
            b.dma("sp", krT[0:64, :], mla_v[0:64, 5, :], reads=[R_mla], writes=[R_c])
            wqh = [b.sb(ps, [128, 3, 192], BF16, "wqh%d" % i) for i in range(2)]
            wqhr = [b.sb(ps, [128, 3, 64], BF16, "wqhr%d" % i) for i in range(2)]
            wkvh = [b.sb(ps, [128, 2, 256], BF16, "wkvh%d" % i) for i in range(2)]
            R_wh = [Res(), Res()]
            qn = [b.sb(ps, [128, S], BF16, "qn%d" % i) for i in range(2)]
            qr = [b.sb(ps, [128, S], BF16, "qr%d" % i) for i in range(2)]
            kn = [b.sb(ps, [128, S], BF16, "kn%d" % i) for i in range(2)]
            vv = [b.sb(ps, [128, 32, 128], BF16, "vv%d" % i) for i in range(2)]
            R_hd = [Res(), Res()]
            for i in range(2):
                b.op("pool", lambda e: e.memset(qr[i][64:128, :], 0.0), writes=[R_hd[i]])
            oTh = [b.sb(ps, [128, S], BF16, "oTh%d" % i) for i in range(2)]
            R_oh = [Res(), Res()]
            Pm = [b.sb(ps, [128, 512], BF16, "Pm%d" % i) for i in range(4)]
            R_Pm = [Res() for _ in range(4)]
            Pacc = [[b.sb(ps, [128, 512], F32, "Pacc%d%d" % (i, k_)) for k_ in range(2)] for i in range(2)]
            R_Pacc = [[Res(), Res()], [Res(), Res()]]
            dn = [b.sb(ps, [128, 512], F32, "dnm%d" % i) for i in range(2)]
            R_dn = [Res(), Res()]
            t1 = b.sb(ps, [128, 512], F32, "t1")
            t2 = b.sb(ps, [128, 512], F32, "t2")
            R_tmp = Res()
            wqb_v = mla_w_qb[li].rearrange("(c p) f -> p c f", p=128)
            wkvb_v = mla_w_kvb[li].rearrange("(c p) f -> p c f", p=128)
            SC = float(192.0 ** -0.5)
            pkm = [0]
            acc_st = [0]
            s_st = [0]
            for h in range(8):
                s = h % 2
                b.dma("pq", wqh[s][:], wqb_v[:, :, h * 192:(h + 1) * 192], writes=[R_wh[s]])
                b.dma("pq", wkvh[s][:], wkvb_v[:, :, h * 256:(h + 1) * 256], writes=[R_wh[s]])
                for c in range(3):
                    rot_weights(wqhr[s][:, c, :], wqh[s][:, c, 128:192], 1, R_wh[s])
                for tc in range(8):
                    t0 = tc * 512
                    pt, pr = fbank([4, 5], s_st)
                    for c in range(3):
                        b.op("pe", lambda e: e.matmul(pt[:, :], lhsT=wqh[s][:, c, 0:128], rhs=cqn[:, c, t0:t0 + 512], start=(c == 0), stop=(c == 2)),
                             reads=[R_wh[s], R_c], writes=[pr])
                    b.op("act", lambda e: e.copy(out=qn[s][:, t0:t0 + 512], in_=pt[:, :]), reads=[pr], writes=[R_hd[s]])
                    pt, pr = fbank([4, 5], s_st)
                    for c in range(2):
                        b.op("pe", lambda e: e.matmul(pt[:, :], lhsT=wkvh[s][:, c, 0:128], rhs=ckvn[:, c, t0:t0 + 512], start=(c == 0), stop=(c == 1)),
                             reads=[R_wh[s], R_c], writes=[pr])
                    b.op("act", lambda e: e.copy(out=kn[s][:, t0:t0 + 512], in_=pt[:, :]), reads=[pr], writes=[R_hd[s]])
                    pa, pra = fbank([4, 5], s_st)
                    pb_, prb = fbank([4, 5], s_st)
                    for c in range(3):
                        b.op("pe", lambda e: e.matmul(pa[0:64, :], lhsT=wqh[s][:, c, 128:192], rhs=cqn[:, c, t0:t0 + 512], start=(c == 0), stop=(c == 2)),
                             reads=[R_wh[s], R_c], writes=[pra])
                    for c in range(3):
                        b.op("pe", lambda e: e.matmul(pb_[0:64, :], lhsT=wqhr[s][:, c, :], rhs=cqn[:, c, t0:t0 + 512], start=(c == 0), stop=(c == 2)),
                             reads=[R_wh[s], R_c], writes=[prb])
                    rope_evac(qr[s][0:64, t0:t0 + 512], pa[0:64, :], pb_[0:64, :], Ct[0:64, t0:t0 + 512], St[0:64, t0:t0 + 512], t1, t2, R_tmp,
                              [pra, prb, R_T], [R_hd[s]], npart=64)
                for i in range(32):
                    pt, pr = fbank([4, 5], s_st)
                    for c in range(2):
                        b.op("pe", lambda e: e.matmul(pt[:, 0:128], lhsT=ckvn[:, c, i * 128:(i + 1) * 128], rhs=wkvh[s][:, c, 128:256],
                                                      start=(c == 0), stop=(c == 1)), reads=[R_wh[s], R_c], writes=[pr])
                    b.op("act", lambda e: e.copy(out=vv[s][:, i, :], in_=pt[:, 0:128]), reads=[pr], writes=[R_hd[s]])
                for qc in range(8):
                    q0 = qc * 512
                    a = acc_st[0] % 2
                    acc_st[0] += 1
                    po, pro = banks[0 + 2 * a]
                    pd, prd = banks[1 + 2 * a]
                    def emitS(kt):
                        pS, prS = fbank([4, 5], s_st)
                        b.op("pe", lambda e: e.matmul(pS[:, :], lhsT=kn[s][:, kt * 128:(kt + 1) * 128], rhs=qn[s][:, q0:q0 + 512], start=True, stop=False),
                             reads=[R_hd[s]], writes=[prS])
                        b.op("pe", lambda e: e.matmul(pS[:, :], lhsT=krT[:, kt * 128:(kt + 1) * 128], rhs=qr[s][:, q0:q0 + 512], start=False, stop=True),
                             reads=[R_hd[s], R_c], writes=[prS])
                        pi = pkm[0] % 4
                        pkm[0] += 1
                        b.op("act", lambda e: e.activation(out=Pm[pi][:], in_=pS[:, :], func=AF.Exp, scale=SC), reads=[prS], writes=[R_Pm[pi]])
                        return pi

                    nxt = emitS(0)
                    for kt in range(32):
                        pi = nxt
                        if kt + 1 < 32:
                            nxt = emitS(kt + 1)
                        b.op("pe", lambda e: e.matmul(po[:, :], lhsT=vv[s][:, kt, :], rhs=Pm[pi][:], start=(kt == 0), stop=(kt == 31)),
                             reads=[R_hd[s], R_Pm[pi]], writes=[pro])
                        w_ = 1 if (kt % 3 == 2) else 0
                        eng_ = "pool" if w_ else "dve"
                        if kt == 0 or kt == 2:
                            b.op(eng_, lambda e: e.tensor_copy(out=Pacc[a][w_][:], in_=Pm[pi][:]), reads=[R_Pm[pi]], writes=[R_Pacc[a][w_]])
                        else:
                            b.op(eng_, lambda e: e.tensor_tensor(out=Pacc[a][w_][:], in0=Pacc[a][w_][:], in1=Pm[pi][:], op=ALU.add),
                                 reads=[R_Pm[pi], R_Pacc[a][w_]], writes=[R_Pacc[a][w_]])
                    for w_ in range(2):
                        b.op("pe", lambda e: e.matmul(pd[:, :], lhsT=meanm[:], rhs=Pacc[a][w_][:], start=(w_ == 0), stop=(w_ == 1)),
                             reads=[R_Pacc[a][w_]], writes=[prd])
                    b.op("dve", lambda e: e.reciprocal(out=dn[a][:], in_=pd[:, :]), reads=[prd], writes=[R_dn[a]])
                    b.op("dve", lambda e: e.scalar_tensor_tensor(out=oTh[s][:, q0:q0 + 512], in0=po[:, :], scalar=1.0 / D, in1=dn[a][:],
                                                                 op0=ALU.mult, op1=ALU.mult),
                         reads=[pro, R_dn[a]], writes=[R_oh[s]])
                b.dma("sp", oT_v[:, h, :], oTh[s][:], reads=[R_oh[s]], writes=[R_o])
            b.barrier()
        out_proj(l, w_out_ab[li], 16)

    def mixer_odd(l):
        li = l // 2
        win = w_in_c[li].rearrange("(dc p) f -> p dc f", p=128)
        with contextlib.ExitStack() as hs:
            hT = b.sb(hs, [128, 8, S], BF16, "hTo")
            R_h = Res()
            norm_all(l, 0, hT, R_h)
            Ct, St, R_T = load_tables(hs)
            num = b.sb(hs, [128, S], F32, "num")
            den = b.sb(hs, [128, S], F32, "den")
            R_nd = Res()
            oTj = b.sb(hs, [128, S], BF16, "oTc")
            R_oj = Res()
            t1 = b.sb(hs, [128, 512], F32, "t1")
            t2 = b.sb(hs, [128, 512], F32, "t2")
            R_tmp = Res()
            wts = [b.sb(hs, [128, 8, 128], BF16, "wc%d" % i) for i in range(5)]
            R_w = Res()
            KPMAX = S + 128 * 16
            qP = b.sb(hs, [128, S], BF16, "qP")
            kP = b.sb(hs, [128, KPMAX], BF16, "kP")
            vP = b.sb(hs, [128, KPMAX], BF16, "vP")
            Vt = b.sb(hs, [128, KPMAX // 128, 128], BF16, "Vt")
            R_q = Res()
            R_k = Res()
            R_vp = Res()
            R_vt = Res()
            Pb = [b.sb(hs, [128, 256], BF16, "Pc%d" % i) for i in range(3)]
            R_P = [Res() for _ in range(3)]
            pkd = [0]
            def load_w(j_, g_):
                base = g_ * 3072 + j_ * 128
                b.dma("pq", wts[0][:], win[:, :, base:base + 128], writes=[R_w])
                b.dma("pq", wts[2][:], win[:, :, base + 1024:base + 1152], writes=[R_w])
                b.dma("pq", wts[4][:], win[:, :, base + 2048:base + 2176], writes=[R_w])
                for dc in range(8):
                    rot_weights(wts[1][:, dc, :], wts[0][:, dc, :], 2, R_w)
                    rot_weights(wts[3][:, dc, :], wts[2][:, dc, :], 2, R_w)

            load_w(0, 0)
            for j in range(8):
                for g, d in enumerate((1, 4, 16)):
                    L = S // d
                    Lp = L + 128
                    nt = L // 128 + 1
                    nq = L // 128
                    qv = qP[:, :].rearrange("p (r m) -> p r m", r=d)
                    kv = kP[:, 0:d * Lp].rearrange("p (r m) -> p r m", r=d)
                    vv_ = vP[:, 0:d * Lp].rearrange("p (r m) -> p r m", r=d)
                    b.op("pool", lambda e: e.memset(kv[:, :, 0:64], 0.0), writes=[R_k])
                    b.op("pool", lambda e: e.memset(kv[:, :, 64 + L:Lp], 0.0), writes=[R_k])
                    b.op("pool", lambda e: e.memset(vv_[:, :, 0:64], 0.0), writes=[R_vp])
                    b.op("pool", lambda e: e.memset(vv_[:, :, 64 + L:Lp], 0.0), writes=[R_vp])
                    for tc in range(8):
                        t0 = tc * 512
                        m0 = t0 // d
                        mw = 512 // d
                        prs = []
                        for wi in range(5):
                            pt, pr = bank()
                            for dc in range(8):
                                b.op("pe", lambda e, dc=dc: e.matmul(pt[:, :], lhsT=wts[wi][:, dc, :], rhs=hT[:, dc, t0:t0 + 512],
                                                                     start=(dc == 0), stop=(dc == 7)), reads=[R_w, R_h], writes=[pr])
                            prs.append((pt, pr))
                            if wi == 1:
                                rope_evac(qv[:, :, m0:m0 + mw], prs[0][0][:, :], prs[1][0][:, :], Ct[:, t0:t0 + 512], St[:, t0:t0 + 512],
                                          t1, t2, R_tmp, [prs[0][1], prs[1][1], R_T], [R_q], perm=d)
                            if wi == 3:
                                rope_evac(kv[:, :, 64 + m0:64 + m0 + mw], prs[2][0][:, :], prs[3][0][:, :], Ct[:, t0:t0 + 512], St[:, t0:t0 + 512],
                                          t1, t2, R_tmp, [prs[2][1], prs[3][1], R_T], [R_k], perm=d)
                            if wi == 4:
                                b.op("act", lambda e: e.copy(out=vv_[:, :, 64 + m0:64 + m0 + mw],
                                                             in_=pt[:, :].rearrange("p (m r) -> p r m", r=d)), reads=[pr], writes=[R_vp])
                    if not (j == 7 and g == 2):
                        load_w(j + (g + 1) // 3, (g + 1) % 3)
                    ntile = d * nt
                    for i0 in range(0, ntile, 8):
                        nn = min(8, ntile - i0)
                        pbt, pbr = bbank()
                        for ii in range(nn):
                            i = i0 + ii
                            r_, jt = divmod(i, nt)
                            c0 = r_ * Lp + jt * 128
                            b.op("pe", lambda e: e.transpose(out=pbt[:, ii * 128:(ii + 1) * 128], in_=vP[:, c0:c0 + 128], identity=identb[:]),
                                 reads=[R_vp], writes=[pbr])
                        b.op("act", lambda e: e.copy(out=Vt[:, i0:i0 + nn, :], in_=pbt[:, 0:nn * 128].rearrange("p (i f) -> p i f", f=128)),
                             reads=[pbr], writes=[R_vt])
                    for hh in range(2):
                        p0 = hh * 64
                        p1 = p0 + 64
                        def dilA(r_, n):
                            q0 = r_ * L + n * 128
                            k0 = r_ * Lp + n * 128
                            pS, prS = bank()
                            for idx in range(2):
                                if idx == 0:
                                    m = 2 if n == 0 else 0
                                else:
                                    m = 3 if n == nq - 1 else 1
                                b.op("pe", lambda e: e.matmul(pS[:, idx * 128:(idx + 1) * 128], lhsT=kP[p0:p1, k0 + idx * 128:k0 + (idx + 1) * 128],
                                                              rhs=qP[p0:p1, q0:q0 + 128], start=True, stop=False),
                                     reads=[R_k, R_q], writes=[prS])
                                b.op("pe", lambda e: e.matmul(pS[:, idx * 128:(idx + 1) * 128], lhsT=identb[:], rhs=maskb[:, m, :],
                                                              start=False, stop=True), writes=[prS])
                            pi = pkd[0] % 3
                            pkd[0] += 1
                            b.op("act", lambda e: e.activation(out=Pb[pi][:], in_=pS[:, 0:256], func=AF.Exp, scale=0.125),
                                 reads=[prS], writes=[R_P[pi]])
                            return (r_, n, pi)

                        def dilB(st_):
                            r_, n, pi = st_
                            po, pro = bank()
                            pd, prd = bank()
                            ti = r_ * nt + n
                            for idx in range(2):
                                b.op("pe", lambda e: e.matmul(po[:, 0:128], lhsT=Vt[:, ti + idx, :], rhs=Pb[pi][:, idx * 128:(idx + 1) * 128],
                                                              start=(idx == 0), stop=(idx == 1)), reads=[R_vt, R_P[pi]], writes=[pro])
                            for idx in range(2):
                                b.op("pe", lambda e: e.matmul(pd[:, 0:128], lhsT=onesb[:], rhs=Pb[pi][:, idx * 128:(idx + 1) * 128],
                                                              start=(idx == 0), stop=(idx == 1)), reads=[R_P[pi]], writes=[prd])
                            nv = num[p0:p1, :].rearrange("p (m r) -> p r m", r=d)[:, r_, n * 128:(n + 1) * 128]
                            dv = den[p0:p1, :].rearrange("p (m r) -> p r m", r=d)[:, r_, n * 128:(n + 1) * 128]
                            if g == 0:
                                b.op("act", lambda e: e.copy(out=nv, in_=po[p0:p1, 0:128]), reads=[pro], writes=[R_nd])
                                b.op("dve", lambda e: e.tensor_copy(out=dv, in_=pd[p0:p1, 0:128]), reads=[prd], writes=[R_nd])
                            else:
                                b.op("dve", lambda e: e.tensor_tensor(out=nv, in0=po[p0:p1, 0:128], in1=nv, op=ALU.add), reads=[pro, R_nd], writes=[R_nd])
                                b.op("dve", lambda e: e.tensor_tensor(out=dv, in0=pd[p0:p1, 0:128], in1=dv, op=ALU.add), reads=[prd, R_nd], writes=[R_nd])

                        ulist = [(r_, n) for r_ in range(d) for n in range(nq)]
                        nxt = dilA(*ulist[0])
                        for ui in range(len(ulist)):
                            cur = nxt
                            if ui + 1 < len(ulist):
                                nxt = dilA(*ulist[ui + 1])
                            dilB(cur)
                for q4 in range(8):
                    sl = slice(q4 * 512, (q4 + 1) * 512)
                    b.op("dve", lambda e: e.reciprocal(out=den[:, sl], in_=den[:, sl]), reads=[R_nd], writes=[R_nd])
                    b.op("dve", lambda e: e.tensor_tensor(out=oTj[:, sl], in0=num[:, sl], in1=den[:, sl], op=ALU.mult), reads=[R_nd], writes=[R_oj])
                b.dma("sp", oT_v[:, j, :], oTj[:], reads=[R_oj], writes=[R_o])
            b.barrier()
        out_proj(l, w_out_c[li], 8)

    for l in layers:
        if cfg["mixer"]:
            if l % 2 == 0:
                mixer_even(l)
            else:
                mixer_odd(l)
        if cfg["ffn"]:
            moe_layer(l)

    with contextlib.ExitStack() as ps:
        xc = [b.sb(ps, [128, 8, 512], F32, "fx%d" % i) for i in range(2)]
        sq = b.sb(ps, [128, 8, 512], F32, "fsq")
        rs = b.sb(ps, [128, 512], F32, "frs")
        yo = [b.sb(ps, [128, D], F32, "fy%d" % i) for i in range(2)]
        R_xc = [Res(), Res()]
        R_sq = Res()
        R_rs = Res()
        R_yo = [Res(), Res()]
        k = 0
        for tc in range(8):
            s = tc % 2
            t0 = tc * 512
            b.dma("sp", xc[s][:], xT_v[:, :, t0:t0 + 512], reads=[R_x], writes=[R_xc[s]])
            b.op("act", lambda e: e.activation(out=sq[:], in_=xc[s][:], func=AF.Square), reads=[R_xc[s]], writes=[R_sq])
            pt, pr = bank()
            for dc in range(8):
                b.op("pe", lambda e, dc=dc: e.matmul(pt[:, :], lhsT=meanm[:], rhs=sq[:, dc, :], start=(dc == 0), stop=(dc == 7)),
                     reads=[R_sq], writes=[pr])
            b.op("act", lambda e: e.activation(out=rs[:], in_=pt[:, :], func=AF.Sqrt, bias=epsc[:], scale=1.0),
                 reads=[pr], writes=[R_rs])
            b.op("dve", lambda e: e.reciprocal(out=rs[:], in_=rs[:]), reads=[R_rs], writes=[R_rs])
            for dc in range(8):
                b.op("dve", lambda e, dc=dc: e.scalar_tensor_tensor(out=xc[s][:, dc, :], in0=xc[s][:, dc, :], scalar=gfin[:, dc:dc + 1],
                                                                    in1=rs[:], op0=ALU.mult, op1=ALU.mult),
                     reads=[R_rs, R_xc[s]], writes=[R_xc[s]])
            for i in range(4):
                ys = k % 2
                k += 1
                for hb in range(2):
                    pt, pr = bank()
                    for cc in range(4):
                        dc = hb * 4 + cc
                        b.op("pe", lambda e, dc=dc, cc=cc: e.transpose(out=pt[:, cc * 128:(cc + 1) * 128], in_=xc[s][:, dc, i * 128:(i + 1) * 128],
                                                                       identity=identf[:]),
                             reads=[R_xc[s]], writes=[pr])
                    if hb == 0:
                        b.op("dve", lambda e: e.tensor_copy(out=yo[ys][:, 0:512], in_=pt[:, :]), reads=[pr], writes=[R_yo[ys]])
                    else:
                        b.op("act", lambda e: e.copy(out=yo[ys][:, 512:1024], in_=pt[:, :]), reads=[pr], writes=[R_yo[ys]])
                r0 = t0 + i * 128
                b.dma("sp", y_out[r0:r0 + 128, :], yo[ys][:], reads=[R_yo[ys]], writes=[R_o])
        b.barrier()
    return nc


def make_consts():
    identf = np.eye(128, dtype=np.float32)
    i = np.arange(128) % 32
    inv = (10000.0 ** (-(2.0 * i.astype(np.float32)) / 64.0)).astype(np.float32).reshape(128, 1)
    a = np.arange(128)[:, None]
    bq = np.arange(128)[None, :]
    NEG = -30000.0
    masks = np.zeros((128, 4, 128), dtype=np.float32)
    masks[:, 0, :] = np.where(a >= bq, 0.0, NEG)
    masks[:, 1, :] = np.where(a <= bq, 0.0, NEG)
    masks[:, 2, :] = np.where((a >= bq) & (a >= 64), 0.0, NEG)
    masks[:, 3, :] = np.where((a <= bq) & (a < 64), 0.0, NEG)
    return {"k_identf": identf, "k_inv": inv, "k_masks": masks}


_CACHE = {}


def kernel(**inputs):
    cfg = CFG
    ncores = cfg["ncores"]
    key = repr(cfg)
    if key not in _CACHE:
        _CACHE[key] = build(cfg)
    nc = _CACHE[key]
    consts = make_consts()
    shared = {}
    for k_, v in inputs.items():
        if k_ in ("x", "c", "positions"):
            continue
        shared[k_] = np.ascontiguousarray(v)
    in_maps = []
    for cidx in range(ncores):
        m = dict(shared)
        m.update(consts)
        m["x"] = np.ascontiguousarray(inputs["x"][cidx])
        m["c"] = np.ascontiguousarray(inputs["c"][cidx])
        m["positions"] = np.ascontiguousarray(inputs["positions"][cidx]).astype(np.int32)
        in_maps.append(m)
    res = run_bass_kernel_spmd(nc, in_maps, core_ids=list(range(ncores)))
    out = np.stack([np.asarray(r["y"]) for r in res.results], axis=0)
    if ncores < 8:
        full = np.zeros((8, S, D), dtype=np.float32)
        full[:ncores] = out
        out = full
    return out.astype(np.float32)
```

```python
import contextlib
import numpy as np
import ml_dtypes
import concourse.bass as bass
import concourse.mybir as mybir
from concourse.bass_utils import run_bass_kernel_spmd

F32 = mybir.dt.float32
BF16 = mybir.dt.bfloat16
I32 = mybir.dt.int32
AF = mybir.ActivationFunctionType
ALU = mybir.AluOpType
AX = mybir.AxisListType

S = 4096
D = 1024
DEPTH = 4
NE = 32
DFF = 1024
EPS = 1e-6
NQ = 8
STRICT = True

CFG = {"layers": [0, 1, 2, 3], "mixer": True, "ffn": True, "ncores": 8}


class Res:
    __slots__ = ("w", "r")

    def __init__(self):
        self.w = None
        self.r = {}


class Builder:
    def __init__(self):
        nc = self.nc = bass.Bass("TRN2", target_bir_lowering=False)
        self.es = contextlib.ExitStack()
        self.streams = {"pe": nc.tensor, "act": nc.scalar, "dve": nc.vector, "pool": nc.gpsimd, "sp": nc.sync}
        self.csem = {}
        self.ccount = {}
        for e in ("pe", "act", "dve", "pool"):
            self.csem[e] = self.es.enter_context(nc.semaphore("c_" + e))
            self.ccount[e] = 0
        self.qsems = {}
        self.qcount = {}
        self.qissuer = {"sp": "sp", "pq": "pool"}
        for q in ("sp", "pq"):
            self.qsems[q] = [self.es.enter_context(nc.semaphore("q_%s%d" % (q, j))) for j in range(NQ)]
            self.qcount[q] = 0
        self.waited = {s: {} for s in self.streams}
        self.uid = 0

    def name(self, p):
        self.uid += 1
        return "%s_%d" % (p, self.uid)

    def sb(self, ctx, shape, dt, nm="t"):
        return ctx.enter_context(self.nc.sbuf_tensor(self.name(nm), list(shape), dt))

    def _wait(self, stream, tok):
        sem, val, owner = tok
        if owner == stream and (stream == "pe" or not STRICT):
            return
        w = self.waited[stream]
        if w.get(id(sem), 0) >= val:
            return
        self.streams[stream].wait_ge(sem, val)
        w[id(sem)] = val

    def _deps(self, stream, reads, writes):
        for r in reads:
            if r.w is not None:
                self._wait(stream, r.w)
        for r in writes:
            if r.w is not None:
                self._wait(stream, r.w)
            for t in r.r.values():
                self._wait(stream, t)

    def _mark(self, tok, reads, writes):
        for r in reads:
            r.r[id(tok[0])] = tok
        for r in writes:
            r.w = tok
            r.r = {}

    def op(self, eng, fn, reads=(), writes=()):
        self._deps(eng, reads, writes)
        ins = fn(self.streams[eng])
        self.ccount[eng] += 1
        tok = (self.csem[eng], self.ccount[eng], eng)
        ins.then_inc(tok[0], 1)
        self._mark(tok, reads, writes)

    def dma(self, q, out, in_, reads=(), writes=(), **kw):
        issuer = self.qissuer[q]
        n = self.qcount[q]
        sem = self.qsems[q][n % NQ]
        if n >= NQ:
            self._wait(issuer, (sem, 16 * (n // NQ), None))
        self._deps(issuer, reads, writes)
        eng = self.nc.sync if q == "sp" else self.nc.gpsimd
        ins = eng.dma_start(out=out, in_=in_, **kw)
        ins.then_inc(sem, 16)
        self.qcount[q] += 1
        tok = (sem, 16 * (n // NQ + 1), None)
        self._mark(tok, reads, writes)

    def barrier(self):
        toks = []
        for e in self.csem:
            if self.ccount[e] > 0:
                toks.append((self.csem[e], self.ccount[e], e))
        for q in self.qsems:
            n = self.qcount[q]
            for j in range(NQ):
                cnt = (n - j + NQ - 1) // NQ if n > j else 0
                if cnt > 0:
                    toks.append((self.qsems[q][j], 16 * cnt, None))
        for s in self.streams:
            for t in toks:
                self._wait(s, t)


def build(cfg):
    b = Builder()
    nc = b.nc
    es = b.es
    layers = cfg["layers"]

    def din(name, shape, dt=F32):
        return nc.dram_tensor(name, list(shape), dt, kind="ExternalInput").ap()

    def dscr(name, shape, dt=F32):
        return nc.dram_tensor(name, list(shape), dt, kind="Internal").ap()

    x_in = din("x", [S, D])
    c_in = din("c", [D])
    pos_in = din("positions", [S], I32)
    w_mod = din("w_mod", [DEPTH, D, 6 * D])
    b_mod = din("b_mod", [DEPTH, 6 * D])
    g_mix = din("g_norm_mix", [DEPTH, D])
    g_ffn = din("g_norm_ffn", [DEPTH, D])
    w_in_ab = din("w_in_ab", [2, D, 1984])
    mla_g_q = din("mla_g_q", [2, 384])
    mla_w_qb = din("mla_w_qb", [2, 384, 1536])
    mla_g_kv = din("mla_g_kv", [2, 256])
    mla_w_kvb = din("mla_w_kvb", [2, 256, 2048])
    swa_sink = din("swa_sink", [2, 16])
    w_out_ab = din("w_out_ab", [2, 2048, D])
    w_in_c = din("w_in_c", [2, D, 9216])
    w_out_c = din("w_out_c", [2, 1024, D])
    w_router = din("w_router", [DEPTH, D, NE])
    b_router = din("b_router", [DEPTH, NE])
    w_gu = din("w_gu", [DEPTH, NE, D, 2 * DFF])
    b_gu = din("b_gu", [DEPTH, NE, 2 * DFF])
    w_down = din("w_down", [DEPTH, NE, DFF, D])
    b_down = din("b_down", [DEPTH, NE, D])
    g_final = din("g_final", [D])
    k_identf = din("k_identf", [128, 128])
    k_inv = din("k_inv", [128, 1])
    k_masks = din("k_masks", [128, 4, 128])
    y_out = nc.dram_tensor("y", [S, D], F32, kind="ExternalOutput").ap()

    xT_d = dscr("xT_d", [8, 128, S])
    oT_d = dscr("oT_d", [16, 128, S], BF16)
    tab_d = dscr("tab_d", [2, 128, S])
    gt_d = dscr("gt_d", [NE, 2048])
    xT_v = xT_d.rearrange("c p t -> p c t")
    oT_v = oT_d.rearrange("c p t -> p c t")
    R_x = Res()
    R_o = Res()
    R_tab = Res()

    identf = b.sb(es, [128, 128], F32, "identf")
    identb = b.sb(es, [128, 128], BF16, "identb")
    meanm = b.sb(es, [128, 128], F32, "meanm")
    onesb = b.sb(es, [128, 128], BF16, "onesb")
    modc = b.sb(es, [128, DEPTH, 6, 8], F32, "modc")
    gcol = b.sb(es, [128, DEPTH, 2, 8], F32, "gcol")
    gfin = b.sb(es, [128, 8], F32, "gfin")
    epsc = b.sb(es, [128, 1], F32, "epsc")
    R_const = Res()

    banks = []
    for i in range(6):
        t = es.enter_context(nc.psum_tensor(b.name("psf"), [128, 512], F32))
        banks.append((t, Res()))
    bbanks = []
    for i in range(2):
        t = es.enter_context(nc.psum_tensor(b.name("psb"), [128, 1024], BF16))
        bbanks.append((t, Res()))
    rr = {"f": 0, "b": 0}

    def bank():
        rr["f"] = (rr["f"] + 1) % 6
        return banks[rr["f"]]

    def bbank():
        rr["b"] = (rr["b"] + 1) % 2
        return bbanks[rr["b"]]

    def col_view(vec_ap, n):
        return vec_ap.rearrange("(j p) -> p j", p=128)

    b.dma("sp", identf[:], k_identf[:, :], writes=[R_const])
    b.op("dve", lambda e: e.tensor_copy(out=identb[:], in_=identf[:]), reads=[R_const], writes=[R_const])
    b.op("pool", lambda e: e.memset(meanm[:], 1.0 / D), writes=[R_const])
    b.op("pool", lambda e: e.memset(onesb[:], 1.0), writes=[R_const])
    b.op("pool", lambda e: e.memset(epsc[:], EPS), writes=[R_const])
    b.dma("sp", gfin[:], col_view(g_final, 8), writes=[R_const], allow_slow_non_contiguous=True)
    for l in layers:
        b.dma("sp", gcol[:, l, 0, :], col_view(g_mix[l], 8), writes=[R_const], allow_slow_non_contiguous=True)
        b.dma("sp", gcol[:, l, 1, :], col_view(g_ffn[l], 8), writes=[R_const], allow_slow_non_contiguous=True)

    with contextlib.ExitStack() as ps:
        cT = b.sb(ps, [128, 8], F32, "cT")
        cS = b.sb(ps, [128, 8], F32, "cS")
        bmc = b.sb(ps, [128, DEPTH, 48], F32, "bmc")
        wm = [b.sb(ps, [128, 8, 1024], F32, "wm%d" % i) for i in range(2)]
        R_wm = [Res(), Res()]
        R_c = Res()
        b.dma("sp", cT[:], col_view(c_in, 8), writes=[R_c], allow_slow_non_contiguous=True)
        for l in layers:
            b.dma("sp", bmc[:, l, :], col_view(b_mod[l], 48), writes=[R_c], allow_slow_non_contiguous=True)
        b.op("act", lambda e: e.activation(out=cS[:], in_=cT[:], func=AF.Sigmoid), reads=[R_c], writes=[R_c])
        b.op("dve", lambda e: e.tensor_tensor(out=cS[:], in0=cS[:], in1=cT[:], op=ALU.mult), reads=[R_c], writes=[R_c])
        k = 0
        for l in layers:
            for v in range(6):
                slot = k % 2
                k += 1
                b.dma("sp", wm[slot][:], w_mod[l].rearrange("(dc p) f -> p dc f", p=128)[:, :, v * 1024:(v + 1) * 1024],
                      writes=[R_wm[slot]])
                pt, pr = bank()
                for j in range(8):
                    for dc in range(8):
                        b.op("pe", lambda e, j=j, dc=dc: e.matmul(pt[:, j:j + 1], lhsT=wm[slot][:, dc, j * 128:(j + 1) * 128],
                                                                   rhs=cS[:, dc:dc + 1], start=(dc == 0), stop=(dc == 7)),
                             reads=[R_wm[slot], R_c], writes=[pr])
                b.op("dve", lambda e: e.tensor_tensor(out=modc[:, l, v, :], in0=pt[:, 0:8], in1=bmc[:, l, v * 8:(v + 1) * 8], op=ALU.add),
                     reads=[pr, R_c], writes=[R_const])
        for l in layers:
            for (v, gi) in ((1, 0), (4, 1)):
                b.op("dve", lambda e, l=l, v=v, gi=gi: e.scalar_tensor_tensor(out=modc[:, l, v, :], in0=modc[:, l, v, :], scalar=1.0,
                                                                              in1=gcol[:, l, gi, :], op0=ALU.add, op1=ALU.mult),
                     reads=[R_const], writes=[R_const])
        b.barrier()

    with contextlib.ExitStack() as ps:
        xt = [b.sb(ps, [128, D], F32, "xt%d" % i) for i in range(2)]
        xo = [b.sb(ps, [128, 8, 128], F32, "xo%d" % i) for i in range(2)]
        R_xt = [Res(), Res()]
        R_xo = [Res(), Res()]
        for i in range(32):
            s = i % 2
            b.dma("sp", xt[s][:], x_in[i * 128:(i + 1) * 128, :], writes=[R_xt[s]])
            for hb in range(2):
                pt, pr = bank()
                for cc in range(4):
                    c = hb * 4 + cc
                    b.op("pe", lambda e, c=c, cc=cc: e.transpose(out=pt[:, cc * 128:(cc + 1) * 128], in_=xt[s][:, c * 128:(c + 1) * 128],
                                                                 identity=identf[:]),
                         reads=[R_xt[s]], writes=[pr])
                eng = "dve" if hb == 0 else "act"
                if eng == "dve":
                    b.op("dve", lambda e: e.tensor_copy(out=xo[s][:, hb * 4:(hb + 1) * 4, :],
                                                        in_=pt[:, :].rearrange("p (c t) -> p c t", c=4)),
                         reads=[pr], writes=[R_xo[s]])
                else:
                    b.op("act", lambda e: e.copy(out=xo[s][:, hb * 4:(hb + 1) * 4, :],
                                                 in_=pt[:, :].rearrange("p (c t) -> p c t", c=4)),
                         reads=[pr], writes=[R_xo[s]])
            b.dma("sp", xT_v[:, :, i * 128:(i + 1) * 128], xo[s][:], reads=[R_xo[s]], writes=[R_x])
        b.barrier()

    def norm_chunk(ctx_bufs, l, which, t0, hT, hcol0, R_h):
        xc, R_xc, sq, R_sq, rs, R_rs, tmp, R_tmp = ctx_bufs[(t0 // 512) % 2]
        va = 1 if which == 0 else 4
        vb = 0 if which == 0 else 3
        b.dma("sp", xc[:], xT_v[:, :, t0:t0 + 512], reads=[R_x], writes=[R_xc])
        b.op("act", lambda e: e.activation(out=sq[:], in_=xc[:], func=AF.Square), reads=[R_xc], writes=[R_sq])
        pt, pr = bank()
        for dc in range(8):
            b.op("pe", lambda e, dc=dc: e.matmul(pt[:, :], lhsT=meanm[:], rhs=sq[:, dc, :], start=(dc == 0), stop=(dc == 7)),
                 reads=[R_sq], writes=[pr])
        b.op("act", lambda e: e.activation(out=rs[:], in_=pt[:, :], func=AF.Sqrt, bias=epsc[:], scale=1.0),
             reads=[pr], writes=[R_rs])
        b.op("dve", lambda e: e.reciprocal(out=rs[:], in_=rs[:]), reads=[R_rs], writes=[R_rs])
        for dc in range(8):
            b.op("dve", lambda e, dc=dc: e.scalar_tensor_tensor(out=tmp[:, dc, :], in0=xc[:, dc, :], scalar=modc[:, l, va, dc:dc + 1],
                                                                in1=rs[:], op0=ALU.mult, op1=ALU.mult),
                 reads=[R_xc, R_rs], writes=[R_tmp])
            b.op("act", lambda e, dc=dc: e.activation(out=hT[:, dc, hcol0:hcol0 + 512], in_=tmp[:, dc, :], func=AF.Identity,
                                                      bias=modc[:, l, vb, dc:dc + 1], scale=1.0),
                 reads=[R_tmp], writes=[R_h])

    def norm_bufs(ps):
        sets = []
        for i in range(2):
            xc = b.sb(ps, [128, 8, 512], F32, "xc%d" % i)
            sq = b.sb(ps, [128, 8, 512], F32, "sq%d" % i)
            rs = b.sb(ps, [128, 512], F32, "rs%d" % i)
            R_sq = Res()
            sets.append((xc, Res(), sq, R_sq, rs, Res(), sq, R_sq))
        return sets

    def moe_layer(l):
        for hf in range(2):
            T0 = hf * 2048
            with contextlib.ExitStack() as hs:
                hT = b.sb(hs, [128, 8, 2048], BF16, "hT")
                acc = b.sb(hs, [128, 8, 2048], F32, "acc")
                R_h = Res()
                R_G = Res()
                R_acc = [[Res() for _ in range(4)] for _ in range(8)]
                bgc = b.sb(hs, [128, 16, NE], F32, "bgc")
                R_b = Res()
                R_gt = Res()
                with contextlib.ExitStack() as ps:
                    nb = norm_bufs(ps)
                    wrf = b.sb(ps, [128, 8, NE], F32, "wrf")
                    wrb = b.sb(ps, [128, 8, NE], BF16, "wrb")
                    brb = b.sb(ps, [128, NE], F32, "brb")
                    R_wr = Res()
                    lg2 = [b.sb(ps, [128, NE], F32, "lg%d" % i_) for i_ in range(2)]
                    t82 = [b.sb(ps, [128, 8], F32, "t8%d" % i_) for i_ in range(2)]
                    ng2 = [b.sb(ps, [128, 1], F32, "ng%d" % i_) for i_ in range(2)]
                    ee2 = [b.sb(ps, [128, NE], F32, "ee%d" % i_) for i_ in range(2)]
                    mk2 = [b.sb(ps, [128, NE], F32, "mk%d" % i_) for i_ in range(2)]
                    sm2 = [b.sb(ps, [128, 1], F32, "sm%d" % i_) for i_ in range(2)]
                    R_r2 = [Res(), Res()]
                    bgr = b.sb(ps, [32, 2048], F32, "bgr")
                    GT = b.sb(ps, [32, 2048], F32, "GT")
                    bdn = b.sb(ps, [32, D], F32, "bdn")
                    b.dma("sp", bgr[:], b_gu[l], writes=[R_b])
                    b.dma("sp", bdn[:], b_down[l], writes=[R_b])
                    pt, pr = bank()
                    for c in range(16):
                        b.op("pe", lambda e, c=c: e.transpose(out=pt[:, c * NE:(c + 1) * NE], in_=bgr[:, c * 128:(c + 1) * 128],
                                                              identity=identf[0:32, 0:32]),
                             reads=[R_b], writes=[pr])
                    b.op("dve", lambda e: e.tensor_copy(out=bgc[:], in_=pt[:, :].rearrange("p (c e) -> p c e", c=16)),
                         reads=[pr], writes=[R_b])
                    b.op("dve", lambda e: e.tensor_scalar(out=bgc[:, 8:16, :], in0=bgc[:, 8:16, :], scalar1=1.0, scalar2=None, op0=ALU.add),
                         reads=[R_b], writes=[R_b])
                    b.dma("sp", wrf[:], w_router[l].rearrange("(dc p) e -> p dc e", p=128), writes=[R_wr])
                    b.dma("sp", brb[:], b_router[l].partition_broadcast(128), writes=[R_wr])
                    b.op("dve", lambda e: e.tensor_copy(out=wrb[:], in_=wrf[:]), reads=[R_wr], writes=[R_wr])
                    for tc in range(4):
                        norm_chunk(nb, l, 1, T0 + tc * 512, hT, tc * 512, R_h)
                    for i in range(16):
                        lg, t8, ng, ee, mk, sm, R_r = lg2[i % 2], t82[i % 2], ng2[i % 2], ee2[i % 2], mk2[i % 2], sm2[i % 2], R_r2[i % 2]
                        pt, pr = bank()
                        for dc in range(8):
                            b.op("pe", lambda e, dc=dc: e.matmul(pt[:, 0:NE], lhsT=hT[:, dc, i * 128:(i + 1) * 128], rhs=wrb[:, dc, :],
                                                                 start=(dc == 0), stop=(dc == 7)),
                                 reads=[R_h, R_wr], writes=[pr])
                        b.op("dve", lambda e: e.tensor_tensor(out=lg[:], in0=pt[:, 0:NE], in1=brb[:], op=ALU.add),
                             reads=[pr, R_wr], writes=[R_r])
                        b.op("dve", lambda e: e.max(out=t8[:], in_=lg[:]), reads=[R_r], writes=[R_r])
                        b.op("dve", lambda e: e.tensor_scalar(out=ng[:], in0=t8[:, 0:1], scalar1=-1.0, scalar2=None, op0=ALU.mult),
                             reads=[R_r], writes=[R_r])
                        b.op("act", lambda e: e.activation(out=ee[:], in_=lg[:], func=AF.Exp, bias=ng[:], scale=1.0),
                             reads=[R_r], writes=[R_r])
                        b.op("dve", lambda e: e.tensor_scalar(out=mk[:], in0=lg[:], scalar1=t8[:, 3:4], scalar2=None, op0=ALU.is_ge),
                             reads=[R_r], writes=[R_r])
                        b.op("dve", lambda e: e.tensor_tensor(out=ee[:], in0=ee[:], in1=mk[:], op=ALU.mult), reads=[R_r], writes=[R_r])
                        b.op("dve", lambda e: e.reduce_sum(out=sm[:], in_=ee[:], axis=AX.X), reads=[R_r], writes=[R_r])
                        b.op("dve", lambda e: e.reciprocal(out=sm[:], in_=sm[:]), reads=[R_r], writes=[R_r])
                        b.op("dve", lambda e: e.tensor_scalar(out=ee[:], in0=ee[:], scalar1=sm[:, 0:1], scalar2=None, op0=ALU.mult),
                             reads=[R_r], writes=[R_r])
                        pt2, pr2 = bank()
                        b.op("pe", lambda e: e.transpose(out=pt2[0:NE, 0:128], in_=ee[:], identity=identf[:]), reads=[R_r], writes=[pr2])
                        b.op("act", lambda e: e.copy(out=GT[:, i * 128:(i + 1) * 128], in_=pt2[0:NE, 0:128]), reads=[pr2], writes=[R_G])
                    b.dma("sp", gt_d[:, :], GT[:], reads=[R_G], writes=[R_gt])
                    for dc in range(8):
                        for tc in range(4):
                            pt, pr = bank()
                            b.op("pe", lambda e, dc=dc, tc=tc: e.matmul(pt[:, :], lhsT=bdn[:, dc * 128:(dc + 1) * 128],
                                                                        rhs=GT[:, tc * 512:(tc + 1) * 512], start=True, stop=True),
                                 reads=[R_b, R_G], writes=[pr])
                            b.op("act", lambda e, dc=dc, tc=tc: e.copy(out=acc[:, dc, tc * 512:(tc + 1) * 512], in_=pt[:, :]),
                                 reads=[pr], writes=[R_acc[dc][tc]])
                    b.barrier()
                with contextlib.ExitStack() as ps:
                    actT = b.sb(ps, [128, 8, 2048], BF16, "actT")
                    R_act = [[Res() for _ in range(4)] for _ in range(8)]
                    NST = 4
                    stg = [b.sb(ps, [128, 8, 256], F32, "stg%d" % i) for i in range(NST)]
                    wbf = [b.sb(ps, [128, 8, 256], BF16, "wbf%d" % i) for i in range(NST)]
                    R_stg = [Res() for _ in range(NST)]
                    R_wbf = [Res() for _ in range(NST)]
                    tt = [b.sb(ps, [128, 512], F32, "tt%d" % i) for i in range(2)]
                    sg = [b.sb(ps, [128, 512], F32, "sg%d" % i) for i in range(2)]
                    uu = [b.sb(ps, [128, 512], F32, "uu%d" % i) for i in range(2)]
                    R_tt = [Res(), Res()]
                    R_sg = [Res(), Res()]
                    R_uu = [Res(), Res()]
                    gbc = b.sb(ps, [128, 2048], F32, "gbc")
                    R_gbc = [Res() for _ in range(4)]
                    steps = []
                    for ex in range(NE):
                        wg = w_gu[l, ex].rearrange("(dc p) f -> p dc f", p=128)
                        wd = w_down[l, ex].rearrange("(fc p) d -> p fc d", p=128)
                        for q2 in range(4):
                            steps.append((ex, "gu", q2, [wg[:, :, q2 * 256:(q2 + 1) * 256], wg[:, :, DFF + q2 * 256:DFF + (q2 + 1) * 256]]))
                        for r2 in range(2):
                            steps.append((ex, "dn", r2, [wd[:, :, (2 * r2) * 256:(2 * r2 + 1) * 256], wd[:, :, (2 * r2 + 1) * 256:(2 * r2 + 2) * 256]]))
                    NS = len(steps)

                    def slots_of(i):
                        return (0, 1) if i % 2 == 0 else (2, 3)

                    def load_dma(i):
                        if i >= NS:
                            return
                        for s_, src in zip(slots_of(i), steps[i][3]):
                            b.dma("sp", stg[s_][:], src, writes=[R_stg[s_]])

                    def load_cast(i):
                        if i >= NS:
                            return
                        for s_ in slots_of(i):
                            b.op("act", lambda e: e.copy(out=wbf[s_][:], in_=stg[s_][:]), reads=[R_stg[s_]], writes=[R_wbf[s_]])

                    def load_gate(ex):
                        for tc in range(4):
                            b.dma("sp", gbc[:, tc * 512:(tc + 1) * 512], gt_d[ex, tc * 512:(tc + 1) * 512].partition_broadcast(128),
                                  reads=[R_gt], writes=[R_gbc[tc]])

                    cnt = [0]

                    def gu_unit(ex, q, tc, sA, sB, sub):
                        pa, pra = bank()
                        pb, prb = bank()
                        for dc in range(8):
                            b.op("pe", lambda e, dc=dc: e.matmul(pa[:, :], lhsT=wbf[sA][:, dc, sub * 128:(sub + 1) * 128],
                                                                 rhs=hT[:, dc, tc * 512:(tc + 1) * 512], start=(dc == 0), stop=(dc == 7)),
                                 reads=[R_wbf[sA], R_h], writes=[pra])
                        for dc in range(8):
                            b.op("pe", lambda e, dc=dc: e.matmul(pb[:, :], lhsT=wbf[sB][:, dc, sub * 128:(sub + 1) * 128],
                                                                 rhs=hT[:, dc, tc * 512:(tc + 1) * 512], start=(dc == 0), stop=(dc == 7)),
                                 reads=[R_wbf[sB], R_h], writes=[prb])
                        i2 = cnt[0] % 2
                        cnt[0] += 1
                        b.op("dve", lambda e: e.tensor_scalar(out=tt[i2][:], in0=pa[:, :], scalar1=bgc[:, q, ex:ex + 1], scalar2=7.0,
                                                              op0=ALU.add, op1=ALU.min),
                             reads=[pra, R_b], writes=[R_tt[i2]])
                        b.op("act", lambda e: e.activation(out=uu[i2][:], in_=pb[:, :], func=AF.Identity,
                                                           bias=bgc[:, 8 + q, ex:ex + 1], scale=1.0),
                             reads=[prb, R_b], writes=[R_uu[i2]])
                        b.op("act", lambda e: e.activation(out=sg[i2][:], in_=tt[i2][:], func=AF.Sigmoid, scale=1.702),
                             reads=[R_tt[i2]], writes=[R_sg[i2]])
                        b.op("pool", lambda e: e.tensor_scalar(out=uu[i2][:], in0=uu[i2][:], scalar1=8.0, scalar2=-6.0,
                                                               op0=ALU.min, op1=ALU.max),
                             reads=[R_uu[i2]], writes=[R_uu[i2]])
                        b.op("pool", lambda e: e.tensor_tensor(out=uu[i2][:], in0=uu[i2][:], in1=gbc[:, tc * 512:(tc + 1) * 512],
                                                               op=ALU.mult),
                             reads=[R_uu[i2], R_gbc[tc]], writes=[R_uu[i2]])
                        b.op("dve", lambda e: e.tensor_tensor(out=tt[i2][:], in0=tt[i2][:], in1=sg[i2][:], op=ALU.mult),
                             reads=[R_tt[i2], R_sg[i2]], writes=[R_tt[i2]])
                        b.op("dve", lambda e: e.tensor_tensor(out=actT[:, q, tc * 512:(tc + 1) * 512], in0=tt[i2][:], in1=uu[i2][:],
                                                              op=ALU.mult),
                             reads=[R_tt[i2], R_uu[i2]], writes=[R_act[q][tc]])

                    def dn_unit(s_, dc, ds, tc):
                        pt, pr = bank()
                        for fc in range(8):
                            b.op("pe", lambda e, fc=fc: e.matmul(pt[:, :], lhsT=wbf[s_][:, fc, ds * 128:(ds + 1) * 128],
                                                                 rhs=actT[:, fc, tc * 512:(tc + 1) * 512],
                                                                 start=(fc == 0), stop=(fc == 7)),
                                 reads=[R_wbf[s_], R_act[fc][tc]], writes=[pr])
                        b.op("dve", lambda e: e.tensor_tensor(out=acc[:, dc, tc * 512:(tc + 1) * 512], in0=pt[:, :],
                                                              in1=acc[:, dc, tc * 512:(tc + 1) * 512], op=ALU.add),
                             reads=[pr, R_acc[dc][tc]], writes=[R_acc[dc][tc]])

                    load_gate(0)
                    load_dma(0)
                    load_dma(1)
                    load_cast(0)
                    for i in range(NS):
                        ex, kind, idx, _ = steps[i]
                        load_dma(i + 2)
                        sl = slots_of(i)
                        if kind == "gu":
                            units = [(idx * 2 + sub, tc, sub) for sub in range(2) for tc in range(4)]
                            for ui, (q, tc, sub) in enumerate(units):
                                if ui == 4:
                                    load_cast(i + 1)
                                gu_unit(ex, q, tc, sl[0], sl[1], sub)
                        else:
                            if idx == 0 and ex + 1 < NE:
                                load_gate(ex + 1)
                            units = [(sl[hh], (idx * 2 + hh) * 2 + ds, ds, tc) for hh in range(2) for tc in range(4) for ds in range(2)]
                            for ui, (s_, dc, ds, tc) in enumerate(units):
                                if ui == 8:
                                    load_cast(i + 1)
                                dn_unit(s_, dc, ds, tc)
                    b.barrier()
                with contextlib.ExitStack() as ps:
                    xc = [b.sb(ps, [128, 8, 512], F32, "xr%d" % i) for i in range(2)]
                    R_xc = [Res(), Res()]
                    for tc in range(4):
                        s = tc % 2
                        t0 = T0 + tc * 512
                        b.dma("sp", xc[s][:], xT_v[:, :, t0:t0 + 512], reads=[R_x], writes=[R_xc[s]])
                        for dc in range(8):
                            b.op("dve", lambda e, dc=dc: e.scalar_tensor_tensor(out=xc[s][:, dc, :], in0=acc[:, dc, tc * 512:(tc + 1) * 512],
                                                                                scalar=modc[:, l, 5, dc:dc + 1], in1=xc[s][:, dc, :],
                                                                                op0=ALU.mult, op1=ALU.add),
                                 reads=[R_xc[s]], writes=[R_xc[s]])
                        b.dma("sp", xT_v[:, :, t0:t0 + 512], xc[s][:], reads=[R_xc[s]], writes=[R_x])
                    b.barrier()

    maskb = b.sb(es, [128, 4, 128], BF16, "maskb")
    invc = b.sb(es, [128, 1], F32, "invc")
    pic = b.sb(es, [128, 1], F32, "pic")
    with contextlib.ExitStack() as ps:
        mkf = b.sb(ps, [128, 4, 128], F32, "mkf")
        posi = b.sb(ps, [128, S], I32, "posi")
        posf = b.sb(ps, [128, S], F32, "posf")
        ang = b.sb(ps, [128, S], F32, "ang")
        R_t = Res()
        b.dma("sp", mkf[:], k_masks[:, :, :], writes=[R_t])
        b.dma("sp", invc[:], k_inv[:, :], writes=[R_t])
        b.dma("sp", posi[:], pos_in.partition_broadcast(128), writes=[R_t])
        b.op("pool", lambda e: e.memset(pic[:], float(np.pi / 2)), writes=[R_t])
        b.op("dve", lambda e: e.tensor_copy(out=maskb[:], in_=mkf[:]), reads=[R_t], writes=[R_t])
        b.op("dve", lambda e: e.tensor_copy(out=posf[:], in_=posi[:]), reads=[R_t], writes=[R_t])
        b.op("dve", lambda e: e.tensor_scalar(out=posf[:], in0=posf[:], scalar1=invc[:, 0:1], scalar2=None, op0=ALU.mult),
             reads=[R_t], writes=[R_t])
        C1 = 6.28125
        C2 = float(np.float32(2 * np.pi - 6.28125))
        C3 = float(2 * np.pi - 6.28125 - np.float64(np.float32(2 * np.pi - 6.28125)))
        kf = posi[:, :].bitcast(F32)
        b.op("dve", lambda e: e.tensor_scalar(out=ang[:], in0=posf[:], scalar1=float(1 / (2 * np.pi)), scalar2=None, op0=ALU.mult),
             reads=[R_t], writes=[R_t])
        b.op("dve", lambda e: e.tensor_copy(out=posi[:], in_=ang[:]), reads=[R_t], writes=[R_t])
        b.op("dve", lambda e: e.tensor_copy(out=ang[:], in_=posi[:]), reads=[R_t], writes=[R_t])
        for cc in (C1, C2, C3):
            b.op("dve", lambda e: e.scalar_tensor_tensor(out=posf[:], in0=ang[:], scalar=-cc, in1=posf[:], op0=ALU.mult, op1=ALU.add),
                 reads=[R_t], writes=[R_t])
        b.op("dve", lambda e: e.tensor_scalar(out=ang[:], in0=posf[:], scalar1=float(np.pi), scalar2=float(-2 * np.pi), op0=ALU.is_gt, op1=ALU.mult),
             reads=[R_t], writes=[R_t])
        b.op("dve", lambda e: e.tensor_tensor(out=posf[:], in0=posf[:], in1=ang[:], op=ALU.add), reads=[R_t], writes=[R_t])
        b.op("dve", lambda e: e.tensor_scalar(out=ang[:], in0=posf[:], scalar1=float(-np.pi), scalar2=float(2 * np.pi), op0=ALU.is_lt, op1=ALU.mult),
             reads=[R_t], writes=[R_t])
        b.op("dve", lambda e: e.tensor_tensor(out=posf[:], in0=posf[:], in1=ang[:], op=ALU.add), reads=[R_t], writes=[R_t])
        b.op("dve", lambda e: e.tensor_scalar(out=posf[:], in0=posf[:], scalar1=3.14159, scalar2=-3.14159, op0=ALU.min, op1=ALU.max),
             reads=[R_t], writes=[R_t])
        b.op("dve", lambda e: e.scalar_tensor_tensor(out=ang[:], in0=posf[:], scalar=-1.0, in1=posf[:], op0=ALU.mult, op1=ALU.max),
             reads=[R_t], writes=[R_t])
        b.op("act", lambda e: e.activation(out=ang[:], in_=ang[:], func=AF.Sin, bias=pic[:], scale=-1.0), reads=[R_t], writes=[R_t])
        b.op("act", lambda e: e.activation(out=posf[:], in_=posf[:], func=AF.Sin), reads=[R_t], writes=[R_t])
        b.dma("sp", tab_d[0], ang[:], reads=[R_t], writes=[R_tab])
        b.dma("sp", tab_d[1], posf[:], reads=[R_t], writes=[R_tab])
        b.barrier()

    def fbank(lst, st):
        st[0] = (st[0] + 1) % len(lst)
        return banks[lst[st[0]]]

    def rot_weights(dst, src, nblk, R_w):
        sv = src.rearrange("p (k h i) -> p k h i", h=2, i=32)
        dv = dst.rearrange("p (k h i) -> p k h i", h=2, i=32)
        b.op("dve", lambda e: e.tensor_scalar(out=dv[:, :, 0, :], in0=sv[:, :, 1, :], scalar1=-1.0, scalar2=None, op0=ALU.mult),
             reads=[R_w], writes=[R_w])
        b.op("dve", lambda e: e.tensor_copy(out=dv[:, :, 1, :], in_=sv[:, :, 0, :]), reads=[R_w], writes=[R_w])

    def rope_evac(out_ap, pa, pb_, C_ap, S_ap, t1, t2, R_tmp, reads, writes, npart=128, perm=None):
        b.op("dve", lambda e: e.tensor_tensor(out=t1[0:npart, :], in0=pa, in1=C_ap, op=ALU.mult), reads=reads, writes=[R_tmp])
        b.op("dve", lambda e: e.tensor_tensor(out=t2[0:npart, :], in0=pb_, in1=S_ap, op=ALU.mult), reads=reads, writes=[R_tmp])
        a1 = t1[0:npart, :]
        a2 = t2[0:npart, :]
        if perm is not None:
            a1 = a1.rearrange("p (m r) -> p r m", r=perm)
            a2 = a2.rearrange("p (m r) -> p r m", r=perm)
        b.op("pool", lambda e: e.tensor_tensor(out=out_ap, in0=a1, in1=a2, op=ALU.add), reads=[R_tmp], writes=writes)

    def load_tables(ps):
        Ct = b.sb(ps, [128, S], F32, "Ct")
        St = b.sb(ps, [128, S], F32, "St")
        R_T = Res()
        b.dma("sp", Ct[:], tab_d[0], reads=[R_tab], writes=[R_T])
        b.dma("sp", St[:], tab_d[1], reads=[R_tab], writes=[R_T])
        return Ct, St, R_T

    def norm_all(l, which, hT, R_h):
        with contextlib.ExitStack() as ps:
            nb = norm_bufs(ps)
            for tc in range(8):
                norm_chunk(nb, l, which, tc * 512, hT, tc * 512, R_h)
            b.barrier()

    def out_proj(l, w_out_l, nfc):
        with contextlib.ExitStack() as ps:
            wo = b.sb(ps, [128, nfc, D], BF16, "wo")
            R_wo = Res()
            ot = [b.sb(ps, [128, nfc, 512], BF16, "ot%d" % i) for i in range(2)]
            xc = [b.sb(ps, [128, 8, 512], F32, "ox%d" % i) for i in range(2)]
            R_ot = [Res(), Res()]
            R_xc = [Res(), Res()]
            wv_ = w_out_l.rearrange("(fc p) d -> p fc d", p=128)
            for i in range(nfc // 4):
                b.dma("pq", wo[:, i * 4:(i + 1) * 4, :], wv_[:, i * 4:(i + 1) * 4, :], writes=[R_wo])
            for tc in range(8):
                s = tc % 2
                t0 = tc * 512
                b.dma("sp", ot[s][:], oT_v[:, 0:nfc, t0:t0 + 512], reads=[R_o], writes=[R_ot[s]])
                b.dma("sp", xc[s][:], xT_v[:, :, t0:t0 + 512], reads=[R_x], writes=[R_xc[s]])
                for dc in range(8):
                    pt, pr = bank()
                    for fc in range(nfc):
                        b.op("pe", lambda e, fc=fc: e.matmul(pt[:, :], lhsT=wo[:, fc, dc * 128:(dc + 1) * 128], rhs=ot[s][:, fc, :],
                                                             start=(fc == 0), stop=(fc == nfc - 1)),
                             reads=[R_wo, R_ot[s]], writes=[pr])
                    b.op("dve", lambda e: e.scalar_tensor_tensor(out=xc[s][:, dc, :], in0=pt[:, :], scalar=modc[:, l, 2, dc:dc + 1],
                                                                 in1=xc[s][:, dc, :], op0=ALU.mult, op1=ALU.add),
                         reads=[pr, R_xc[s]], writes=[R_xc[s]])
                b.dma("sp", xT_v[:, :, t0:t0 + 512], xc[s][:], reads=[R_xc[s]], writes=[R_x])
            b.barrier()

    mla_d = dscr("mla_d", [6, 128, S], BF16)
    mla_v = mla_d.rearrange("c p t -> p c t")
    R_mla = Res()

    def mixer_even(l):
        li = l // 2
        win = w_in_ab[li].rearrange("(dc p) f -> p dc f", p=128)
        with contextlib.ExitStack() as hs:
            hT = b.sb(hs, [128, 8, S], BF16, "hTe")
            R_h = Res()
            norm_all(l, 0, hT, R_h)
            with contextlib.ExitStack() as ps:
                Ct, St, R_T = load_tables(ps)
                wA = b.sb(ps, [128, 8, 704], BF16, "wA")
                wKr = b.sb(ps, [128, 8, 64], BF16, "wKr")
                R_w = Res()
                gq = b.sb(ps, [128, 3], F32, "gq")
                gkv = b.sb(ps, [128, 2], F32, "gkv")
                b.dma("pq", wA[:], win[:, :, 0:704], writes=[R_w])
                b.dma("sp", gq[:], col_view(mla_g_q[li], 3), writes=[R_w], allow_slow_non_contiguous=True)
                b.dma("sp", gkv[:], col_view(mla_g_kv[li], 2), writes=[R_w], allow_slow_non_contiguous=True)
                for dc in range(8):
                    rot_weights(wKr[:, dc, :], wA[:, dc, 640:704], 1, R_w)
                sqb = [b.sb(ps, [128, 3, 512], BF16, "sqb%d" % i) for i in range(2)]
                R_sqb = [Res(), Res()]
                rsq = [b.sb(ps, [128, 512], F32, "rsq%d" % i) for i in range(2)]
                R_rsq = [Res(), Res()]
                ob = [b.sb(ps, [128, 6, 512], BF16, "mob%d" % i) for i in range(2)]
                R_ob = [Res(), Res()]
                t1 = b.sb(ps, [128, 512], F32, "t1")
                t2 = b.sb(ps, [128, 512], F32, "t2")
                R_tmp = Res()
                kk = 0
                for tc in range(8):
                    t0 = tc * 512
                    so = tc % 2
                    for (c0, nch, gcolv, oc0) in ((0, 3, gq, 0), (384, 2, gkv, 3)):
                        s2 = kk % 2
                        kk += 1
                        pcs = []
                        for c in range(nch):
                            pt, pr = bank()
                            for dc in range(8):
                                b.op("pe", lambda e, dc=dc: e.matmul(pt[:, :], lhsT=wA[:, dc, c0 + c * 128:c0 + (c + 1) * 128], rhs=hT[:, dc, t0:t0 + 512],
                                                                     start=(dc == 0), stop=(dc == 7)),
                                     reads=[R_w, R_h], writes=[pr])
                            b.op("act", lambda e: e.activation(out=sqb[s2][:, c, :], in_=pt[:, :], func=AF.Square), reads=[pr], writes=[R_sqb[s2]])
                            pcs.append((pt, pr))
                        pss, prs = bank()
                        for c in range(nch):
                            b.op("pe", lambda e: e.matmul(pss[:, :], lhsT=onesb[:], rhs=sqb[s2][:, c, :], start=(c == 0), stop=(c == nch - 1)),
                                 reads=[R_sqb[s2]], writes=[prs])
                        b.op("act", lambda e: e.activation(out=rsq[s2][:], in_=pss[:, :], func=AF.Sqrt, bias=epsc[:], scale=1.0 / (nch * 128)),
                             reads=[prs], writes=[R_rsq[s2]])
                        b.op("dve", lambda e: e.reciprocal(out=rsq[s2][:], in_=rsq[s2][:]), reads=[R_rsq[s2]], writes=[R_rsq[s2]])
                        for c in range(nch):
                            pt, pr = pcs[c]
                            b.op("dve", lambda e: e.scalar_tensor_tensor(out=ob[so][:, oc0 + c, :], in0=pt[:, :], scalar=gcolv[:, c:c + 1],
                                                                         in1=rsq[s2][:], op0=ALU.mult, op1=ALU.mult),
                                 reads=[pr, R_rsq[s2], R_w], writes=[R_ob[so]])
                    pa, pra = bank()
                    pb_, prb = bank()
                    for dc in range(8):
                        b.op("pe", lambda e, dc=dc: e.matmul(pa[0:64, :], lhsT=wA[:, dc, 640:704], rhs=hT[:, dc, t0:t0 + 512],
                                                             start=(dc == 0), stop=(dc == 7)), reads=[R_w, R_h], writes=[pra])
                    for dc in range(8):
                        b.op("pe", lambda e, dc=dc: e.matmul(pb_[0:64, :], lhsT=wKr[:, dc, :], rhs=hT[:, dc, t0:t0 + 512],
                                                             start=(dc == 0), stop=(dc == 7)), reads=[R_w, R_h], writes=[prb])
                    rope_evac(ob[so][0:64, 5, :], pa[0:64, :], pb_[0:64, :], Ct[0:64, t0:t0 + 512], St[0:64, t0:t0 + 512], t1, t2, R_tmp,
                              [pra, prb, R_T], [R_ob[so]], npart=64)
                    b.dma("sp", mla_v[:, 0:5, t0:t0 + 512], ob[so][:, 0:5, :], reads=[R_ob[so]], writes=[R_mla])
                    b.dma("sp", mla_v[0:64, 5, t0:t0 + 512], ob[so][0:64, 5, :], reads=[R_ob[so]], writes=[R_mla])
                b.barrier()
            with contextlib.ExitStack() as ps:
                Ct, St, R_T = load_tables(ps)
                kT = b.sb(ps, [128, 2, S], BF16, "kTb")
                vd = b.sb(ps, [128, 2, 32, 128], BF16, "vdb")
                R_k = Res()
                R_v = Res()
                wk = b.sb(ps, [128, 8, 256], BF16, "wkd")
                wkr = b.sb(ps, [128, 8, 256], BF16, "wkdr")
                wv = b.sb(ps, [128, 8, 256], BF16, "wvd")
                R_w = Res()
                es_ = b.sb(ps, [128, 16], F32, "esink")
                b.dma("sp", es_[:], swa_sink[li].partition_broadcast(128), writes=[R_w])
                b.op("act", lambda e: e.activation(out=es_[:], in_=es_[:], func=AF.Exp), reads=[R_w], writes=[R_w])
                for g in range(2):
                    for dup in range(2):
                        o0 = g * 128 + dup * 64
                        b.dma("pq", wk[:, :, o0:o0 + 64], win[:, :, 1728 + g * 64:1728 + (g + 1) * 64], writes=[R_w])
                        b.dma("pq", wv[:, :, o0:o0 + 64], win[:, :, 1856 + g * 64:1856 + (g + 1) * 64], writes=[R_w])
                for dc in range(8):
                    rot_weights(wkr[:, dc, :], wk[:, dc, :], 4, R_w)
                t1 = b.sb(ps, [128, 512], F32, "t1")
                t2 = b.sb(ps, [128, 512], F32, "t2")
                R_tmp = Res()
                for tc in range(8):
                    t0 = tc * 512
                    for g in range(2):
                        pa, pra = bank()
                        pb_, prb = bank()
                        for dc in range(8):
                            b.op("pe", lambda e, dc=dc: e.matmul(pa[:, :], lhsT=wk[:, dc, g * 128:(g + 1) * 128], rhs=hT[:, dc, t0:t0 + 512],
                                                                 start=(dc == 0), stop=(dc == 7)), reads=[R_w, R_h], writes=[pra])
                        for dc in range(8):
                            b.op("pe", lambda e, dc=dc: e.matmul(pb_[:, :], lhsT=wkr[:, dc, g * 128:(g + 1) * 128], rhs=hT[:, dc, t0:t0 + 512],
                                                                 start=(dc == 0), stop=(dc == 7)), reads=[R_w, R_h], writes=[prb])
                        rope_evac(kT[:, g, t0:t0 + 512], pa[:, :], pb_[:, :], Ct[:, t0:t0 + 512], St[:, t0:t0 + 512], t1, t2, R_tmp,
                                  [pra, prb, R_T], [R_k])
                for i in range(32):
                    pt, pr = bank()
                    for dc in range(8):
                        b.op("pe", lambda e, dc=dc: e.matmul(pt[:, 0:256], lhsT=hT[:, dc, i * 128:(i + 1) * 128], rhs=wv[:, dc, :],
                                                             start=(dc == 0), stop=(dc == 7)), reads=[R_w, R_h], writes=[pr])
                    b.op("act", lambda e: e.copy(out=vd[:, :, i, :], in_=pt[:, 0:256].rearrange("p (g f) -> p g f", g=2)),
                         reads=[pr], writes=[R_v])
                wq = [b.sb(ps, [128, 8, 128], BF16, "wq%d" % i) for i in range(2)]
                wqr = [b.sb(ps, [128, 8, 128], BF16, "wqr%d" % i) for i in range(2)]
                R_wq = [Res(), Res()]
                qT = [b.sb(ps, [128, S], BF16, "qTb%d" % i) for i in range(2)]
                R_q = [Res(), Res()]
                oTj = [b.sb(ps, [128, S], BF16, "oTb%d" % i) for i in range(2)]
                R_oj = [Res(), Res()]
                Pb = [b.sb(ps, [128, 384], BF16, "Pb%d" % i) for i in range(3)]
                R_P = [Res() for _ in range(3)]
                dn = [b.sb(ps, [128, 128], F32, "dn%d" % i) for i in range(2)]
                R_dn = [Res(), Res()]
                pks = [0, 0]
                for j in range(8):
                    s = j % 2
                    g = j // 4
                    b.dma("pq", wq[s][:], win[:, :, 704 + j * 128:704 + (j + 1) * 128], writes=[R_wq[s]])
                    for dc in range(8):
                        rot_weights(wqr[s][:, dc, :], wq[s][:, dc, :], 2, R_wq[s])
                    for tc in range(8):
                        t0 = tc * 512
                        pa, pra = bank()
                        pb_, prb = bank()
                        for dc in range(8):
                            b.op("pe", lambda e, dc=dc: e.matmul(pa[:, :], lhsT=wq[s][:, dc, :], rhs=hT[:, dc, t0:t0 + 512],
                                                                 start=(dc == 0), stop=(dc == 7)), reads=[R_wq[s], R_h], writes=[pra])
                        for dc in range(8):
                            b.op("pe", lambda e, dc=dc: e.matmul(pb_[:, :], lhsT=wqr[s][:, dc, :], rhs=hT[:, dc, t0:t0 + 512],
                                                                 start=(dc == 0), stop=(dc == 7)), reads=[R_wq[s], R_h], writes=[prb])
                        rope_evac(qT[s][:, t0:t0 + 512], pa[:, :], pb_[:, :], Ct[:, t0:t0 + 512], St[:, t0:t0 + 512], t1, t2, R_tmp,
                                  [pra, prb, R_T], [R_q[s]])
                    for hh in range(2):
                        h = 2 * j + hh
                        p0 = hh * 64
                        p1 = p0 + 64
                        def swaA(qb):
                            kbs = [kb for kb in (qb - 1, qb, qb + 1) if 0 <= kb < 32]
                            n = len(kbs)
                            pS, prS = bank()
                            for idx, kb in enumerate(kbs):
                                b.op("pe", lambda e: e.matmul(pS[:, idx * 128:(idx + 1) * 128], lhsT=kT[p0:p1, g, kb * 128:(kb + 1) * 128],
                                                              rhs=qT[s][p0:p1, qb * 128:(qb + 1) * 128], start=True, stop=(kb == qb)),
                                     reads=[R_k, R_q[s]], writes=[prS])
                                if kb != qb:
                                    m = 0 if kb < qb else 1
                                    b.op("pe", lambda e: e.matmul(pS[:, idx * 128:(idx + 1) * 128], lhsT=identb[:], rhs=maskb[:, m, :],
                                                                  start=False, stop=True), writes=[prS])
                            pi = pks[0] % 3
                            pks[0] += 1
                            b.op("act", lambda e: e.activation(out=Pb[pi][:, 0:n * 128], in_=pS[:, 0:n * 128], func=AF.Exp, scale=0.125),
                                 reads=[prS], writes=[R_P[pi]])
                            return (qb, kbs, pi)

                        def swaB(st_):
                            qb, kbs, pi = st_
                            n = len(kbs)
                            po, pro = bank()
                            pd, prd = bank()
                            for idx, kb in enumerate(kbs):
                                b.op("pe", lambda e: e.matmul(po[:, 0:128], lhsT=vd[:, g, kb, :], rhs=Pb[pi][:, idx * 128:(idx + 1) * 128],
                                                              start=(idx == 0), stop=(idx == n - 1)), reads=[R_v, R_P[pi]], writes=[pro])
                            for idx, kb in enumerate(kbs):
                                b.op("pe", lambda e: e.matmul(pd[:, 0:128], lhsT=onesb[:], rhs=Pb[pi][:, idx * 128:(idx + 1) * 128],
                                                              start=(idx == 0), stop=(idx == n - 1)), reads=[R_P[pi]], writes=[prd])
                            di = pks[1] % 2
                            pks[1] += 1
                            b.op("dve", lambda e: e.tensor_scalar(out=dn[di][p0:p1, :], in0=pd[p0:p1, 0:128], scalar1=es_[p0:p1, h:h + 1], scalar2=None,
                                                                  op0=ALU.add), reads=[prd, R_w], writes=[R_dn[di]])
                            b.op("dve", lambda e: e.reciprocal(out=dn[di][p0:p1, :], in_=dn[di][p0:p1, :]), reads=[R_dn[di]], writes=[R_dn[di]])
                            b.op("dve", lambda e: e.tensor_tensor(out=oTj[s][p0:p1, qb * 128:(qb + 1) * 128], in0=po[p0:p1, 0:128],
                                                                  in1=dn[di][p0:p1, :], op=ALU.mult), reads=[pro, R_dn[di]], writes=[R_oj[s]])

                        nxt = swaA(0)
                        for qb in range(32):
                            cur = nxt
                            if qb + 1 < 32:
                                nxt = swaA(qb + 1)
                            swaB(cur)
                    b.dma("sp", oT_v[:, 8 + j, :], oTj[s][:], reads=[R_oj[s]], writes=[R_o])
                b.barrier()
        with contextlib.ExitStack() as ps:
            Ct, St, R_T = load_tables(ps)
            cqn = b.sb(ps, [128, 3, S], BF16, "cqn")
            ckvn = b.sb(ps, [128, 2, S], BF16, "ckvn")
            krT = b.sb(ps, [128, S], BF16, "krT")
            R_c = Res()
            b.op("pool", lambda e: e.memset(krT[64:128, :], 0.0), writes=[R_c])
            b.dma("sp", cqn[:], mla_v[:, 0:3, :], reads=[R_mla], writes=[R_c])
            b.dma("sp", ckvn[:], mla_v[:, 3:5, :], reads=[R_mla], writes=[R_c])
            b.dma("sp", krT[0:64, :], mla_v[0:64, 5, :], reads=[R_mla], writes=[R_c])
            wqh = [b.sb(ps, [128, 3, 192], BF16, "wqh%d" % i) for i in range(2)]
            wqhr = [b.sb(ps, [128, 3, 64], BF16, "wqhr%d" % i) for i in range(2)]
            wkvh = [b.sb(ps, [128, 2, 256], BF16, "wkvh%d" % i) for i in range(2)]
            R_wh = [Res(), Res()]
            qn = [b.sb(ps, [128, S], BF16, "qn%d" % i) for i in range(2)]
            qr = [b.sb(ps, [128, S], BF16, "qr%d" % i) for i in range(2)]
            kn = [b.sb(ps, [128, S], BF16, "kn%d" % i) for i in range(2)]
            vv = [b.sb(ps, [128, 32, 128], BF16, "vv%d" % i) for i in range(2)]
            R_hd = [Res(), Res()]
            for i in range(2):
                b.op("pool", lambda e: e.memset(qr[i][64:128, :], 0.0), writes=[R_hd[i]])
            oTh = [b.sb(ps, [128, S], BF16, "oTh%d" % i) for i in range(2)]
            R_oh = [Res(), Res()]
            Pm = [b.sb(ps, [128, 512], BF16, "Pm%d" % i) for i in range(4)]
            R_Pm = [Res() for _ in range(4)]
            Pacc = [[b.sb(ps, [128, 512], F32, "Pacc%d%d" % (i, k_)) for k_ in range(2)] for i in range(2)]
            R_Pacc = [[Res(), Res()], [Res(), Res()]]
            dn = [b.sb(ps, [128, 512], F32, "dnm%d" % i) for i in range(2)]
            R_dn = [Res(), Res()]
            t1 = b.sb(ps, [128, 512], F32, "t1")
            t2 = b.sb(ps, [128, 512], F32, "t2")
            R_tmp = Res()
            wqb_v = mla_w_qb[li].rearrange("(c p) f -> p c f", p=128)
            wkvb_v = mla_w_kvb[li].rearrange("(c p) f -> p c f", p=128)
            SC = float(192.0 ** -0.5)
            pkm = [0]
            acc_st = [0]
            s_st = [0]
            for h in range(8):
                s = h % 2
                b.dma("pq", wqh[s][:], wqb_v[:, :, h * 192:(h + 1) * 192], writes=[R_wh[s]])
                b.dma("pq", wkvh[s][:], wkvb_v[:, :, h * 256:(h + 1) * 256], writes=[R_wh[s]])
                for c in range(3):
                    rot_weights(wqhr[s][:, c, :], wqh[s][:, c, 128:192], 1, R_wh[s])
                for tc in range(8):
                    t0 = tc * 512
                    pt, pr = fbank([4, 5], s_st)
                    for c in range(3):
                        b.op("pe", lambda e: e.matmul(pt[:, :], lhsT=wqh[s][:, c, 0:128], rhs=cqn[:, c, t0:t0 + 512], start=(c == 0), stop=(c == 2)),
                             reads=[R_wh[s], R_c], writes=[pr])
                    b.op("act", lambda e: e.copy(out=qn[s][:, t0:t0 + 512], in_=pt[:, :]), reads=[pr], writes=[R_hd[s]])
                    pt, pr = fbank([4, 5], s_st)
                    for c in range(2):
                        b.op("pe", lambda e: e.matmul(pt[:, :], lhsT=wkvh[s][:, c, 0:128], rhs=ckvn[:, c, t0:t0 + 512], start=(c == 0), stop=(c == 1)),
                             reads=[R_wh[s], R_c], writes=[pr])
                    b.op("act", lambda e: e.copy(out=kn[s][:, t0:t0 + 512], in_=pt[:, :]), reads=[pr], writes=[R_hd[s]])
                    pa, pra = fbank([4, 5], s_st)
                    pb_, prb = fbank([4, 5], s_st)
                    for c in range(3):
                        b.op("pe", lambda e: e.matmul(pa[0:64, :], lhsT=wqh[s][:, c, 128:192], rhs=cqn[:, c, t0:t0 + 512], start=(c == 0), stop=(c == 2)),
                             reads=[R_wh[s], R_c], writes=[pra])
                    for c in range(3):
                        b.op("pe", lambda e: e.matmul(pb_[0:64, :], lhsT=wqhr[s][:, c, :], rhs=cqn[:, c, t0:t0 + 512], start=(c == 0), stop=(c == 2)),
                             reads=[R_wh[s], R_c], writes=[prb])
                    rope_evac(qr[s][0:64, t0:t0 + 512], pa[0:64, :], pb_[0:64, :], Ct[0:64, t0:t0 + 512], St[0:64, t0:t0 + 512], t1, t2, R_tmp,
                              [pra, prb, R_T], [R_hd[s]], npart=64)
                for i in range(32):
                    pt, pr = fbank([4, 5], s_st)
                    for c in range(2):
                        b.op("pe", lambda e: e.matmul(pt[:, 0:128], lhsT=ckvn[:, c, i * 128:(i + 1) * 128], rhs=wkvh[s][:, c, 128:256],
                                                      start=(c == 0), stop=(c == 1)), reads=[R_wh[s], R_c], writes=[pr])
                    b.op("act", lambda e: e.copy(out=vv[s][:, i, :], in_=pt[:, 0:128]), reads=[pr], writes=[R_hd[s]])
                for qc in range(8):
                    q0 = qc * 512
                    a = acc_st[0] % 2
                    acc_st[0] += 1
                    po, pro = banks[0 + 2 * a]
                    pd, prd = banks[1 + 2 * a]
                    def emitS(kt):
                        pS, prS = fbank([4, 5], s_st)
                        b.op("pe", lambda e: e.matmul(pS[:, :], lhsT=kn[s][:, kt * 128:(kt + 1) * 128], rhs=qn[s][:, q0:q0 + 512], start=True, stop=False),
                             reads=[R_hd[s]], writes=[prS])
                        b.op("pe", lambda e: e.matmul(pS[:, :], lhsT=krT[:, kt * 128:(kt + 1) * 128], rhs=qr[s][:, q0:q0 + 512], start=False, stop=True),
                             reads=[R_hd[s], R_c], writes=[prS])
                        pi = pkm[0] % 4
                        pkm[0] += 1
                        b.op("act", lambda e: e.activation(out=Pm[pi][:], in_=pS[:, :], func=AF.Exp, scale=SC), reads=[prS], writes=[R_Pm[pi]])
                        return pi

                    nxt = emitS(0)
                    for kt in range(32):
                        pi = nxt
                        if kt + 1 < 32:
                            nxt = emitS(kt + 1)
                        b.op("pe", lambda e: e.matmul(po[:, :], lhsT=vv[s][:, kt, :], rhs=Pm[pi][:], start=(kt == 0), stop=(kt == 31)),
                             reads=[R_hd[s], R_Pm[pi]], writes=[pro])
                        w_ = 1 if (kt % 3 == 2) else 0
                        eng_ = "pool" if w_ else "dve"
                        if kt == 0 or kt == 2:
                            b.op(eng_, lambda e: e.tensor_copy(out=Pacc[a][w_][:], in_=Pm[pi][:]), reads=[R_Pm[pi]], writes=[R_Pacc[a][w_]])
                        else:
                            b.op(eng_, lambda e: e.tensor_tensor(out=Pacc[a][w_][:], in0=Pacc[a][w_][:], in1=Pm[pi][:], op=ALU.add),
                                 reads=[R_Pm[pi], R_Pacc[a][w_]], writes=[R_Pacc[a][w_]])
                    for w_ in range(2):
                        b.op("pe", lambda e: e.matmul(pd[:, :], lhsT=meanm[:], rhs=Pacc[a][w_][:], start=(w_ == 0), stop=(w_ == 1)),
                             reads=[R_Pacc[a][w_]], writes=[prd])
                    b.op("dve", lambda e: e.reciprocal(out=dn[a][:], in_=pd[:, :]), reads=[prd], writes=[R_dn[a]])
                    b.op("dve", lambda e: e.scalar_tensor_tensor(out=oTh[s][:, q0:q0 + 512], in0=po[:, :], scalar=1.0 / D, in1=dn[a][:],
                                                                 op0=ALU.mult, op1=ALU.mult),
                         reads=[pro, R_dn[a]], writes=[R_oh[s]])
                b.dma("sp", oT_v[:, h, :], oTh[s][:], reads=[R_oh[s]], writes=[R_o])
            b.barrier()
        out_proj(l, w_out_ab[li], 16)

    def mixer_odd(l):
        li = l // 2
        win = w_in_c[li].rearrange("(dc p) f -> p dc f", p=128)
        with contextlib.ExitStack() as hs:
            hT = b.sb(hs, [128, 8, S], BF16, "hTo")
            R_h = Res()
            norm_all(l, 0, hT, R_h)
            Ct, St, R_T = load_tables(hs)
            num = b.sb(hs, [128, S], F32, "num")
            den = b.sb(hs, [128, S], F32, "den")
            R_nd = Res()
            oTj = b.sb(hs, [128, S], BF16, "oTc")
            R_oj = Res()
            t1 = b.sb(hs, [128, 512], F32, "t1")
            t2 = b.sb(hs, [128, 512], F32, "t2")
            R_tmp = Res()
            wts = [b.sb(hs, [128, 8, 128], BF16, "wc%d" % i) for i in range(5)]
            R_w = Res()
            KPMAX = S + 128 * 16
            qP = b.sb(hs, [128, S], BF16, "qP")
            kP = b.sb(hs, [128, KPMAX], BF16, "kP")
            vP = b.sb(hs, [128, KPMAX], BF16, "vP")
            Vt = b.sb(hs, [128, KPMAX // 128, 128], BF16, "Vt")
            R_q = Res()
            R_k = Res()
            R_vp = Res()
            R_vt = Res()
            Pb = [b.sb(hs, [128, 256], BF16, "Pc%d" % i) for i in range(3)]
            R_P = [Res() for _ in range(3)]
            pkd = [0]
            def load_w(j_, g_):
                base = g_ * 3072 + j_ * 128
                b.dma("pq", wts[0][:], win[:, :, base:base + 128], writes=[R_w])
                b.dma("pq", wts[2][:], win[:, :, base + 1024:base + 1152], writes=[R_w])
                b.dma("pq", wts[4][:], win[:, :, base + 2048:base + 2176], writes=[R_w])
                for dc in range(8):
                    rot_weights(wts[1][:, dc, :], wts[0][:, dc, :], 2, R_w)
                    rot_weights(wts[3][:, dc, :], wts[2][:, dc, :], 2, R_w)

            load_w(0, 0)
            for j in range(8):
                for g, d in enumerate((1, 4, 16)):
                    L = S // d
                    Lp = L + 128
                    nt = L // 128 + 1
                    nq = L // 128
                    qv = qP[:, :].rearrange("p (r m) -> p r m", r=d)
                    kv = kP[:, 0:d * Lp].rearrange("p (r m) -> p r m", r=d)
                    vv_ = vP[:, 0:d * Lp].rearrange("p (r m) -> p r m", r=d)
                    b.op("pool", lambda e: e.memset(kv[:, :, 0:64], 0.0), writes=[R_k])
                    b.op("pool", lambda e: e.memset(kv[:, :, 64 + L:Lp], 0.0), writes=[R_k])
                    b.op("pool", lambda e: e.memset(vv_[:, :, 0:64], 0.0), writes=[R_vp])
                    b.op("pool", lambda e: e.memset(vv_[:, :, 64 + L:Lp], 0.0), writes=[R_vp])
                    for tc in range(8):
                        t0 = tc * 512
                        m0 = t0 // d
                        mw = 512 // d
                        prs = []
                        for wi in range(5):
                            pt, pr = bank()
                            for dc in range(8):
                                b.op("pe", lambda e, dc=dc: e.matmul(pt[:, :], lhsT=wts[wi][:, dc, :], rhs=hT[:, dc, t0:t0 + 512],
                                                                     start=(dc == 0), stop=(dc == 7)), reads=[R_w, R_h], writes=[pr])
                            prs.append((pt, pr))
                            if wi == 1:
                                rope_evac(qv[:, :, m0:m0 + mw], prs[0][0][:, :], prs[1][0][:, :], Ct[:, t0:t0 + 512], St[:, t0:t0 + 512],
                                          t1, t2, R_tmp, [prs[0][1], prs[1][1], R_T], [R_q], perm=d)
                            if wi == 3:
                                rope_evac(kv[:, :, 64 + m0:64 + m0 + mw], prs[2][0][:, :], prs[3][0][:, :], Ct[:, t0:t0 + 512], St[:, t0:t0 + 512],
                                          t1, t2, R_tmp, [prs[2][1], prs[3][1], R_T], [R_k], perm=d)
                            if wi == 4:
                                b.op("act", lambda e: e.copy(out=vv_[:, :, 64 + m0:64 + m0 + mw],
                                                             in_=pt[:, :].rearrange("p (m r) -> p r m", r=d)), reads=[pr], writes=[R_vp])
                    if not (j == 7 and g == 2):
                        load_w(j + (g + 1) // 3, (g + 1) % 3)
                    ntile = d * nt
                    for i0 in range(0, ntile, 8):
                        nn = min(8, ntile - i0)
                        pbt, pbr = bbank()
                        for ii in range(nn):
                            i = i0 + ii
                            r_, jt = divmod(i, nt)
                            c0 = r_ * Lp + jt * 128
                            b.op("pe", lambda e: e.transpose(out=pbt[:, ii * 128:(ii + 1) * 128], in_=vP[:, c0:c0 + 128], identity=identb[:]),
                                 reads=[R_vp], writes=[pbr])
                        b.op("act", lambda e: e.copy(out=Vt[:, i0:i0 + nn, :], in_=pbt[:, 0:nn * 128].rearrange("p (i f) -> p i f", f=128)),
                             reads=[pbr], writes=[R_vt])
                    for hh in range(2):
                        p0 = hh * 64
                        p1 = p0 + 64
                        def dilA(r_, n):
                            q0 = r_ * L + n * 128
                            k0 = r_ * Lp + n * 128
                            pS, prS = bank()
                            for idx in range(2):
                                if idx == 0:
                                    m = 2 if n == 0 else 0
                                else:
                                    m = 3 if n == nq - 1 else 1
                                b.op("pe", lambda e: e.matmul(pS[:, idx * 128:(idx + 1) * 128], lhsT=kP[p0:p1, k0 + idx * 128:k0 + (idx + 1) * 128],
                                                              rhs=qP[p0:p1, q0:q0 + 128], start=True, stop=False),
                                     reads=[R_k, R_q], writes=[prS])
                                b.op("pe", lambda e: e.matmul(pS[:, idx * 128:(idx + 1) * 128], lhsT=identb[:], rhs=maskb[:, m, :],
                                                              start=False, stop=True), writes=[prS])
                            pi = pkd[0] % 3
                            pkd[0] += 1
                            b.op("act", lambda e: e.activation(out=Pb[pi][:], in_=pS[:, 0:256], func=AF.Exp, scale=0.125),
                                 reads=[prS], writes=[R_P[pi]])
                            return (r_, n, pi)

                        def dilB(st_):
                            r_, n, pi = st_
                            po, pro = bank()
                            pd, prd = bank()
                            ti = r_ * nt + n
                            for idx in range(2):
                                b.op("pe", lambda e: e.matmul(po[:, 0:128], lhsT=Vt[:, ti + idx, :], rhs=Pb[pi][:, idx * 128:(idx + 1) * 128],
                                                              start=(idx == 0), stop=(idx == 1)), reads=[R_vt, R_P[pi]], writes=[pro])
                            for idx in range(2):
                                b.op("pe", lambda e: e.matmul(pd[:, 0:128], lhsT=onesb[:], rhs=Pb[pi][:, idx * 128:(idx + 1) * 128],
                                                              start=(idx == 0), stop=(idx == 1)), reads=[R_P[pi]], writes=[prd])
                            nv = num[p0:p1, :].rearrange("p (m r) -> p r m", r=d)[:, r_, n * 128:(n + 1) * 128]
                            dv = den[p0:p1, :].rearrange("p (m r) -> p r m", r=d)[:, r_, n * 128:(n + 1) * 128]
                            if g == 0:
                                b.op("act", lambda e: e.copy(out=nv, in_=po[p0:p1, 0:128]), reads=[pro], writes=[R_nd])
                                b.op("dve", lambda e: e.tensor_copy(out=dv, in_=pd[p0:p1, 0:128]), reads=[prd], writes=[R_nd])
                            else:
                                b.op("dve", lambda e: e.tensor_tensor(out=nv, in0=po[p0:p1, 0:128], in1=nv, op=ALU.add), reads=[pro, R_nd], writes=[R_nd])
                                b.op("dve", lambda e: e.tensor_tensor(out=dv, in0=pd[p0:p1, 0:128], in1=dv, op=ALU.add), reads=[prd, R_nd], writes=[R_nd])

                        ulist = [(r_, n) for r_ in range(d) for n in range(nq)]
                        nxt = dilA(*ulist[0])
                        for ui in range(len(ulist)):
                            cur = nxt
                            if ui + 1 < len(ulist):
                                nxt = dilA(*ulist[ui + 1])
                            dilB(cur)
                for q4 in range(8):
                    sl = slice(q4 * 512, (q4 + 1) * 512)
                    b.op("dve", lambda e: e.reciprocal(out=den[:, sl], in_=den[:, sl]), reads=[R_nd], writes=[R_nd])
                    b.op("dve", lambda e: e.tensor_tensor(out=oTj[:, sl], in0=num[:, sl], in1=den[:, sl], op=ALU.mult), reads=[R_nd], writes=[R_oj])
                b.dma("sp", oT_v[:, j, :], oTj[:], reads=[R_oj], writes=[R_o])
            b.barrier()
        out_proj(l, w_out_c[li], 8)

    for l in layers:
        if cfg["mixer"]:
            if l % 2 == 0:
                mixer_even(l)
            else:
                mixer_odd(l)
        if cfg["ffn"]:
            moe_layer(l)

    with contextlib.ExitStack() as ps:
        xc = [b.sb(ps, [128, 8, 512], F32, "fx%d" % i) for i in range(2)]
        sq = b.sb(ps, [128, 8, 512], F32, "fsq")
        rs = b.sb(ps, [128, 512], F32, "frs")
        yo = [b.sb(ps, [128, D], F32, "fy%d" % i) for i in range(2)]
        R_xc = [Res(), Res()]
        R_sq = Res()
        R_rs = Res()
        R_yo = [Res(), Res()]
        k = 0
        for tc in range(8):
            s = tc % 2
            t0 = tc * 512
            b.dma("sp", xc[s][:], xT_v[:, :, t0:t0 + 512], reads=[R_x], writes=[R_xc[s]])
            b.op("act", lambda e: e.activation(out=sq[:], in_=xc[s][:], func=AF.Square), reads=[R_xc[s]], writes=[R_sq])
            pt, pr = bank()
            for dc in range(8):
                b.op("pe", lambda e, dc=dc: e.matmul(pt[:, :], lhsT=meanm[:], rhs=sq[:, dc, :], start=(dc == 0), stop=(dc == 7)),
                     reads=[R_sq], writes=[pr])
            b.op("act", lambda e: e.activation(out=rs[:], in_=pt[:, :], func=AF.Sqrt, bias=epsc[:], scale=1.0),
                 reads=[pr], writes=[R_rs])
            b.op("dve", lambda e: e.reciprocal(out=rs[:], in_=rs[:]), reads=[R_rs], writes=[R_rs])
            for dc in range(8):
                b.op("dve", lambda e, dc=dc: e.scalar_tensor_tensor(out=xc[s][:, dc, :], in0=xc[s][:, dc, :], scalar=gfin[:, dc:dc + 1],
                                                                    in1=rs[:], op0=ALU.mult, op1=ALU.mult),
                     reads=[R_rs, R_xc[s]], writes=[R_xc[s]])
            for i in range(4):
                ys = k % 2
                k += 1
                for hb in range(2):
                    pt, pr = bank()
                    for cc in range(4):
                        dc = hb * 4 + cc
                        b.op("pe", lambda e, dc=dc, cc=cc: e.transpose(out=pt[:, cc * 128:(cc + 1) * 128], in_=xc[s][:, dc, i * 128:(i + 1) * 128],
                                                                       identity=identf[:]),
                             reads=[R_xc[s]], writes=[pr])
                    if hb == 0:
                        b.op("dve", lambda e: e.tensor_copy(out=yo[ys][:, 0:512], in_=pt[:, :]), reads=[pr], writes=[R_yo[ys]])
                    else:
                        b.op("act", lambda e: e.copy(out=yo[ys][:, 512:1024], in_=pt[:, :]), reads=[pr], writes=[R_yo[ys]])
                r0 = t0 + i * 128
                b.dma("sp", y_out[r0:r0 + 128, :], yo[ys][:], reads=[R_yo[ys]], writes=[R_o])
        b.barrier()
    return nc


def make_consts():
    identf = np.eye(128, dtype=np.float32)
    i = np.arange(128) % 32
    inv = (10000.0 ** (-(2.0 * i.astype(np.float32)) / 64.0)).astype(np.float32).reshape(128, 1)
    a = np.arange(128)[:, None]
    bq = np.arange(128)[None, :]
    NEG = -30000.0
    masks = np.zeros((128, 4, 128), dtype=np.float32)
    masks[:, 0, :] = np.where(a >= bq, 0.0, NEG)
    masks[:, 1, :] = np.where(a <= bq, 0.0, NEG)
    masks[:, 2, :] = np.where((a >= bq) & (a >= 64), 0.0, NEG)
    masks[:, 3, :] = np.where((a <= bq) & (a < 64), 0.0, NEG)
    return {"k_identf": identf, "k_inv": inv, "k_masks": masks}


_CACHE = {}


def kernel(**inputs):
    cfg = CFG
    ncores = cfg["ncores"]
    key = repr(cfg)
    if key not in _CACHE:
        _CACHE[key] = build(cfg)
    nc = _CACHE[key]
    consts = make_consts()
    shared = {}
    for k_, v in inputs.items():
        if k_ in ("x", "c", "positions"):
            continue
        shared[k_] = np.ascontiguousarray(v)
    in_maps = []
    for cidx in range(ncores):
        m = dict(shared)
        m.update(consts)
        m["x"] = np.ascontiguousarray(inputs["x"][cidx])
        m["c"] = np.ascontiguousarray(inputs["c"][cidx])
        m["positions"] = np.ascontiguousarray(inputs["positions"][cidx]).astype(np.int32)
        in_maps.append(m)
    res = run_bass_kernel_spmd(nc, in_maps, core_ids=list(range(ncores)))
    out = np.stack([np.asarray(r["y"]) for r in res.results], axis=0)
    if ncores < 8:
        full = np.zeros((8, S, D), dtype=np.float32)
        full[:ncores] = out
        out = full
    return out.astype(np.float32)
```

```python
import contextlib
import numpy as np
import ml_dtypes
import concourse.bass as bass
import concourse.mybir as mybir
from concourse.bass_utils import run_bass_kernel_spmd

F32 = mybir.dt.float32
BF16 = mybir.dt.bfloat16
I32 = mybir.dt.int32
AF = mybir.ActivationFunctionType
ALU = mybir.AluOpType
AX = mybir.AxisListType

S = 4096
D = 1024
DEPTH = 4
NE = 32
DFF = 1024
EPS = 1e-6
NQ = 8
STRICT = True

CFG = {"layers": [0, 1, 2, 3], "mixer": True, "ffn": True, "ncores": 8}


class Res:
    __slots__ = ("w", "r")

    def __init__(self):
        self.w = None
        self.r = {}


class Builder:
    def __init__(self):
        nc = self.nc = bass.Bass("TRN2", target_bir_lowering=False)
        self.es = contextlib.ExitStack()
        self.streams = {"pe": nc.tensor, "act": nc.scalar, "dve": nc.vector, "pool": nc.gpsimd, "sp": nc.sync}
        self.csem = {}
        self.ccount = {}
        for e in ("pe", "act", "dve", "pool"):
            self.csem[e] = self.es.enter_context(nc.semaphore("c_" + e))
            self.ccount[e] = 0
        self.qsems = {}
        self.qcount = {}
        self.qissuer = {"sp": "sp", "pq": "pool"}
        for q in ("sp", "pq"):
            self.qsems[q] = [self.es.enter_context(nc.semaphore("q_%s%d" % (q, j))) for j in range(NQ)]
            self.qcount[q] = 0
        self.waited = {s: {} for s in self.streams}
        self.uid = 0

    def name(self, p):
        self.uid += 1
        return "%s_%d" % (p, self.uid)

    def sb(self, ctx, shape, dt, nm="t"):
        return ctx.enter_context(self.nc.sbuf_tensor(self.name(nm), list(shape), dt))

    def _wait(self, stream, tok):
        sem, val, owner = tok
        if owner == stream and (stream == "pe" or not STRICT):
            return
        w = self.waited[stream]
        if w.get(id(sem), 0) >= val:
            return
        self.streams[stream].wait_ge(sem, val)
        w[id(sem)] = val

    def _deps(self, stream, reads, writes):
        for r in reads:
            if r.w is not None:
                self._wait(stream, r.w)
        for r in writes:
            if r.w is not None:
                self._wait(stream, r.w)
            for t in r.r.values():
                self._wait(stream, t)

    def _mark(self, tok, reads, writes):
        for r in reads:
            r.r[id(tok[0])] = tok
        for r in writes:
            r.w = tok
            r.r = {}

    def op(self, eng, fn, reads=(), writes=()):
        self._deps(eng, reads, writes)
        ins = fn(self.streams[eng])
        self.ccount[eng] += 1
        tok = (self.csem[eng], self.ccount[eng], eng)
        ins.then_inc(tok[0], 1)
        self._mark(tok, reads, writes)

    def dma(self, q, out, in_, reads=(), writes=(), **kw):
        issuer = self.qissuer[q]
        n = self.qcount[q]
        sem = self.qsems[q][n % NQ]
        if n >= NQ:
            self._wait(issuer, (sem, 16 * (n // NQ), None))
        self._deps(issuer, reads, writes)
        eng = self.nc.sync if q == "sp" else self.nc.gpsimd
        ins = eng.dma_start(out=out, in_=in_, **kw)
        ins.then_inc(sem, 16)
        self.qcount[q] += 1
        tok = (sem, 16 * (n // NQ + 1), None)
        self._mark(tok, reads, writes)

    def barrier(self):
        toks = []
        for e in self.csem:
            if self.ccount[e] > 0:
                toks.append((self.csem[e], self.ccount[e], e))
        for q in self.qsems:
            n = self.qcount[q]
            for j in range(NQ):
                cnt = (n - j + NQ - 1) // NQ if n > j else 0
                if cnt > 0:
                    toks.append((self.qsems[q][j], 16 * cnt, None))
        for s in self.streams:
            for t in toks:
                self._wait(s, t)


def build(cfg):
    b = Builder()
    nc = b.nc
    es = b.es
    layers = cfg["layers"]

    def din(name, shape, dt=F32):
        return nc.dram_tensor(name, list(shape), dt, kind="ExternalInput").ap()

    def dscr(name, shape, dt=F32):
        return nc.dram_tensor(name, list(shape), dt, kind="Internal").ap()

    x_in = din("x", [S, D])
    c_in = din("c", [D])
    pos_in = din("positions", [S], I32)
    w_mod = din("w_mod", [DEPTH, D, 6 * D])
    b_mod = din("b_mod", [DEPTH, 6 * D])
    g_mix = din("g_norm_mix", [DEPTH, D])
    g_ffn = din("g_norm_ffn", [DEPTH, D])
    w_in_ab = din("w_in_ab", [2, D, 1984])
    mla_g_q = din("mla_g_q", [2, 384])
    mla_w_qb = din("mla_w_qb", [2, 384, 1536])
    mla_g_kv = din("mla_g_kv", [2, 256])
    mla_w_kvb = din("mla_w_kvb", [2, 256, 2048])
    swa_sink = din("swa_sink", [2, 16])
    w_out_ab = din("w_out_ab", [2, 2048, D])
    w_in_c = din("w_in_c", [2, D, 9216])
    w_out_c = din("w_out_c", [2, 1024, D])
    w_router = din("w_router", [DEPTH, D, NE])
    b_router = din("b_router", [DEPTH, NE])
    w_gu = din("w_gu", [DEPTH, NE, D, 2 * DFF])
    b_gu = din("b_gu", [DEPTH, NE, 2 * DFF])
    w_down = din("w_down", [DEPTH, NE, DFF, D])
    b_down = din("b_down", [DEPTH, NE, D])
    g_final = din("g_final", [D])
    k_identf = din("k_identf", [128, 128])
    k_inv = din("k_inv", [128, 1])
    k_masks = din("k_masks", [128, 4, 128])
    y_out = nc.dram_tensor("y", [S, D], F32, kind="ExternalOutput").ap()

    xT_d = dscr("xT_d", [8, 128, S])
    oT_d = dscr("oT_d", [16, 128, S], BF16)
    tab_d = dscr("tab_d", [2, 128, S])
    gt_d = dscr("gt_d", [NE, 2048])
    xT_v = xT_d.rearrange("c p t -> p c t")
    oT_v = oT_d.rearrange("c p t -> p c t")
    R_x = Res()
    R_o = Res()
    R_tab = Res()

    identf = b.sb(es, [128, 128], F32, "identf")
    identb = b.sb(es, [128, 128], BF16, "identb")
    meanm = b.sb(es, [128, 128], F32, "meanm")
    onesb = b.sb(es, [128, 128], BF16, "onesb")
    modc = b.sb(es, [128, DEPTH, 6, 8], F32, "modc")
    gcol = b.sb(es, [128, DEPTH, 2, 8], F32, "gcol")
    gfin = b.sb(es, [128, 8], F32, "gfin")
    epsc = b.sb(es, [128, 1], F32, "epsc")
    R_const = Res()

    banks = []
    for i in range(6):
        t = es.enter_context(nc.psum_tensor(b.name("psf"), [128, 512], F32))
        banks.append((t, Res()))
    bbanks = []
    for i in range(2):
        t = es.enter_context(nc.psum_tensor(b.name("psb"), [128, 1024], BF16))
        bbanks.append((t, Res()))
    rr = {"f": 0, "b": 0}

    def bank():
        rr["f"] = (rr["f"] + 1) % 6
        return banks[rr["f"]]

    def bbank():
        rr["b"] = (rr["b"] + 1) % 2
        return bbanks[rr["b"]]

    def col_view(vec_ap, n):
        return vec_ap.rearrange("(j p) -> p j", p=128)

    b.dma("sp", identf[:], k_identf[:, :], writes=[R_const])
    b.op("dve", lambda e: e.tensor_copy(out=identb[:], in_=identf[:]), reads=[R_const], writes=[R_const])
    b.op("pool", lambda e: e.memset(meanm[:], 1.0 / D), writes=[R_const])
    b.op("pool", lambda e: e.memset(onesb[:], 1.0), writes=[R_const])
    b.op("pool", lambda e: e.memset(epsc[:], EPS), writes=[R_const])
    b.dma("sp", gfin[:], col_view(g_final, 8), writes=[R_const], allow_slow_non_contiguous=True)
    for l in layers:
        b.dma("sp", gcol[:, l, 0, :], col_view(g_mix[l], 8), writes=[R_const], allow_slow_non_contiguous=True)
        b.dma("sp", gcol[:, l, 1, :], col_view(g_ffn[l], 8), writes=[R_const], allow_slow_non_contiguous=True)

    with contextlib.ExitStack() as ps:
        cT = b.sb(ps, [128, 8], F32, "cT")
        cS = b.sb(ps, [128, 8], F32, "cS")
        bmc = b.sb(ps, [128, DEPTH, 48], F32, "bmc")
        wm = [b.sb(ps, [128, 8, 1024], F32, "wm%d" % i) for i in range(2)]
        R_wm = [Res(), Res()]
        R_c = Res()
        b.dma("sp", cT[:], col_view(c_in, 8), writes=[R_c], allow_slow_non_contiguous=True)
        for l in layers:
            b.dma("sp", bmc[:, l, :], col_view(b_mod[l], 48), writes=[R_c], allow_slow_non_contiguous=True)
        b.op("act", lambda e: e.activation(out=cS[:], in_=cT[:], func=AF.Sigmoid), reads=[R_c], writes=[R_c])
        b.op("dve", lambda e: e.tensor_tensor(out=cS[:], in0=cS[:], in1=cT[:], op=ALU.mult), reads=[R_c], writes=[R_c])
        k = 0
        for l in layers:
            for v in range(6):
                slot = k % 2
                k += 1
                b.dma("sp", wm[slot][:], w_mod[l].rearrange("(dc p) f -> p dc f", p=128)[:, :, v * 1024:(v + 1) * 1024],
                      writes=[R_wm[slot]])
                pt, pr = bank()
                for j in range(8):
                    for dc in range(8):
                        b.op("pe", lambda e, j=j, dc=dc: e.matmul(pt[:, j:j + 1], lhsT=wm[slot][:, dc, j * 128:(j + 1) * 128],
                                                                   rhs=cS[:, dc:dc + 1], start=(dc == 0), stop=(dc == 7)),
                             reads=[R_wm[slot], R_c], writes=[pr])
                b.op("dve", lambda e: e.tensor_tensor(out=modc[:, l, v, :], in0=pt[:, 0:8], in1=bmc[:, l, v * 8:(v + 1) * 8], op=ALU.add),
                     reads=[pr, R_c], writes=[R_const])
        for l in layers:
            for (v, gi) in ((1, 0), (4, 1)):
                b.op("dve", lambda e, l=l, v=v, gi=gi: e.scalar_tensor_tensor(out=modc[:, l, v, :], in0=modc[:, l, v, :], scalar=1.0,
                                                                              in1=gcol[:, l, gi, :], op0=ALU.add, op1=ALU.mult),
                     reads=[R_const], writes=[R_const])
        b.barrier()

    with contextlib.ExitStack() as ps:
        xt = [b.sb(ps, [128, D], F32, "xt%d" % i) for i in range(2)]
        xo = [b.sb(ps, [128, 8, 128], F32, "xo%d" % i) for i in range(2)]
        R_xt = [Res(), Res()]
        R_xo = [Res(), Res()]
        for i in range(32):
            s = i % 2
            b.dma("sp", xt[s][:], x_in[i * 128:(i + 1) * 128, :], writes=[R_xt[s]])
            for hb in range(2):
                pt, pr = bank()
                for cc in range(4):
                    c = hb * 4 + cc
                    b.op("pe", lambda e, c=c, cc=cc: e.transpose(out=pt[:, cc * 128:(cc + 1) * 128], in_=xt[s][:, c * 128:(c + 1) * 128],
                                                                 identity=identf[:]),
                         reads=[R_xt[s]], writes=[pr])
                eng = "dve" if hb == 0 else "act"
                if eng == "dve":
                    b.op("dve", lambda e: e.tensor_copy(out=xo[s][:, hb * 4:(hb + 1) * 4, :],
                                                        in_=pt[:, :].rearrange("p (c t) -> p c t", c=4)),
                         reads=[pr], writes=[R_xo[s]])
                else:
                    b.op("act", lambda e: e.copy(out=xo[s][:, hb * 4:(hb + 1) * 4, :],
                                                 in_=pt[:, :].rearrange("p (c t) -> p c t", c=4)),
                         reads=[pr], writes=[R_xo[s]])
            b.dma("sp", xT_v[:, :, i * 128:(i + 1) * 128], xo[s][:], reads=[R_xo[s]], writes=[R_x])
        b.barrier()

    def norm_chunk(ctx_bufs, l, which, t0, hT, hcol0, R_h):
        xc, R_xc, sq, R_sq, rs, R_rs, tmp, R_tmp = ctx_bufs[(t0 // 512) % 2]
        va = 1 if which == 0 else 4
        vb = 0 if which == 0 else 3
        b.dma("sp", xc[:], xT_v[:, :, t0:t0 + 512], reads=[R_x], writes=[R_xc])
        b.op("act", lambda e: e.activation(out=sq[:], in_=xc[:], func=AF.Square), reads=[R_xc], writes=[R_sq])
        pt, pr = bank()
        for dc in range(8):
            b.op("pe", lambda e, dc=dc: e.matmul(pt[:, :], lhsT=meanm[:], rhs=sq[:, dc, :], start=(dc == 0), stop=(dc == 7)),
                 reads=[R_sq], writes=[pr])
        b.op("act", lambda e: e.activation(out=rs[:], in_=pt[:, :], func=AF.Sqrt, bias=epsc[:], scale=1.0),
             reads=[pr], writes=[R_rs])
        b.op("dve", lambda e: e.reciprocal(out=rs[:], in_=rs[:]), reads=[R_rs], writes=[R_rs])
        for dc in range(8):
            b.op("dve", lambda e, dc=dc: e.scalar_tensor_tensor(out=tmp[:, dc, :], in0=xc[:, dc, :], scalar=modc[:, l, va, dc:dc + 1],
                                                                in1=rs[:], op0=ALU.mult, op1=ALU.mult),
                 reads=[R_xc, R_rs], writes=[R_tmp])
            b.op("act", lambda e, dc=dc: e.activation(out=hT[:, dc, hcol0:hcol0 + 512], in_=tmp[:, dc, :], func=AF.Identity,
                                                      bias=modc[:, l, vb, dc:dc + 1], scale=1.0),
                 reads=[R_tmp], writes=[R_h])

    def norm_bufs(ps):
        sets = []
        for i in range(2):
            xc = b.sb(ps, [128, 8, 512], F32, "xc%d" % i)
            sq = b.sb(ps, [128, 8, 512], F32, "sq%d" % i)
            rs = b.sb(ps, [128, 512], F32, "rs%d" % i)
            R_sq = Res()
            sets.append((xc, Res(), sq, R_sq, rs, Res(), sq, R_sq))
        return sets

    def moe_layer(l):
        for hf in range(2):
            T0 = hf * 2048
            with contextlib.ExitStack() as hs:
                hT = b.sb(hs, [128, 8, 2048], BF16, "hT")
                acc = b.sb(hs, [128, 8, 2048], F32, "acc")
                R_h = Res()
                R_G = Res()
                R_acc = [[Res() for _ in range(4)] for _ in range(8)]
                bgc = b.sb(hs, [128, 16, NE], F32, "bgc")
                R_b = Res()
                R_gt = Res()
                with contextlib.ExitStack() as ps:
                    nb = norm_bufs(ps)
                    wrf = b.sb(ps, [128, 8, NE], F32, "wrf")
                    wrb = b.sb(ps, [128, 8, NE], BF16, "wrb")
                    brb = b.sb(ps, [128, NE], F32, "brb")
                    R_wr = Res()
                    lg2 = [b.sb(ps, [128, NE], F32, "lg%d" % i_) for i_ in range(2)]
                    t82 = [b.sb(ps, [128, 8], F32, "t8%d" % i_) for i_ in range(2)]
                    ng2 = [b.sb(ps, [128, 1], F32, "ng%d" % i_) for i_ in range(2)]
                    ee2 = [b.sb(ps, [128, NE], F32, "ee%d" % i_) for i_ in range(2)]
                    mk2 = [b.sb(ps, [128, NE], F32, "mk%d" % i_) for i_ in range(2)]
                    sm2 = [b.sb(ps, [128, 1], F32, "sm%d" % i_) for i_ in range(2)]
                    R_r2 = [Res(), Res()]
                    bgr = b.sb(ps, [32, 2048], F32, "bgr")
                    GT = b.sb(ps, [32, 2048], F32, "GT")
                    bdn = b.sb(ps, [32, D], F32, "bdn")
                    b.dma("sp", bgr[:], b_gu[l], writes=[R_b])
                    b.dma("sp", bdn[:], b_down[l], writes=[R_b])
                    pt, pr = bank()
                    for c in range(16):
                        b.op("pe", lambda e, c=c: e.transpose(out=pt[:, c * NE:(c + 1) * NE], in_=bgr[:, c * 128:(c + 1) * 128],
                                                              identity=identf[0:32, 0:32]),
                             reads=[R_b], writes=[pr])
                    b.op("dve", lambda e: e.tensor_copy(out=bgc[:], in_=pt[:, :].rearrange("p (c e) -> p c e", c=16)),
                         reads=[pr], writes=[R_b])
                    b.op("dve", lambda e: e.tensor_scalar(out=bgc[:, 8:16, :], in0=bgc[:, 8:16, :], scalar1=1.0, scalar2=None, op0=ALU.add),
                         reads=[R_b], writes=[R_b])
                    b.dma("sp", wrf[:], w_router[l].rearrange("(dc p) e -> p dc e", p=128), writes=[R_wr])
                    b.dma("sp", brb[:], b_router[l].partition_broadcast(128), writes=[R_wr])
                    b.op("dve", lambda e: e.tensor_copy(out=wrb[:], in_=wrf[:]), reads=[R_wr], writes=[R_wr])
                    for tc in range(4):
                        norm_chunk(nb, l, 1, T0 + tc * 512, hT, tc * 512, R_h)
                    for i in range(16):
                        lg, t8, ng, ee, mk, sm, R_r = lg2[i % 2], t82[i % 2], ng2[i % 2], ee2[i % 2], mk2[i % 2], sm2[i % 2], R_r2[i % 2]
                        pt, pr = bank()
                        for dc in range(8):
                            b.op("pe", lambda e, dc=dc: e.matmul(pt[:, 0:NE], lhsT=hT[:, dc, i * 128:(i + 1) * 128], rhs=wrb[:, dc, :],
                                                                 start=(dc == 0), stop=(dc == 7)),
                                 reads=[R_h, R_wr], writes=[pr])
                        b.op("dve", lambda e: e.tensor_tensor(out=lg[:], in0=pt[:, 0:NE], in1=brb[:], op=ALU.add),
                             reads=[pr, R_wr], writes=[R_r])
                        b.op("dve", lambda e: e.max(out=t8[:], in_=lg[:]), reads=[R_r], writes=[R_r])
                        b.op("dve", lambda e: e.tensor_scalar(out=ng[:], in0=t8[:, 0:1], scalar1=-1.0, scalar2=None, op0=ALU.mult),
                             reads=[R_r], writes=[R_r])
                        b.op("act", lambda e: e.activation(out=ee[:], in_=lg[:], func=AF.Exp, bias=ng[:], scale=1.0),
                             reads=[R_r], writes=[R_r])
                        b.op("dve", lambda e: e.tensor_scalar(out=mk[:], in0=lg[:], scalar1=t8[:, 3:4], scalar2=None, op0=ALU.is_ge),
                             reads=[R_r], writes=[R_r])
                        b.op("dve", lambda e: e.tensor_tensor(out=ee[:], in0=ee[:], in1=mk[:], op=ALU.mult), reads=[R_r], writes=[R_r])
                        b.op("dve", lambda e: e.reduce_sum(out=sm[:], in_=ee[:], axis=AX.X), reads=[R_r], writes=[R_r])
                        b.op("dve", lambda e: e.reciprocal(out=sm[:], in_=sm[:]), reads=[R_r], writes=[R_r])
                        b.op("dve", lambda e: e.tensor_scalar(out=ee[:], in0=ee[:], scalar1=sm[:, 0:1], scalar2=None, op0=ALU.mult),
                             reads=[R_r], writes=[R_r])
                        pt2, pr2 = bank()
                        b.op("pe", lambda e: e.transpose(out=pt2[0:NE, 0:128], in_=ee[:], identity=identf[:]), reads=[R_r], writes=[pr2])
                        b.op("act", lambda e: e.copy(out=GT[:, i * 128:(i + 1) * 128], in_=pt2[0:NE, 0:128]), reads=[pr2], writes=[R_G])
                    b.dma("sp", gt_d[:, :], GT[:], reads=[R_G], writes=[R_gt])
                    for dc in range(8):
                        for tc in range(4):
                            pt, pr = bank()
                            b.op("pe", lambda e, dc=dc, tc=tc: e.matmul(pt[:, :], lhsT=bdn[:, dc * 128:(dc + 1) * 128],
                                                                        rhs=GT[:, tc * 512:(tc + 1) * 512], start=True, stop=True),
                                 reads=[R_b, R_G], writes=[pr])
                            b.op("act", lambda e, dc=dc, tc=tc: e.copy(out=acc[:, dc, tc * 512:(tc + 1) * 512], in_=pt[:, :]),
                                 reads=[pr], writes=[R_acc[dc][tc]])
                    b.barrier()
                with contextlib.ExitStack() as ps:
                    actT = b.sb(ps, [128, 8, 2048], BF16, "actT")
                    R_act = [[Res() for _ in range(4)] for _ in range(8)]
                    NST = 4
                    stg = [b.sb(ps, [128, 8, 256], F32, "stg%d" % i) for i in range(NST)]
                    wbf = [b.sb(ps, [128, 8, 256], BF16, "wbf%d" % i) for i in range(NST)]
                    R_stg = [Res() for _ in range(NST)]
                    R_wbf = [Res() for _ in range(NST)]
                    tt = [b.sb(ps, [128, 512], F32, "tt%d" % i) for i in range(2)]
                    sg = [b.sb(ps, [128, 512], F32, "sg%d" % i) for i in range(2)]
                    uu = [b.sb(ps, [128, 512], F32, "uu%d" % i) for i in range(2)]
                    R_tt = [Res(), Res()]
                    R_sg = [Res(), Res()]
                    R_uu = [Res(), Res()]
                    gbc = b.sb(ps, [128, 2048], F32, "gbc")
                    R_gbc = [Res() for _ in range(4)]
                    steps = []
                    for ex in range(NE):
                        wg = w_gu[l, ex].rearrange("(dc p) f -> p dc f", p=128)
                        wd = w_down[l, ex].rearrange("(fc p) d -> p fc d", p=128)
                        for q2 in range(4):
                            steps.append((ex, "gu", q2, [wg[:, :, q2 * 256:(q2 + 1) * 256], wg[:, :, DFF + q2 * 256:DFF + (q2 + 1) * 256]]))
                        for r2 in range(2):
                            steps.append((ex, "dn", r2, [wd[:, :, (2 * r2) * 256:(2 * r2 + 1) * 256], wd[:, :, (2 * r2 + 1) * 256:(2 * r2 + 2) * 256]]))
                    NS = len(steps)

                    def slots_of(i):
                        return (0, 1) if i % 2 == 0 else (2, 3)

                    def load_dma(i):
                        if i >= NS:
                            return
                        for s_, src in zip(slots_of(i), steps[i][3]):
                            b.dma("sp", stg[s_][:], src, writes=[R_stg[s_]])

                    def load_cast(i):
                        if i >= NS:
                            return
                        for s_ in slots_of(i):
                            b.op("act", lambda e: e.copy(out=wbf[s_][:], in_=stg[s_][:]), reads=[R_stg[s_]], writes=[R_wbf[s_]])

                    def load_gate(ex):
                        for tc in range(4):
                            b.dma("sp", gbc[:, tc * 512:(tc + 1) * 512], gt_d[ex, tc * 512:(tc + 1) * 512].partition_broadcast(128),
                                  reads=[R_gt], writes=[R_gbc[tc]])

                    cnt = [0]

                    def gu_unit(ex, q, tc, sA, sB, sub):
                        pa, pra = bank()
                        pb, prb = bank()
                        for dc in range(8):
                            b.op("pe", lambda e, dc=dc: e.matmul(pa[:, :], lhsT=wbf[sA][:, dc, sub * 128:(sub + 1) * 128],
                                                                 rhs=hT[:, dc, tc * 512:(tc + 1) * 512], start=(dc == 0), stop=(dc == 7)),
                                 reads=[R_wbf[sA], R_h], writes=[pra])
                        for dc in range(8):
                            b.op("pe", lambda e, dc=dc: e.matmul(pb[:, :], lhsT=wbf[sB][:, dc, sub * 128:(sub + 1) * 128],
                                                                 rhs=hT[:, dc, tc * 512:(tc + 1) * 512], start=(dc == 0), stop=(dc == 7)),
                                 reads=[R_wbf[sB], R_h], writes=[prb])
                        i2 = cnt[0] % 2
                        cnt[0] += 1
                        b.op("dve", lambda e: e.tensor_scalar(out=tt[i2][:], in0=pa[:, :], scalar1=bgc[:, q, ex:ex + 1], scalar2=7.0,
                                                              op0=ALU.add, op1=ALU.min),
                             reads=[pra, R_b], writes=[R_tt[i2]])
                        b.op("act", lambda e: e.activation(out=uu[i2][:], in_=pb[:, :], func=AF.Identity,
                                                           bias=bgc[:, 8 + q, ex:ex + 1], scale=1.0),
                             reads=[prb, R_b], writes=[R_uu[i2]])
                        b.op("act", lambda e: e.activation(out=sg[i2][:], in_=tt[i2][:], func=AF.Sigmoid, scale=1.702),
                             reads=[R_tt[i2]], writes=[R_sg[i2]])
                        b.op("pool", lambda e: e.tensor_scalar(out=uu[i2][:], in0=uu[i2][:], scalar1=8.0, scalar2=-6.0,
                                                               op0=ALU.min, op1=ALU.max),
                             reads=[R_uu[i2]], writes=[R_uu[i2]])
                        b.op("pool", lambda e: e.tensor_tensor(out=uu[i2][:], in0=uu[i2][:], in1=gbc[:, tc * 512:(tc + 1) * 512],
                                                               op=ALU.mult),
                             reads=[R_uu[i2], R_gbc[tc]], writes=[R_uu[i2]])
                        b.op("dve", lambda e: e.tensor_tensor(out=tt[i2][:], in0=tt[i2][:], in1=sg[i2][:], op=ALU.mult),
                             reads=[R_tt[i2], R_sg[i2]], writes=[R_tt[i2]])
                        b.op("dve", lambda e: e.tensor_tensor(out=actT[:, q, tc * 512:(tc + 1) * 512], in0=tt[i2][:], in1=uu[i2][:],
                                                              op=ALU.mult),
                             reads=[R_tt[i2], R_uu[i2]], writes=[R_act[q][tc]])

                    def dn_unit(s_, dc, ds, tc):
                        pt, pr = bank()
                        for fc in range(8):
                            b.op("pe", lambda e, fc=fc: e.matmul(pt[:, :], lhsT=wbf[s_][:, fc, ds * 128:(ds + 1) * 128],
                                                                 rhs=actT[:, fc, tc * 512:(tc + 1) * 512],
                                                                 start=(fc == 0), stop=(fc == 7)),
                                 reads=[R_wbf[s_], R_act[fc][tc]], writes=[pr])
                        b.op("dve", lambda e: e.tensor_tensor(out=acc[:, dc, tc * 512:(tc + 1) * 512], in0=pt[:, :],
                                                              in1=acc[:, dc, tc * 512:(tc + 1) * 512], op=ALU.add),
                             reads=[pr, R_acc[dc][tc]], writes=[R_acc[dc][tc]])

                    load_gate(0)
                    load_dma(0)
                    load_dma(1)
                    load_cast(0)
                    for i in range(NS):
                        ex, kind, idx, _ = steps[i]
                        load_dma(i + 2)
                        sl = slots_of(i)
                        if kind == "gu":
                            units = [(idx * 2 + sub, tc, sub) for sub in range(2) for tc in range(4)]
                            for ui, (q, tc, sub) in enumerate(units):
                                if ui == 4:
                                    load_cast(i + 1)
                                gu_unit(ex, q, tc, sl[0], sl[1], sub)
                        else:
                            if idx == 0 and ex + 1 < NE:
                                load_gate(ex + 1)
                            units = [(sl[hh], (idx * 2 + hh) * 2 + ds, ds, tc) for hh in range(2) for tc in range(4) for ds in range(2)]
                            for ui, (s_, dc, ds, tc) in enumerate(units):
                                if ui == 8:
                                    load_cast(i + 1)
                                dn_unit(s_, dc, ds, tc)
                    b.barrier()
                with contextlib.ExitStack() as ps:
                    xc = [b.sb(ps, [128, 8, 512], F32, "xr%d" % i) for i in range(2)]
                    R_xc = [Res(), Res()]
                    for tc in range(4):
                        s = tc % 2
                        t0 = T0 + tc * 512
                        b.dma("sp", xc[s][:], xT_v[:, :, t0:t0 + 512], reads=[R_x], writes=[R_xc[s]])
                        for dc in range(8):
                            b.op("dve", lambda e, dc=dc: e.scalar_tensor_tensor(out=xc[s][:, dc, :], in0=acc[:, dc, tc * 512:(tc + 1) * 512],
                                                                                scalar=modc[:, l, 5, dc:dc + 1], in1=xc[s][:, dc, :],
                                                                                op0=ALU.mult, op1=ALU.add),
                                 reads=[R_xc[s]], writes=[R_xc[s]])
                        b.dma("sp", xT_v[:, :, t0:t0 + 512], xc[s][:], reads=[R_xc[s]], writes=[R_x])
                    b.barrier()

    maskb = b.sb(es, [128, 4, 128], BF16, "maskb")
    invc = b.sb(es, [128, 1], F32, "invc")
    pic = b.sb(es, [128, 1], F32, "pic")
    with contextlib.ExitStack() as ps:
        mkf = b.sb(ps, [128, 4, 128], F32, "mkf")
        posi = b.sb(ps, [128, S], I32, "posi")
        posf = b.sb(ps, [128, S], F32, "posf")
        ang = b.sb(ps, [128, S], F32, "ang")
        R_t = Res()
        b.dma("sp", mkf[:], k_masks[:, :, :], writes=[R_t])
        b.dma("sp", invc[:], k_inv[:, :], writes=[R_t])
        b.dma("sp", posi[:], pos_in.partition_broadcast(128), writes=[R_t])
        b.op("pool", lambda e: e.memset(pic[:], float(np.pi / 2)), writes=[R_t])
        b.op("dve", lambda e: e.tensor_copy(out=maskb[:], in_=mkf[:]), reads=[R_t], writes=[R_t])
        b.op("dve", lambda e: e.tensor_copy(out=posf[:], in_=posi[:]), reads=[R_t], writes=[R_t])
        b.op("dve", lambda e: e.tensor_scalar(out=posf[:], in0=posf[:], scalar1=invc[:, 0:1], scalar2=None, op0=ALU.mult),
             reads=[R_t], writes=[R_t])
        C1 = 6.28125
        C2 = float(np.float32(2 * np.pi - 6.28125))
        C3 = float(2 * np.pi - 6.28125 - np.float64(np.float32(2 * np.pi - 6.28125)))
        kf = posi[:, :].bitcast(F32)
        b.op("dve", lambda e: e.tensor_scalar(out=ang[:], in0=posf[:], scalar1=float(1 / (2 * np.pi)), scalar2=None, op0=ALU.mult),
             reads=[R_t], writes=[R_t])
        b.op("dve", lambda e: e.tensor_copy(out=posi[:], in_=ang[:]), reads=[R_t], writes=[R_t])
        b.op("dve", lambda e: e.tensor_copy(out=ang[:], in_=posi[:]), reads=[R_t], writes=[R_t])
        for cc in (C1, C2, C3):
            b.op("dve", lambda e: e.scalar_tensor_tensor(out=posf[:], in0=ang[:], scalar=-cc, in1=posf[:], op0=ALU.mult, op1=ALU.add),
                 reads=[R_t], writes=[R_t])
        b.op("dve", lambda e: e.tensor_scalar(out=ang[:], in0=posf[:], scalar1=float(np.pi), scalar2=float(-2 * np.pi), op0=ALU.is_gt, op1=ALU.mult),
             reads=[R_t], writes=[R_t])
        b.op("dve", lambda e: e.tensor_tensor(out=posf[:], in0=posf[:], in1=ang[:], op=ALU.add), reads=[R_t], writes=[R_t])
        b.op("dve", lambda e: e.tensor_scalar(out=ang[:], in0=posf[:], scalar1=float(-np.pi), scalar2=float(2 * np.pi), op0=ALU.is_lt, op1=ALU.mult),
             reads=[R_t], writes=[R_t])
        b.op("dve", lambda e: e.tensor_tensor(out=posf[:], in0=posf[:], in1=ang[:], op=ALU.add), reads=[R_t], writes=[R_t])
        b.op("dve", lambda e: e.tensor_scalar(out=posf[:], in0=posf[:], scalar1=3.14159, scalar2=-3.14159, op0=ALU.min, op1=ALU.max),
             reads=[R_t], writes=[R_t])
        b.op("dve", lambda e: e.scalar_tensor_tensor(out=ang[:], in0=posf[:], scalar=-1.0, in1=posf[:], op0=ALU.mult, op1=ALU.max),
             reads=[R_t], writes=[R_t])
        b.op("act", lambda e: e.activation(out=ang[:], in_=ang[:], func=AF.Sin, bias=pic[:], scale=-1.0), reads=[R_t], writes=[R_t])
        b.op("act", lambda e: e.activation(out=posf[:], in_=posf[:], func=AF.Sin), reads=[R_t], writes=[R_t])
        b.dma("sp", tab_d[0], ang[:], reads=[R_t], writes=[R_tab])
        b.dma("sp", tab_d[1], posf[:], reads=[R_t], writes=[R_tab])
        b.barrier()

    def fbank(lst, st):
        st[0] = (st[0] + 1) % len(lst)
        return banks[lst[st[0]]]

    def rot_weights(dst, src, nblk, R_w):
        sv = src.rearrange("p (k h i) -> p k h i", h=2, i=32)
        dv = dst.rearrange("p (k h i) -> p k h i", h=2, i=32)
        b.op("dve", lambda e: e.tensor_scalar(out=dv[:, :, 0, :], in0=sv[:, :, 1, :], scalar1=-1.0, scalar2=None, op0=ALU.mult),
             reads=[R_w], writes=[R_w])
        b.op("dve", lambda e: e.tensor_copy(out=dv[:, :, 1, :], in_=sv[:, :, 0, :]), reads=[R_w], writes=[R_w])

    def rope_evac(out_ap, pa, pb_, C_ap, S_ap, t1, t2, R_tmp, reads, writes, npart=128, perm=None):
        b.op("dve", lambda e: e.tensor_tensor(out=t1[0:npart, :], in0=pa, in1=C_ap, op=ALU.mult), reads=reads, writes=[R_tmp])
        b.op("dve", lambda e: e.tensor_tensor(out=t2[0:npart, :], in0=pb_, in1=S_ap, op=ALU.mult), reads=reads, writes=[R_tmp])
        a1 = t1[0:npart, :]
        a2 = t2[0:npart, :]
        if perm is not None:
            a1 = a1.rearrange("p (m r) -> p r m", r=perm)
            a2 = a2.rearrange("p (m r) -> p r m", r=perm)
        b.op("pool", lambda e: e.tensor_tensor(out=out_ap, in0=a1, in1=a2, op=ALU.add), reads=[R_tmp], writes=writes)

    def load_tables(ps):
        Ct = b.sb(ps, [128, S], F32, "Ct")
        St = b.sb(ps, [128, S], F32, "St")
        R_T = Res()
        b.dma("sp", Ct[:], tab_d[0], reads=[R_tab], writes=[R_T])
        b.dma("sp", St[:], tab_d[1], reads=[R_tab], writes=[R_T])
        return Ct, St, R_T

    def norm_all(l, which, hT, R_h):
        with contextlib.ExitStack() as ps:
            nb = norm_bufs(ps)
            for tc in range(8):
                norm_chunk(nb, l, which, tc * 512, hT, tc * 512, R_h)
            b.barrier()

    def out_proj(l, w_out_l, nfc):
        with contextlib.ExitStack() as ps:
            wo = b.sb(ps, [128, nfc, D], BF16, "wo")
            R_wo = Res()
            ot = [b.sb(ps, [128, nfc, 512], BF16, "ot%d" % i) for i in range(2)]
            xc = [b.sb(ps, [128, 8, 512], F32, "ox%d" % i) for i in range(2)]
            R_ot = [Res(), Res()]
            R_xc = [Res(), Res()]
            wv_ = w_out_l.rearrange("(fc p) d -> p fc d", p=128)
            for i in range(nfc // 4):
                b.dma("pq", wo[:, i * 4:(i + 1) * 4, :], wv_[:, i * 4:(i + 1) * 4, :], writes=[R_wo])
            for tc in range(8):
                s = tc % 2
                t0 = tc * 512
                b.dma("sp", ot[s][:], oT_v[:, 0:nfc, t0:t0 + 512], reads=[R_o], writes=[R_ot[s]])
                b.dma("sp", xc[s][:], xT_v[:, :, t0:t0 + 512], reads=[R_x], writes=[R_xc[s]])
                for dc in range(8):
                    pt, pr = bank()
                    for fc in range(nfc):
                        b.op("pe", lambda e, fc=fc: e.matmul(pt[:, :], lhsT=wo[:, fc, dc * 128:(dc + 1) * 128], rhs=ot[s][:, fc, :],
                                                             start=(fc == 0), stop=(fc == nfc - 1)),
                             reads=[R_wo, R_ot[s]], writes=[pr])
                    b.op("dve", lambda e: e.scalar_tensor_tensor(out=xc[s][:, dc, :], in0=pt[:, :], scalar=modc[:, l, 2, dc:dc + 1],
                                                                 in1=xc[s][:, dc, :], op0=ALU.mult, op1=ALU.add),
                         reads=[pr, R_xc[s]], writes=[R_xc[s]])
                b.dma("sp", xT_v[:, :, t0:t0 + 512], xc[s][:], reads=[R_xc[s]], writes=[R_x])
            b.barrier()

    mla_d = dscr("mla_d", [6, 128, S], BF16)
    mla_v = mla_d.rearrange("c p t -> p c t")
    R_mla = Res()

    def mixer_even(l):
        li = l // 2
        win = w_in_ab[li].rearrange("(dc p) f -> p dc f", p=128)
        with contextlib.ExitStack() as hs:
            hT = b.sb(hs, [128, 8, S], BF16, "hTe")
            R_h = Res()
            norm_all(l, 0, hT, R_h)
            with contextlib.ExitStack() as ps:
                Ct, St, R_T = load_tables(ps)
                wA = b.sb(ps, [128, 8, 704], BF16, "wA")
                wKr = b.sb(ps, [128, 8, 64], BF16, "wKr")
                R_w = Res()
                gq = b.sb(ps, [128, 3], F32, "gq")
                gkv = b.sb(ps, [128, 2], F32, "gkv")
                b.dma("pq", wA[:], win[:, :, 0:704], writes=[R_w])
                b.dma("sp", gq[:], col_view(mla_g_q[li], 3), writes=[R_w], allow_slow_non_contiguous=True)
                b.dma("sp", gkv[:], col_view(mla_g_kv[li], 2), writes=[R_w], allow_slow_non_contiguous=True)
                for dc in range(8):
                    rot_weights(wKr[:, dc, :], wA[:, dc, 640:704], 1, R_w)
                sqb = [b.sb(ps, [128, 3, 512], BF16, "sqb%d" % i) for i in range(2)]
                R_sqb = [Res(), Res()]
                rsq = [b.sb(ps, [128, 512], F32, "rsq%d" % i) for i in range(2)]
                R_rsq = [Res(), Res()]
                ob = [b.sb(ps, [128, 6, 512], BF16, "mob%d" % i) for i in range(2)]
                R_ob = [Res(), Res()]
                t1 = b.sb(ps, [128, 512], F32, "t1")
                t2 = b.sb(ps, [128, 512], F32, "t2")
                R_tmp = Res()
                kk = 0
                for tc in range(8):
                    t0 = tc * 512
                    so = tc % 2
                    for (c0, nch, gcolv, oc0) in ((0, 3, gq, 0), (384, 2, gkv, 3)):
                        s2 = kk % 2
                        kk += 1
                        pcs = []
                        for c in range(nch):
                            pt, pr = bank()
                            for dc in range(8):
                                b.op("pe", lambda e, dc=dc: e.matmul(pt[:, :], lhsT=wA[:, dc, c0 + c * 128:c0 + (c + 1) * 128], rhs=hT[:, dc, t0:t0 + 512],
                                                                     start=(dc == 0), stop=(dc == 7)),
                                     reads=[R_w, R_h], writes=[pr])
                            b.op("act", lambda e: e.activation(out=sqb[s2][:, c, :], in_=pt[:, :], func=AF.Square), reads=[pr], writes=[R_sqb[s2]])
                            pcs.append((pt, pr))
                        pss, prs = bank()
                        for c in range(nch):
                            b.op("pe", lambda e: e.matmul(pss[:, :], lhsT=onesb[:], rhs=sqb[s2][:, c, :], start=(c == 0), stop=(c == nch - 1)),
                                 reads=[R_sqb[s2]], writes=[prs])
                        b.op("act", lambda e: e.activation(out=rsq[s2][:], in_=pss[:, :], func=AF.Sqrt, bias=epsc[:], scale=1.0 / (nch * 128)),
                             reads=[prs], writes=[R_rsq[s2]])
                        b.op("dve", lambda e: e.reciprocal(out=rsq[s2][:], in_=rsq[s2][:]), reads=[R_rsq[s2]], writes=[R_rsq[s2]])
                        for c in range(nch):
                            pt, pr = pcs[c]
                            b.op("dve", lambda e: e.scalar_tensor_tensor(out=ob[so][:, oc0 + c, :], in0=pt[:, :], scalar=gcolv[:, c:c + 1],
                                                                         in1=rsq[s2][:], op0=ALU.mult, op1=ALU.mult),
                                 reads=[pr, R_rsq[s2], R_w], writes=[R_ob[so]])
                    pa, pra = bank()
                    pb_, prb = bank()
                    for dc in range(8):
                        b.op("pe", lambda e, dc=dc: e.matmul(pa[0:64, :], lhsT=wA[:, dc, 640:704], rhs=hT[:, dc, t0:t0 + 512],
                                                             start=(dc == 0), stop=(dc == 7)), reads=[R_w, R_h], writes=[pra])
                    for dc in range(8):
                        b.op("pe", lambda e, dc=dc: e.matmul(pb_[0:64, :], lhsT=wKr[:, dc, :], rhs=hT[:, dc, t0:t0 + 512],
                                                             start=(dc == 0), stop=(dc == 7)), reads=[R_w, R_h], writes=[prb])
                    rope_evac(ob[so][0:64, 5, :], pa[0:64, :], pb_[0:64, :], Ct[0:64, t0:t0 + 512], St[0:64, t0:t0 + 512], t1, t2, R_tmp,
                              [pra, prb, R_T], [R_ob[so]], npart=64)
                    b.dma("sp", mla_v[:, 0:5, t0:t0 + 512], ob[so][:, 0:5, :], reads=[R_ob[so]], writes=[R_mla])
                    b.dma("sp", mla_v[0:64, 5, t0:t0 + 512], ob[so][0:64, 5, :], reads=[R_ob[so]], writes=[R_mla])
                b.barrier()
            with contextlib.ExitStack() as ps:
                Ct, St, R_T = load_tables(ps)
                kT = b.sb(ps, [128, 2, S], BF16, "kTb")
                vd = b.sb(ps, [128, 2, 32, 128], BF16, "vdb")
                R_k = Res()
                R_v = Res()
                wk = b.sb(ps, [128, 8, 256], BF16, "wkd")
                wkr = b.sb(ps, [128, 8, 256], BF16, "wkdr")
                wv = b.sb(ps, [128, 8, 256], BF16, "wvd")
                R_w = Res()
                es_ = b.sb(ps, [128, 16], F32, "esink")
                b.dma("sp", es_[:], swa_sink[li].partition_broadcast(128), writes=[R_w])
                b.op("act", lambda e: e.activation(out=es_[:], in_=es_[:], func=AF.Exp), reads=[R_w], writes=[R_w])
                for g in range(2):
                    for dup in range(2):
                        o0 = g * 128 + dup * 64
                        b.dma("pq", wk[:, :, o0:o0 + 64], win[:, :, 1728 + g * 64:1728 + (g + 1) * 64], writes=[R_w])
                        b.dma("pq", wv[:, :, o0:o0 + 64], win[:, :, 1856 + g * 64:1856 + (g + 1) * 64], writes=[R_w])
                for dc in range(8):
                    rot_weights(wkr[:, dc, :], wk[:, dc, :], 4, R_w)
                t1 = b.sb(ps, [128, 512], F32, "t1")
                t2 = b.sb(ps, [128, 512], F32, "t2")
                R_tmp = Res()
                for tc in range(8):
                    t0 = tc * 512
                    for g in range(2):
                        pa, pra = bank()
                        pb_, prb = bank()
                        for dc in range(8):
                            b.op("pe", lambda e, dc=dc: e.matmul(pa[:, :], lhsT=wk[:, dc, g * 128:(g + 1) * 128], rhs=hT[:, dc, t0:t0 + 512],
                                                                 start=(dc == 0), stop=(dc == 7)), reads=[R_w, R_h], writes=[pra])
                        for dc in range(8):
                            b.op("pe", lambda e, dc=dc: e.matmul(pb_[:, :], lhsT=wkr[:, dc, g * 128:(g + 1) * 128], rhs=hT[:, dc, t0:t0 + 512],
                                                                 start=(dc == 0), stop=(dc == 7)), reads=[R_w, R_h], writes=[prb])
                        rope_evac(kT[:, g, t0:t0 + 512], pa[:, :], pb_[:, :], Ct[:, t0:t0 + 512], St[:, t0:t0 + 512], t1, t2, R_tmp,
                                  [pra, prb, R_T], [R_k])
                for i in range(32):
                    pt, pr = bank()
                    for dc in range(8):
                        b.op("pe", lambda e, dc=dc: e.matmul(pt[:, 0:256], lhsT=hT[:, dc, i * 128:(i + 1) * 128], rhs=wv[:, dc, :],
                                                             start=(dc == 0), stop=(dc == 7)), reads=[R_w, R_h], writes=[pr])
                    b.op("act", lambda e: e.copy(out=vd[:, :, i, :], in_=pt[:, 0:256].rearrange("p (g f) -> p g f", g=2)),
                         reads=[pr], writes=[R_v])
                wq = [b.sb(ps, [128, 8, 128], BF16, "wq%d" % i) for i in range(2)]
                wqr = [b.sb(ps, [128, 8, 128], BF16, "wqr%d" % i) for i in range(2)]
                R_wq = [Res(), Res()]
                qT = [b.sb(ps, [128, S], BF16, "qTb%d" % i) for i in range(2)]
                R_q = [Res(), Res()]
                oTj = [b.sb(ps, [128, S], BF16, "oTb%d" % i) for i in range(2)]
                R_oj = [Res(), Res()]
                Pb = [b.sb(ps, [128, 384], BF16, "Pb%d" % i) for i in range(3)]
                R_P = [Res() for _ in range(3)]
                dn = [b.sb(ps, [128, 128], F32, "dn%d" % i) for i in range(2)]
                R_dn = [Res(), Res()]
                pks = [0, 0]
                for j in range(8):
                    s = j % 2
                    g = j // 4
                    b.dma("pq", wq[s][:], win[:, :, 704 + j * 128:704 + (j + 1) * 128], writes=[R_wq[s]])
                    for dc in range(8):
                        rot_weights(wqr[s][:, dc, :], wq[s][:, dc, :], 2, R_wq[s])
                    for tc in range(8):
                        t0 = tc * 512
                        pa, pra = bank()
                        pb_, prb = bank()
                        for dc in range(8):
                            b.op("pe", lambda e, dc=dc: e.matmul(pa[:, :], lhsT=wq[s][:, dc, :], rhs=hT[:, dc, t0:t0 + 512],
                                                                 start=(dc == 0), stop=(dc == 7)), reads=[R_wq[s], R_h], writes=[pra])
                        for dc in range(8):
                            b.op("pe", lambda e, dc=dc: e.matmul(pb_[:, :], lhsT=wqr[s][:, dc, :], rhs=hT[:, dc, t0:t0 + 512],
                                                                 start=(dc == 0), stop=(dc == 7)), reads=[R_wq[s], R_h], writes=[prb])
                        rope_evac(qT[s][:, t0:t0 + 512], pa[:, :], pb_[:, :], Ct[:, t0:t0 + 512], St[:, t0:t0 + 512], t1, t2, R_tmp,
                                  [pra, prb, R_T], [R_q[s]])
                    for hh in range(2):
                        h = 2 * j + hh
                        p0 = hh * 64
                        p1 = p0 + 64
                        def swaA(qb):
                            kbs = [kb for kb in (qb - 1, qb, qb + 1) if 0 <= kb < 32]
                            n = len(kbs)
                            pS, prS = bank()
                            for idx, kb in enumerate(kbs):
                                b.op("pe", lambda e: e.matmul(pS[:, idx * 128:(idx + 1) * 128], lhsT=kT[p0:p1, g, kb * 128:(kb + 1) * 128],
                                                              rhs=qT[s][p0:p1, qb * 128:(qb + 1) * 128], start=True, stop=(kb == qb)),
                                     reads=[R_k, R_q[s]], writes=[prS])
                                if kb != qb:
                                    m = 0 if kb < qb else 1
                                    b.op("pe", lambda e: e.matmul(pS[:, idx * 128:(idx + 1) * 128], lhsT=identb[:], rhs=maskb[:, m, :],
                                                                  start=False, stop=True), writes=[prS])
                            pi = pks[0] % 3
                            pks[0] += 1
                            b.op("act", lambda e: e.activation(out=Pb[pi][:, 0:n * 128], in_=pS[:, 0:n * 128], func=AF.Exp, scale=0.125),
                                 reads=[prS], writes=[R_P[pi]])
                            return (qb, kbs, pi)

                        def swaB(st_):
                            qb, kbs, pi = st_
                            n = len(kbs)
                            po, pro = bank()
                            pd, prd = bank()
                            for idx, kb in enumerate(kbs):
                                b.op("pe", lambda e: e.matmul(po[:, 0:128], lhsT=vd[:, g, kb, :], rhs=Pb[pi][:, idx * 128:(idx + 1) * 128],
                                                              start=(idx == 0), stop=(idx == n - 1)), reads=[R_v, R_P[pi]], writes=[pro])
                            for idx, kb in enumerate(kbs):
                                b.op("pe", lambda e: e.matmul(pd[:, 0:128], lhsT=onesb[:], rhs=Pb[pi][:, idx * 128:(idx + 1) * 128],
                                                              start=(idx == 0), stop=(idx == n - 1)), reads=[R_P[pi]], writes=[prd])
                            di = pks[1] % 2
                            pks[1] += 1
                            b.op("dve", lambda e: e.tensor_scalar(out=dn[di][p0:p1, :], in0=pd[p0:p1, 0:128], scalar1=es_[p0:p1, h:h + 1], scalar2=None,
                                                                  op0=ALU.add), reads=[prd, R_w], writes=[R_dn[di]])
                            b.op("dve", lambda e: e.reciprocal(out=dn[di][p0:p1, :], in_=dn[di][p0:p1, :]), reads=[R_dn[di]], writes=[R_dn[di]])
                            b.op("dve", lambda e: e.tensor_tensor(out=oTj[s][p0:p1, qb * 128:(qb + 1) * 128], in0=po[p0:p1, 0:128],
                                                                  in1=dn[di][p0:p1, :], op=ALU.mult), reads=[pro, R_dn[di]], writes=[R_oj[s]])

                        nxt = swaA(0)
                        for qb in range(32):
                            cur = nxt
                            if qb + 1 < 32:
                                nxt = swaA(qb + 1)
                            swaB(cur)
                    b.dma("sp", oT_v[:, 8 + j, :], oTj[s][:], reads=[R_oj[s]], writes=[R_o])
                b.barrier()
        with contextlib.ExitStack() as ps:
            Ct, St, R_T = load_tables(ps)
            cqn = b.sb(ps, [128, 3, S], BF16, "cqn")
            ckvn = b.sb(ps, [128, 2, S], BF16, "ckvn")
            krT = b.sb(ps, [128, S], BF16, "krT")
            R_c = Res()
            b.op("pool", lambda e: e.memset(krT[64:128, :], 0.0), writes=[R_c])
            b.dma("sp", cqn[:], mla_v[:, 0:3, :], reads=[R_mla], writes=[R_c])
            b.dma("sp", ckvn[:], mla_v[:, 3:5, :], reads=[R_mla], writes=[R_c])
            b.dma("sp", krT[0:64, :], mla_v[0:64, 5, :], reads=[R_mla], writes=[R_c])
            wqh = [b.sb(ps, [128, 3, 192], BF16, "wqh%d" % i) for i in range(2)]
            wqhr = [b.sb(ps, [128, 3, 64], BF16, "wqhr%d" % i) for i in range(2)]
            wkvh = [b.sb(ps, [128, 2, 256], BF16, "wkvh%d" % i) for i in range(2)]
            R_wh = [Res(), Res()]
            qn = [b.sb(ps, [128, S], BF16, "qn%d" % i) for i in range(2)]
            qr = [b.sb(ps, [128, S], BF16, "qr%d" % i) for i in range(2)]
            kn = [b.sb(ps, [128, S], BF16, "kn%d" % i) for i in range(2)]
            vv = [b.sb(ps, [128, 32, 128], BF16, "vv%d" % i) for i in range(2)]
            R_hd = [Res(), Res()]
            for i in range(2):
                b.op("pool", lambda e: e.memset(qr[i][64:128, :], 0.0), writes=[R_hd[i]])
            oTh = [b.sb(ps, [128, S], BF16, "oTh%d" % i) for i in range(2)]
            R_oh = [Res(), Res()]
            Pm = [b.sb(ps, [128, 512], BF16, "Pm%d" % i) for i in range(4)]
            R_Pm = [Res() for _ in range(4)]
            Pacc = [[b.sb(ps, [128, 512], F32, "Pacc%d%d" % (i, k_)) for k_ in range(2)] for i in range(2)]
            R_Pacc = [[Res(), Res()], [Res(), Res()]]
            dn = [b.sb(ps, [128, 512], F32, "dnm%d" % i) for i in range(2)]
            R_dn = [Res(), Res()]
            t1 = b.sb(ps, [128, 512], F32, "t1")
            t2 = b.sb(ps, [128, 512], F32, "t2")
            R_tmp = Res()
            wqb_v = mla_w_qb[li].rearrange("(c p) f -> p c f", p=128)
            wkvb_v = mla_w_kvb[li].rearrange("(c p) f -> p c f", p=128)
            SC = float(192.0 ** -0.5)
            pkm = [0]
            acc_st = [0]
            s_st = [0]
            for h in range(8):
                s = h % 2
                b.dma("pq", wqh[s][:], wqb_v[:, :, h * 192:(h + 1) * 192], writes=[R_wh[s]])
                b.dma("pq", wkvh[s][:], wkvb_v[:, :, h * 256:(h + 1) * 256], writes=[R_wh[s]])
                for c in range(3):
                    rot_weights(wqhr[s][:, c, :], wqh[s][:, c, 128:192], 1, R_wh[s])
                for tc in range(8):
                    t0 = tc * 512
                    pt, pr = fbank([4, 5], s_st)
                    for c in range(3):
                        b.op("pe", lambda e: e.matmul(pt[:, :], lhsT=wqh[s][:, c, 0:128], rhs=cqn[:, c, t0:t0 + 512], start=(c == 0), stop=(c == 2)),
                             reads=[R_wh[s], R_c], writes=[pr])
                    b.op("act", lambda e: e.copy(out=qn[s][:, t0:t0 + 512], in_=pt[:, :]), reads=[pr], writes=[R_hd[s]])
                    pt, pr = fbank([4, 5], s_st)
                    for c in range(2):
                        b.op("pe", lambda e: e.matmul(pt[:, :], lhsT=wkvh[s][:, c, 0:128], rhs=ckvn[:, c, t0:t0 + 512], start=(c == 0), stop=(c == 1)),
                             reads=[R_wh[s], R_c], writes=[pr])
                    b.op("act", lambda e: e.copy(out=kn[s][:, t0:t0 + 512], in_=pt[:, :]), reads=[pr], writes=[R_hd[s]])
                    pa, pra = fbank([4, 5], s_st)
                    pb_, prb = fbank([4, 5], s_st)
                    for c in range(3):
                        b.op("pe", lambda e: e.matmul(pa[0:64, :], lhsT=wqh[s][:, c, 128:192], rhs=cqn[:, c, t0:t0 + 512], start=(c == 0), stop=(c == 2)),
                             reads=[R_wh[s], R_c], writes=[pra])
                    for c in range(3):
                        b.op("pe", lambda e: e.matmul(pb_[0:64, :], lhsT=wqhr[s][:, c, :], rhs=cqn[:, c, t0:t0 + 512], start=(c == 0), stop=(c == 2)),
                             reads=[R_wh[s], R_c], writes=[prb])
                    rope_evac(qr[s][0:64, t0:t0 + 512], pa[0:64, :], pb_[0:64, :], Ct[0:64, t0:t0 + 512], St[0:64, t0:t0 + 512], t1, t2, R_tmp,
                              [pra, prb, R_T], [R_hd[s]], npart=64)
                for i in range(32):
                    pt, pr = fbank([4, 5], s_st)
                    for c in range(2):
                        b.op("pe", lambda e: e.matmul(pt[:, 0:128], lhsT=ckvn[:, c, i * 128:(i + 1) * 128], rhs=wkvh[s][:, c, 128:256],
                                                      start=(c == 0), stop=(c == 1)), reads=[R_wh[s], R_c], writes=[pr])
                    b.op("act", lambda e: e.copy(out=vv[s][:, i, :], in_=pt[:, 0:128]), reads=[pr], writes=[R_hd[s]])
                for qc in range(8):
                    q0 = qc * 512
                    a = acc_st[0] % 2
                    acc_st[0] += 1
                    po, pro = banks[0 + 2 * a]
                    pd, prd = banks[1 + 2 * a]
                    def emitS(kt):
                        pS, prS = fbank([4, 5], s_st)
                        b.op("pe", lambda e: e.matmul(pS[:, :], lhsT=kn[s][:, kt * 128:(kt + 1) * 128], rhs=qn[s][:, q0:q0 + 512], start=True, stop=False),
                             reads=[R_hd[s]], writes=[prS])
                        b.op("pe", lambda e: e.matmul(pS[:, :], lhsT=krT[:, kt * 128:(kt + 1) * 128], rhs=qr[s][:, q0:q0 + 512], start=False, stop=True),
                             reads=[R_hd[s], R_c], writes=[prS])
                        pi = pkm[0] % 4
                        pkm[0] += 1
                        b.op("act", lambda e: e.activation(out=Pm[pi][:], in_=pS[:, :], func=AF.Exp, scale=SC), reads=[prS], writes=[R_Pm[pi]])
                        return pi

                    nxt = emitS(0)
                    for kt in range(32):
                        pi = nxt
                        if kt + 1 < 32:
                            nxt = emitS(kt + 1)
                        b.op("pe", lambda e: e.matmul(po[:, :], lhsT=vv[s][:, kt, :], rhs=Pm[pi][:], start=(kt == 0), stop=(kt == 31)),
                             reads=[R_hd[s], R_Pm[pi]], writes=[pro])
                        w_ = 1 if (kt % 3 == 2) else 0
                        eng_ = "pool" if w_ else "dve"
                        if kt == 0 or kt == 2:
                            b.op(eng_, lambda e: e.tensor_copy(out=Pacc[a][w_][:], in_=Pm[pi][:]), reads=[R_Pm[pi]], writes=[R_Pacc[a][w_]])
                        else:
                            b.op(eng_, lambda e: e.tensor_tensor(out=Pacc[a][w_][:], in0=Pacc[a][w_][:], in1=Pm[pi][:], op=ALU.add),
                                 reads=[R_Pm[pi], R_Pacc[a][w_]], writes=[R_Pacc[a][w_]])
                    for w_ in range(2):
                        b.op("pe", lambda e: e.matmul(pd[:, :], lhsT=meanm[:], rhs=Pacc[a][w_][:], start=(w_ == 0), stop=(w_ == 1)),
                             reads=[R_Pacc[a][w_]], writes=[prd])
                    b.op("dve", lambda e: e.reciprocal(out=dn[a][:], in_=pd[:, :]), reads=[prd], writes=[R_dn[a]])
                    b.op("dve", lambda e: e.scalar_tensor_tensor(out=oTh[s][:, q0:q0 + 512], in0=po[:, :], scalar=1.0 / D, in1=dn[a][:],
                                                                 op0=ALU.mult, op1=ALU.mult),
                         reads=[pro, R_dn[a]], writes=[R_oh[s]])
                b.dma("sp", oT_v[:, h, :], oTh[s][:], reads=[R_oh[s]], writes=[R_o])
            b.barrier()
        out_proj(l, w_out_ab[li], 16)

    def mixer_odd(l):
        li = l // 2
        win = w_in_c[li].rearrange("(dc p) f -> p dc f", p=128)
        with contextlib.ExitStack() as hs:
            hT = b.sb(hs, [128, 8, S], BF16, "hTo")
            R_h = Res()
            norm_all(l, 0, hT, R_h)
            Ct, St, R_T = load_tables(hs)
            num = b.sb(hs, [128, S], F32, "num")
            den = b.sb(hs, [128, S], F32, "den")
            R_num = Res()
            R_den = Res()
            oTj = b.sb(hs, [128, S], BF16, "oTc")
            R_oj = Res()
            t1 = b.sb(hs, [128, 512], F32, "t1")
            t2 = b.sb(hs, [128, 512], F32, "t2")
            R_tmp = Res()
            wts = [b.sb(hs, [128, 8, 128], BF16, "wc%d" % i) for i in range(5)]
            R_w = Res()
            KPMAX = S + 128 * 16
            qP = b.sb(hs, [128, S], BF16, "qP")
            kP = b.sb(hs, [128, KPMAX], BF16, "kP")
            vP = b.sb(hs, [128, KPMAX], BF16, "vP")
            Vt = b.sb(hs, [128, KPMAX // 128, 128], BF16, "Vt")
            R_q = Res()
            R_k = Res()
            R_vp = Res()
            R_vt = Res()
            Pb = [b.sb(hs, [128, 256], BF16, "Pc%d" % i) for i in range(3)]
            R_P = [Res() for _ in range(3)]
            pkd = [0]
            def load_w(j_, g_):
                base = g_ * 3072 + j_ * 128
                b.dma("pq", wts[0][:], win[:, :, base:base + 128], writes=[R_w])
                b.dma("pq", wts[2][:], win[:, :, base + 1024:base + 1152], writes=[R_w])
                b.dma("pq", wts[4][:], win[:, :, base + 2048:base + 2176], writes=[R_w])
                for dc in range(8):
                    rot_weights(wts[1][:, dc, :], wts[0][:, dc, :], 2, R_w)
                    rot_weights(wts[3][:, dc, :], wts[2][:, dc, :], 2, R_w)

            load_w(0, 0)
            for j in range(8):
                for g, d in enumerate((1, 4, 16)):
                    L = S // d
                    Lp = L + 128
                    nt = L // 128 + 1
                    nq = L // 128
                    qv = qP[:, :].rearrange("p (r m) -> p r m", r=d)
                    kv = kP[:, 0:d * Lp].rearrange("p (r m) -> p r m", r=d)
                    vv_ = vP[:, 0:d * Lp].rearrange("p (r m) -> p r m", r=d)
                    b.op("pool", lambda e: e.memset(kv[:, :, 0:64], 0.0), writes=[R_k])
                    b.op("pool", lambda e: e.memset(kv[:, :, 64 + L:Lp], 0.0), writes=[R_k])
                    b.op("pool", lambda e: e.memset(vv_[:, :, 0:64], 0.0), writes=[R_vp])
                    b.op("pool", lambda e: e.memset(vv_[:, :, 64 + L:Lp], 0.0), writes=[R_vp])
                    for tc in range(8):
                        t0 = tc * 512
                        m0 = t0 // d
                        mw = 512 // d
                        prs = []
                        for wi in range(5):
                            pt, pr = bank()
                            for dc in range(8):
                                b.op("pe", lambda e, dc=dc: e.matmul(pt[:, :], lhsT=wts[wi][:, dc, :], rhs=hT[:, dc, t0:t0 + 512],
                                                                     start=(dc == 0), stop=(dc == 7)), reads=[R_w, R_h], writes=[pr])
                            prs.append((pt, pr))
                            if wi == 1:
                                rope_evac(qv[:, :, m0:m0 + mw], prs[0][0][:, :], prs[1][0][:, :], Ct[:, t0:t0 + 512], St[:, t0:t0 + 512],
                                          t1, t2, R_tmp, [prs[0][1], prs[1][1], R_T], [R_q], perm=d)
                            if wi == 3:
                                rope_evac(kv[:, :, 64 + m0:64 + m0 + mw], prs[2][0][:, :], prs[3][0][:, :], Ct[:, t0:t0 + 512], St[:, t0:t0 + 512],
                                          t1, t2, R_tmp, [prs[2][1], prs[3][1], R_T], [R_k], perm=d)
                            if wi == 4:
                                b.op("act", lambda e: e.copy(out=vv_[:, :, 64 + m0:64 + m0 + mw],
                                                             in_=pt[:, :].rearrange("p (m r) -> p r m", r=d)), reads=[pr], writes=[R_vp])
                    if not (j == 7 and g == 2):
                        load_w(j + (g + 1) // 3, (g + 1) % 3)
                    ntile = d * nt
                    for i0 in range(0, ntile, 8):
                        nn = min(8, ntile - i0)
                        pbt, pbr = bbank()
                        for ii in range(nn):
                            i = i0 + ii
                            r_, jt = divmod(i, nt)
                            c0 = r_ * Lp + jt * 128
                            b.op("pe", lambda e: e.transpose(out=pbt[:, ii * 128:(ii + 1) * 128], in_=vP[:, c0:c0 + 128], identity=identb[:]),
                                 reads=[R_vp], writes=[pbr])
                        b.op("act", lambda e: e.copy(out=Vt[:, i0:i0 + nn, :], in_=pbt[:, 0:nn * 128].rearrange("p (i f) -> p i f", f=128)),
                             reads=[pbr], writes=[R_vt])
                    for hh in range(2):
                        p0 = hh * 64
                        p1 = p0 + 64
                        def dilA(r_, n):
                            q0 = r_ * L + n * 128
                            k0 = r_ * Lp + n * 128
                            pS, prS = bank()
                            for idx in range(2):
                                if idx == 0:
                                    m = 2 if n == 0 else 0
                                else:
                                    m = 3 if n == nq - 1 else 1
                                b.op("pe", lambda e: e.matmul(pS[:, idx * 128:(idx + 1) * 128], lhsT=kP[p0:p1, k0 + idx * 128:k0 + (idx + 1) * 128],
                                                              rhs=qP[p0:p1, q0:q0 + 128], start=True, stop=False),
                                     reads=[R_k, R_q], writes=[prS])
                                b.op("pe", lambda e: e.matmul(pS[:, idx * 128:(idx + 1) * 128], lhsT=identb[:], rhs=maskb[:, m, :],
                                                              start=False, stop=True), writes=[prS])
                            pi = pkd[0] % 3
                            pkd[0] += 1
                            b.op("act", lambda e: e.activation(out=Pb[pi][:], in_=pS[:, 0:256], func=AF.Exp, scale=0.125),
                                 reads=[prS], writes=[R_P[pi]])
                            return (r_, n, pi)

                        def dilB(st_):
                            r_, n, pi = st_
                            po, pro = bank()
                            pd, prd = bank()
                            ti = r_ * nt + n
                            for idx in range(2):
                                b.op("pe", lambda e: e.matmul(po[:, 0:128], lhsT=Vt[:, ti + idx, :], rhs=Pb[pi][:, idx * 128:(idx + 1) * 128],
                                                              start=(idx == 0), stop=(idx == 1)), reads=[R_vt, R_P[pi]], writes=[pro])
                            for idx in range(2):
                                b.op("pe", lambda e: e.matmul(pd[:, 0:128], lhsT=onesb[:], rhs=Pb[pi][:, idx * 128:(idx + 1) * 128],
                                                              start=(idx == 0), stop=(idx == 1)), reads=[R_P[pi]], writes=[prd])
                            nv = num[p0:p1, :].rearrange("p (m r) -> p r m", r=d)[:, r_, n * 128:(n + 1) * 128]
                            dv = den[p0:p1, :].rearrange("p (m r) -> p r m", r=d)[:, r_, n * 128:(n + 1) * 128]
                            if g == 0:
                                b.op("act", lambda e: e.copy(out=nv, in_=po[p0:p1, 0:128]), reads=[pro], writes=[R_num])
                                b.op("dve", lambda e: e.tensor_copy(out=dv, in_=pd[p0:p1, 0:128]), reads=[prd], writes=[R_den])
                            else:
                                b.op("dve", lambda e: e.tensor_tensor(out=nv, in0=po[p0:p1, 0:128], in1=nv, op=ALU.add), reads=[pro, R_num], writes=[R_num])
                                b.op("dve", lambda e: e.tensor_tensor(out=dv, in0=pd[p0:p1, 0:128], in1=dv, op=ALU.add), reads=[prd, R_den], writes=[R_den])

                        ulist = [(r_, n) for r_ in range(d) for n in range(nq)]
                        nxt = dilA(*ulist[0])
                        for ui in range(len(ulist)):
                            cur = nxt
                            if ui + 1 < len(ulist):
                                nxt = dilA(*ulist[ui + 1])
                            dilB(cur)
                for q4 in range(8):
                    sl = slice(q4 * 512, (q4 + 1) * 512)
                    b.op("dve", lambda e: e.reciprocal(out=den[:, sl], in_=den[:, sl]), reads=[R_den], writes=[R_den])
                    b.op("dve", lambda e: e.tensor_tensor(out=oTj[:, sl], in0=num[:, sl], in1=den[:, sl], op=ALU.mult), reads=[R_num, R_den], writes=[R_oj])
                b.dma("sp", oT_v[:, j, :], oTj[:], reads=[R_oj], writes=[R_o])
            b.barrier()
        out_proj(l, w_out_c[li], 8)

    for l in layers:
        if cfg["mixer"]:
            if l % 2 == 0:
                mixer_even(l)
            else:
                mixer_odd(l)
        if cfg["ffn"]:
            moe_layer(l)

    with contextlib.ExitStack() as ps:
        xc = [b.sb(ps, [128, 8, 512], F32, "fx%d" % i) for i in range(2)]
        sq = b.sb(ps, [128, 8, 512], F32, "fsq")
        rs = b.sb(ps, [128, 512], F32, "frs")
        yo = [b.sb(ps, [128, D], F32, "fy%d" % i) for i in range(2)]
        R_xc = [Res(), Res()]
        R_sq = Res()
        R_rs = Res()
        R_yo = [Res(), Res()]
        k = 0
        for tc in range(8):
            s = tc % 2
            t0 = tc * 512
            b.dma("sp", xc[s][:], xT_v[:, :, t0:t0 + 512], reads=[R_x], writes=[R_xc[s]])
            b.op("act", lambda e: e.activation(out=sq[:], in_=xc[s][:], func=AF.Square), reads=[R_xc[s]], writes=[R_sq])
            pt, pr = bank()
            for dc in range(8):
                b.op("pe", lambda e, dc=dc: e.matmul(pt[:, :], lhsT=meanm[:], rhs=sq[:, dc, :], start=(dc == 0), stop=(dc == 7)),
                     reads=[R_sq], writes=[pr])
            b.op("act", lambda e: e.activation(out=rs[:], in_=pt[:, :], func=AF.Sqrt, bias=epsc[:], scale=1.0),
                 reads=[pr], writes=[R_rs])
            b.op("dve", lambda e: e.reciprocal(out=rs[:], in_=rs[:]), reads=[R_rs], writes=[R_rs])
            for dc in range(8):
                b.op("dve", lambda e, dc=dc: e.scalar_tensor_tensor(out=xc[s][:, dc, :], in0=xc[s][:, dc, :], scalar=gfin[:, dc:dc + 1],
                                                                    in1=rs[:], op0=ALU.mult, op1=ALU.mult),
                     reads=[R_rs, R_xc[s]], writes=[R_xc[s]])
            for i in range(4):
                ys = k % 2
                k += 1
                for hb in range(2):
                    pt, pr = bank()
                    for cc in range(4):
                        dc = hb * 4 + cc
                        b.op("pe", lambda e, dc=dc, cc=cc: e.transpose(out=pt[:, cc * 128:(cc + 1) * 128], in_=xc[s][:, dc, i * 128:(i + 1) * 128],
                                                                       identity=identf[:]),
                             reads=[R_xc[s]], writes=[pr])
                    if hb == 0:
                        b.op("dve", lambda e: e.tensor_copy(out=yo[ys][:, 0:512], in_=pt[:, :]), reads=[pr], writes=[R_yo[ys]])
                    else:
                        b.op("act", lambda e: e.copy(out=yo[ys][:, 512:1024], in_=pt[:, :]), reads=[pr], writes=[R_yo[ys]])
                r0 = t0 + i * 128
                b.dma("sp", y_out[r0:r0 + 128, :], yo[ys][:], reads=[R_yo[ys]], writes=[R_o])
        b.barrier()
    return nc


def make_consts():
    identf = np.eye(128, dtype=np.float32)
    i = np.arange(128) % 32
    inv = (10000.0 ** (-(2.0 * i.astype(np.float32)) / 64.0)).astype(np.float32).reshape(128, 1)
    a = np.arange(128)[:, None]
    bq = np.arange(128)[None, :]
    NEG = -30000.0
    masks = np.zeros((128, 4, 128), dtype=np.float32)
    masks[:, 0, :] = np.where(a >= bq, 0.0, NEG)
    masks[:, 1, :] = np.where(a <= bq, 0.0, NEG)
    masks[:, 2, :] = np.where((a >= bq) & (a >= 64), 0.0, NEG)
    masks[:, 3, :] = np.where((a <= bq) & (a < 64), 0.0, NEG)
    return {"k_identf": identf, "k_inv": inv, "k_masks": masks}


_CACHE = {}


def kernel(**inputs):
    cfg = CFG
    ncores = cfg["ncores"]
    key = repr(cfg)
    if key not in _CACHE:
        _CACHE[key] = build(cfg)
    nc = _CACHE[key]
    consts = make_consts()
    shared = {}
    for k_, v in inputs.items():
        if k_ in ("x", "c", "positions"):
            continue
        shared[k_] = np.ascontiguousarray(v)
    in_maps = []
    for cidx in range(ncores):
        m = dict(shared)
        m.update(consts)
        m["x"] = np.ascontiguousarray(inputs["x"][cidx])
        m["c"] = np.ascontiguousarray(inputs["c"][cidx])
        m["positions"] = np.ascontiguousarray(inputs["positions"][cidx]).astype(np.int32)
        in_maps.append(m)
    res = run_bass_kernel_spmd(nc, in_maps, core_ids=list(range(ncores)))
    out = np.stack([np.asarray(r["y"]) for r in res.results], axis=0)
    if ncores < 8:
        full = np.zeros((8, S, D), dtype=np.float32)
        full[:ncores] = out
        out = full
    return out.astype(np.float32)
```
